# Optimizing a Trainium2 kernel written in Bass

```python
import jax
import jax.numpy as jnp
from jax import lax
import numpy as np


D_MODEL = 2048
BATCH = 4
SEQ = 4096
DEPTH = 2

HEAD_DIM = 128
NSA_HEADS = 8
NSA_KV_HEADS = 2
NSA_GROUP = NSA_HEADS // NSA_KV_HEADS
NSA_WIDTH = NSA_HEADS * HEAD_DIM
NSA_KV_WIDTH = NSA_KV_HEADS * HEAD_DIM
CMP_LEN = 32
CMP_STRIDE = 16
SLC_LEN = 64
SLC_TOPK = 16
WIN = 512
SEL_QBLOCK = 64
WIN_QBLOCK = 128
GMLP_GROUPS = 4
GMLP_CHUNK = 128
GMLP_WIDTH = GMLP_GROUPS * HEAD_DIM
RET_HEADS = 4
RET_CHUNK = 128
RET_WIDTH = RET_HEADS * HEAD_DIM
MIX_WIDTH = NSA_WIDTH + GMLP_WIDTH + RET_WIDTH
IN_SPLITS = (NSA_WIDTH,) + (NSA_KV_WIDTH,) * 6 + (3 * NSA_HEADS, 2 * GMLP_WIDTH) + (RET_WIDTH,) * 4
IN_WIDTH = NSA_WIDTH + 6 * NSA_KV_WIDTH + 3 * NSA_HEADS + 2 * GMLP_WIDTH + 4 * RET_WIDTH
DENSE_FF = 5632
N_EXPERTS = 8
TOP_K = 2
MOE_FF = 2816
EPS = 1e-6
NEG_INF = -1e30
FORCE_SCORE = 1e4

kernel_name = 'hybrid_nsa_gmlp_retention_moe'


def rmsnorm(x, g):
    xf = x.astype(jnp.float32)
    y = xf * lax.rsqrt(jnp.mean(xf * xf, axis=-1, keepdims=True) + EPS)
    return (y * g.astype(jnp.float32)).astype(x.dtype)


def layernorm(x, g, b):
    xf = x.astype(jnp.float32)
    mu = jnp.mean(xf, axis=-1, keepdims=True)
    var = jnp.mean(jnp.square(xf - mu), axis=-1, keepdims=True)
    y = (xf - mu) * lax.rsqrt(var + EPS)
    return (y * g.astype(jnp.float32) + b.astype(jnp.float32)).astype(x.dtype)


def masked_softmax(s, mask):
    p = jax.nn.softmax(jnp.where(mask, s, NEG_INF), axis=-1)
    return jnp.where(mask, p, 0.0)


def alibi_slopes():
    h = jnp.arange(NSA_HEADS, dtype=jnp.float32)
    return jnp.exp2(-8.0 * (h + 1.0) / NSA_HEADS).reshape(NSA_KV_HEADS, NSA_GROUP)


def to_heads(z, n):
    b, t, _ = z.shape
    return z.reshape(b, t, n, HEAD_DIM).transpose(0, 2, 1, 3)


def nsa_compress(k, pe, w1, w2):
    b, g, t, d = k.shape
    nc = (t - CMP_LEN) // CMP_STRIDE + 1
    idx = jnp.arange(nc)[:, None] * CMP_STRIDE + jnp.arange(CMP_LEN)[None, :]
    blk = (k[:, :, idx] + pe).reshape(b, g, nc, CMP_LEN * d)
    return jax.nn.gelu(blk @ w1) @ w2


def nsa_compressed_attn(q, kc, vc, slopes):
    t = q.shape[3]
    nc = kc.shape[2]
    end = jnp.arange(nc) * CMP_STRIDE + (CMP_LEN - 1)
    dist = (jnp.arange(t)[:, None] - end[None, :]).astype(jnp.float32)
    s = jnp.einsum('bgrtd,bgcd->bgrtc', q, kc).astype(jnp.float32) * (HEAD_DIM ** -0.5)
    s = s - slopes[None, :, :, None, None] * dist
    p = masked_softmax(s, dist >= 0)
    o = jnp.einsum('bgrtc,bgcd->bgrtd', p.astype(vc.dtype), vc)
    return o, p


def nsa_select_blocks(p_cmp, t):
    nc = p_cmp.shape[-1]
    nb = t // SLC_LEN
    c0 = jnp.arange(nc)[:, None] * CMP_STRIDE
    s0 = jnp.arange(nb)[None, :] * SLC_LEN
    overlap = jnp.minimum(c0 + CMP_LEN, s0 + SLC_LEN) - jnp.maximum(c0, s0)
    share = jnp.clip(overlap, 0, CMP_LEN).astype(jnp.float32) / CMP_LEN
    imp = jnp.einsum('bgrtc,cj->bgtj', p_cmp, share)
    cur = (jnp.arange(t) // SLC_LEN)[:, None]
    j = jnp.arange(nb)[None, :]
    forced = (j == 0) | (j == cur) | (j == cur - 1)
    score = jnp.where(forced, FORCE_SCORE, imp)
    score = jnp.where(j > cur, -jnp.inf, score)
    _, idx = lax.top_k(score, min(SLC_TOPK, nb))
    return idx


def nsa_selected_attn(q, k, v, idx, slopes):
    b, g, r, t, d = q.shape
    nb = t // SLC_LEN
    n = idx.shape[-1]
    nq = t // SEL_QBLOCK
    kb = k.reshape(b, g, nb, SLC_LEN, d)
    vb = v.reshape(b, g, nb, SLC_LEN, d)
    qs = q.reshape(b, g, r, nq, SEL_QBLOCK, d).transpose(3, 0, 1, 2, 4, 5)
    ids = idx.reshape(b, g, nq, SEL_QBLOCK, n).transpose(2, 0, 1, 3, 4)
    ts = jnp.arange(t).reshape(nq, SEL_QBLOCK)
    bi = jnp.arange(b)[:, None, None, None]
    gi = jnp.arange(g)[None, :, None, None]
    scale = HEAD_DIM ** -0.5

    def one_block(args):
        qb, ib, tb = args
        kg = kb[bi, gi, ib]
        vg = vb[bi, gi, ib].reshape(b, g, SEL_QBLOCK, n * SLC_LEN, d)
        pos = ib[..., None] * SLC_LEN + jnp.arange(SLC_LEN)
        dist = (tb[None, None, :, None, None] - pos).reshape(b, g, 1, SEL_QBLOCK, n * SLC_LEN)
        s = jnp.einsum('bgrqd,bgqnld->bgrqnl', qb, kg).astype(jnp.float32) * scale
        s = s.reshape(b, g, r, SEL_QBLOCK, n * SLC_LEN)
        s = s - slopes[None, :, :, None, None] * dist.astype(jnp.float32)
        p = masked_softmax(s, dist >= 0)
        return jnp.einsum('bgrqk,bgqkd->bgrqd', p.astype(vg.dtype), vg)

    out = lax.map(one_block, (qs, ids, ts))
    return out.transpose(1, 2, 3, 0, 4, 5).reshape(b, g, r, t, d)


def nsa_window_attn(q, k, v, slopes):
    b, g, r, t, d = q.shape
    nq = t // WIN_QBLOCK
    span = WIN + WIN_QBLOCK
    band = jnp.arange(nq)[:, None] * WIN_QBLOCK + jnp.arange(span)[None, :]
    pad = ((0, 0), (0, 0), (WIN, 0), (0, 0))
    kb = jnp.pad(k, pad)[:, :, band]
    vb = jnp.pad(v, pad)[:, :, band]
    spos = band - WIN
    tpos = jnp.arange(t).reshape(nq, WIN_QBLOCK)
    dist = tpos[:, :, None] - spos[:, None, :]
    mask = (spos[:, None, :] >= 0) & (dist >= 0) & (dist < WIN)
    qb = q.reshape(b, g, r, nq, WIN_QBLOCK, d)
    s = jnp.einsum('bgrnqd,bgnkd->bgrnqk', qb, kb).astype(jnp.float32) * (HEAD_DIM ** -0.5)
    s = s - slopes[None, :, :, None, None, None] * dist.astype(jnp.float32)
    p = masked_softmax(s, mask)
    o = jnp.einsum('bgrnqk,bgnkd->bgrnqd', p.astype(vb.dtype), vb)
    return o.reshape(b, g, r, t, d)


def nsa_mixer(q, k_cmp, v_cmp, k_slc, v_slc, k_win, v_win, gates, pe_k, w1_k, w2_k, pe_v, w1_v, w2_v):
    slopes = alibi_slopes()
    kc = nsa_compress(k_cmp, pe_k, w1_k, w2_k)
    vc = nsa_compress(v_cmp, pe_v, w1_v, w2_v)
    o_cmp, p_cmp = nsa_compressed_attn(q, kc, vc, slopes)
    idx = nsa_select_blocks(p_cmp, q.shape[3])
    o_slc = nsa_selected_attn(q, k_slc, v_slc, idx, slopes)
    o_win = nsa_window_attn(q, k_win, v_win, slopes)
    return gates[..., 0:1] * o_cmp + gates[..., 1:2] * o_slc + gates[..., 2:3] * o_win


def gmlp_gate(uv, ln_g, ln_b, ws, bs):
    b, t, _ = uv.shape
    u, v = jnp.split(jax.nn.gelu(uv), 2, axis=-1)
    v = layernorm(v, ln_g, ln_b).reshape(b, t // GMLP_CHUNK, GMLP_CHUNK, GMLP_GROUPS, HEAD_DIM)
    w = ws * jnp.tril(jnp.ones((GMLP_CHUNK, GMLP_CHUNK), ws.dtype))
    s = jnp.einsum('gts,bcsgd->bctgd', w, v) + bs.T[None, None, :, :, None]
    return u * s.reshape(b, t, GMLP_WIDTH)


def retention(q, k, v, gate, gn_g):
    b, t, _ = q.shape
    h, d, c = RET_HEADS, HEAD_DIM, RET_CHUNK
    nc = t // c
    dt = q.dtype
    qc = to_heads(q, h).reshape(b, h, nc, c, d)
    kc = (to_heads(k, h) * (d ** -0.5)).reshape(b, h, nc, c, d)
    vc = to_heads(v, h).reshape(b, h, nc, c, d)
    lg = jnp.log1p(-jnp.exp2(-5.0 - jnp.arange(h, dtype=jnp.float32)))
    n = jnp.arange(c, dtype=jnp.float32)
    rel = n[:, None] - n[None, :]
    decay_in = jnp.where(rel >= 0, jnp.exp(lg[:, None, None] * jnp.maximum(rel, 0.0)), 0.0).astype(dt)
    zeta = jnp.exp(lg[:, None] * (c - 1.0 - n)).astype(dt)
    xi = jnp.exp(lg[:, None] * (n + 1.0)).astype(dt)
    decay_chunk = jnp.exp(lg * c).astype(dt)
    scores = jnp.einsum('bhcnd,bhcmd->bhcnm', qc, kc) * decay_in[None, :, None]
    inner = jnp.einsum('bhcnm,bhcme->bhcne', scores, vc)
    kv = jnp.einsum('bhcmd,bhcme->cbhde', kc * zeta[None, :, None, :, None], vc)

    def step(state, kv_c):
        return decay_chunk[None, :, None, None] * state + kv_c, state

    _, prev = lax.scan(step, jnp.zeros((b, h, d, d), dt), kv)
    cross = jnp.einsum('bhcnd,cbhde->bhcne', qc, prev) * xi[None, :, None, :, None]
    y = (inner + cross).reshape(b, h, t, d).astype(jnp.float32)
    mu = jnp.mean(y, axis=-1, keepdims=True)
    var = jnp.mean(jnp.square(y - mu), axis=-1, keepdims=True)
    y = (y - mu) * lax.rsqrt(var + EPS)
    y = y.transpose(0, 2, 1, 3).reshape(b, t, h * d) * gn_g.astype(jnp.float32)
    return jax.nn.silu(gate) * y.astype(dt)


def token_mixer(a, w_in, pe_k, w1_k, w2_k, pe_v, w1_v, w2_v, g_ln_g, g_ln_b, g_ws, g_bs, ret_gn_g, w_out):
    b, t, _ = a.shape
    proj = a @ w_in
    parts = []
    off = 0
    for width in IN_SPLITS:
        parts.append(proj[..., off:off + width])
        off += width
    q, kcmp, vcmp, kslc, vslc, kwin, vwin, gate_logits, uv, rq, rk, rv, rg = parts
    qh = to_heads(q, NSA_HEADS).reshape(b, NSA_KV_HEADS, NSA_GROUP, t, HEAD_DIM)
    gates = jax.nn.sigmoid(gate_logits).reshape(b, t, NSA_KV_HEADS, NSA_GROUP, 3).transpose(0, 2, 3, 1, 4)
    o = nsa_mixer(qh, to_heads(kcmp, NSA_KV_HEADS), to_heads(vcmp, NSA_KV_HEADS),
                  to_heads(kslc, NSA_KV_HEADS), to_heads(vslc, NSA_KV_HEADS),
                  to_heads(kwin, NSA_KV_HEADS), to_heads(vwin, NSA_KV_HEADS),
                  gates, pe_k, w1_k, w2_k, pe_v, w1_v, w2_v)
    o_nsa = o.reshape(b, NSA_HEADS, t, HEAD_DIM).transpose(0, 2, 1, 3).reshape(b, t, NSA_WIDTH)
    o_gmlp = gmlp_gate(uv, g_ln_g, g_ln_b, g_ws, g_bs)
    o_ret = retention(rq, rk, rv, rg, ret_gn_g)
    return jnp.concatenate([o_nsa, o_gmlp, o_ret], axis=-1) @ w_out


def swiglu(x, w1, w3, w2):
    return (jax.nn.silu(x @ w1) * (x @ w3)) @ w2


def moe_swiglu(x, w_r, b_r, w1, w3, w2):
    b, t, d = x.shape
    xf = x.reshape(b * t, d)
    logits = (xf @ w_r + b_r).astype(jnp.float32)
    top_v, top_i = lax.top_k(logits, TOP_K)
    gate = jax.nn.softmax(top_v, axis=-1)
    combine = jnp.sum(jax.nn.one_hot(top_i, N_EXPERTS, dtype=jnp.float32) * gate[..., None], axis=1).astype(x.dtype)
    out = jnp.zeros_like(xf)
    for e in range(N_EXPERTS):
        out = out + combine[:, e:e + 1] * swiglu(xf, w1[e], w3[e], w2[e])
    return out.reshape(b, t, d)


def setup_inputs(seed: int = 0) -> dict:
    key = jax.random.key(seed)
    ks = jax.random.split(key, 32)
    counter = [0]

    def nrm(shape, scale):
        k = ks[counter[0]]
        counter[0] += 1
        return jax.random.normal(k, shape, jnp.float32) * scale

    n_dense = (DEPTH + 1) // 2
    n_moe = DEPTH // 2
    L = DEPTH
    cf = CMP_LEN * HEAD_DIM
    return {
        'x': nrm((BATCH, SEQ, D_MODEL), 1.0),
        'ln1_g': 1.0 + nrm((L, D_MODEL), 0.01),
        'w_in': nrm((L, D_MODEL, IN_WIDTH), D_MODEL ** -0.5),
        'cmp_pe_k': nrm((L, CMP_LEN, HEAD_DIM), 0.1),
        'cmp_w1_k': nrm((L, cf, HEAD_DIM), cf ** -0.5),
        'cmp_w2_k': nrm((L, HEAD_DIM, HEAD_DIM), HEAD_DIM ** -0.5),
        'cmp_pe_v': nrm((L, CMP_LEN, HEAD_DIM), 0.1),
        'cmp_w1_v': nrm((L, cf, HEAD_DIM), cf ** -0.5),
        'cmp_w2_v': nrm((L, HEAD_DIM, HEAD_DIM), HEAD_DIM ** -0.5),
        'gmlp_ln_g': 1.0 + nrm((L, GMLP_WIDTH), 0.01),
        'gmlp_ln_b': nrm((L, GMLP_WIDTH), 0.01),
        'gmlp_ws': nrm((L, GMLP_GROUPS, GMLP_CHUNK, GMLP_CHUNK), GMLP_CHUNK ** -0.5),
        'gmlp_bs': 1.0 + nrm((L, GMLP_GROUPS, GMLP_CHUNK), 0.01),
        'ret_gn_g': 1.0 + nrm((L, RET_WIDTH), 0.01),
        'w_out': nrm((L, MIX_WIDTH, D_MODEL), MIX_WIDTH ** -0.5),
        'ln2_g': 1.0 + nrm((L, D_MODEL), 0.01),
        'ffn_w1': nrm((n_dense, D_MODEL, DENSE_FF), D_MODEL ** -0.5),
        'ffn_w3': nrm((n_dense, D_MODEL, DENSE_FF), D_MODEL ** -0.5),
        'ffn_w2': nrm((n_dense, DENSE_FF, D_MODEL), DENSE_FF ** -0.5),
        'moe_wr': nrm((n_moe, D_MODEL, N_EXPERTS), D_MODEL ** -0.5),
        'moe_br': nrm((n_moe, N_EXPERTS), 0.01),
        'moe_w1': nrm((n_moe, N_EXPERTS, D_MODEL, MOE_FF), D_MODEL ** -0.5),
        'moe_w3': nrm((n_moe, N_EXPERTS, D_MODEL, MOE_FF), D_MODEL ** -0.5),
        'moe_w2': nrm((n_moe, N_EXPERTS, MOE_FF, D_MODEL), MOE_FF ** -0.5),
        'final_g': 1.0 + nrm((D_MODEL,), 0.01),
    }


def reference(x, ln1_g, w_in, cmp_pe_k, cmp_w1_k, cmp_w2_k, cmp_pe_v, cmp_w1_v, cmp_w2_v,
              gmlp_ln_g, gmlp_ln_b, gmlp_ws, gmlp_bs, ret_gn_g, w_out, ln2_g,
              ffn_w1, ffn_w3, ffn_w2, moe_wr, moe_br, moe_w1, moe_w3, moe_w2, final_g):
    h = x
    for layer in range(DEPTH):
        a = rmsnorm(h, ln1_g[layer])
        h = h + token_mixer(a, w_in[layer], cmp_pe_k[layer], cmp_w1_k[layer], cmp_w2_k[layer],
                            cmp_pe_v[layer], cmp_w1_v[layer], cmp_w2_v[layer],
                            gmlp_ln_g[layer], gmlp_ln_b[layer], gmlp_ws[layer], gmlp_bs[layer],
                            ret_gn_g[layer], w_out[layer])
        f = rmsnorm(h, ln2_g[layer])
        i = layer // 2
        if layer % 2 == 0:
            h = h + swiglu(f, ffn_w1[i], ffn_w3[i], ffn_w2[i])
        else:
            h = h + moe_swiglu(f, moe_wr[i], moe_br[i], moe_w1[i], moe_w3[i], moe_w2[i])
    return rmsnorm(h, final_g)
```

```python
import contextlib
import numpy as np
import ml_dtypes
import concourse.bass as bass
import concourse.mybir as mybir
from concourse.bass_utils import run_bass_kernel_spmd

F32 = mybir.dt.float32
BF16 = mybir.dt.bfloat16
AF = mybir.ActivationFunctionType
ALU = mybir.AluOpType
AX = mybir.AxisListType
NPBF = ml_dtypes.bfloat16

NCORES = 8
D = 2048
KC = 16
B, T = 4, 4096
TOK = 2048
EPS = 1e-6
NRING = 4
SCALE = 128.0 ** -0.5


class Ctx:
    def __init__(self, nc, stack, pfx="", semstack=None):
        self.nc = nc
        self.stack = stack
        self.pfx = pfx
        self.fused = semstack is not None
        self.all_sems = []

        def mksem(name):
            if self.fused:
                h = nc.alloc_semaphore(name=name)
                self.all_sems.append(h)
                return h
            return stack.enter_context(nc.semaphore(name))
        self.E = {"pe": nc.tensor, "act": nc.scalar, "dve": nc.vector,
                  "pool": nc.gpsimd, "sp": nc.sync}
        self.csem = {}
        self.ccnt = {}
        for e in ("pe", "act", "dve", "pool"):
            self.csem[e] = mksem(pfx + "c_" + e)
            self.ccnt[e] = 0
        self.dsem = {}
        self.dcnt = {}
        for q in ("sp", "pool"):
            self.dsem[q] = [mksem(pfx + "d_%s%d" % (q, i)) for i in range(NRING)]
            self.dcnt[q] = 0
        self.waited = {e: {} for e in self.E}
        self.writer = {}
        self.readers = {}
        self.n_wait = 0
        self.n_ins = 0
        self.psum_names = set()
        self.psum_last = {}

    def sb(self, name, shape, dt):
        return self.stack.enter_context(self.nc.sbuf_tensor(self.pfx + name, shape, dt))

    def ps(self, name, shape, dt=F32):
        t = self.stack.enter_context(self.nc.psum_tensor(self.pfx + name, shape, dt))
        self.psum_names.add(t.name)
        return t

    @staticmethod
    def key(x):
        if isinstance(x, (str, tuple)):
            return x
        t = getattr(x, "tensor", None)
        if t is not None:
            return t.name
        return x.name

    def _need(self, e, ev, skip_same=None):
        if ev is None:
            return
        sem, val, src = ev
        if src == skip_same:
            return
        w = self.waited[e]
        k = id(sem)
        if w.get(k, 0) >= val:
            return
        self.E[e].wait_ge(sem, val)
        self.n_wait += 1
        w[k] = val

    def _base(self, b):
        k = self.key(b)
        return k[0] if isinstance(k, tuple) else k

    def _sync(self, e, reads, writes, same_ok=False):
        skip = e if same_ok else None
        for b in list(reads) + list(writes):
            base = self._base(b)
            if base in self.psum_names:
                for e2, ev in self.psum_last.get(base, {}).items():
                    if e2 != e:
                        self._need(e, ev, skip)
        for b in reads:
            self._need(e, self.writer.get(self.key(b)), skip)
        for b in writes:
            k = self.key(b)
            self._need(e, self.writer.get(k), skip)
            for ev in self.readers.get(k, {}).values():
                self._need(e, ev, skip)

    def _record(self, ev, reads, writes):
        for b in list(reads) + list(writes):
            base = self._base(b)
            if base in self.psum_names:
                self.psum_last.setdefault(base, {})[ev[2]] = ev
        for b in reads:
            self.readers.setdefault(self.key(b), {})[id(ev[0])] = ev
        for b in writes:
            k = self.key(b)
            self.writer[k] = ev
            self.readers[k] = {}

    def op(self, e, name, *args, reads=(), writes=(), **kw):
        self._sync(e, reads, writes, same_ok=(e == "pe"))
        ins = getattr(self.E[e], name)(*args, **kw)
        self.ccnt[e] += 1
        ins.then_inc(self.csem[e], 1)
        ev = (self.csem[e], self.ccnt[e], e)
        self._record(ev, reads, writes)
        self.n_ins += 1
        return ins

    def dma(self, q, out, in_, reads=(), writes=(), **kw):
        self._sync(q, reads, writes)
        i = self.dcnt[q]
        self.dcnt[q] += 1
        sem = self.dsem[q][i % NRING]
        val = 16 * (i // NRING + 1)
        self.E[q].dma_start(out=out, in_=in_, **kw).then_inc(sem, 16)
        ev = (sem, val, "dma_" + q)
        self._record(ev, reads, writes)
        self.n_ins += 1
        return ev

    def release(self):
        self.nc.all_engine_barrier()
        self.nc.clear_and_free_semaphores(self.all_sems)
        self.nc.all_engine_barrier()

    def finish(self):
        for q in ("sp", "pool"):
            n = self.dcnt[q]
            for s in range(NRING):
                cnt = (n - s + NRING - 1) // NRING if n > s else 0
                if cnt > 0:
                    self.E[q].wait_ge(self.dsem[q][s], 16 * cnt)


def bcast_rows(ap, nparts):
    return bass.AP(ap.tensor, ap.offset, [[0, nparts]] + [list(x) for x in ap.ap[1:]])


def rmsnorm_tiles(c, hT_dram, g_col, aT, ones_bf, eps_t, ht, sq, ps_stat, rt, rstd, ntt, q="sp"):
    hv = hT_dram.rearrange("(kc p) t -> p kc t", p=128)
    for tt in range(ntt):
        ts = slice(tt * 512, (tt + 1) * 512)
        for k4 in range(4):
            c.dma(q, ht[:, k4 * 4:(k4 + 1) * 4, :], hv[:, k4 * 4:(k4 + 1) * 4, ts],
                  writes=[(ht.name, k4)])
        c.op("act", "activation", sq[:], ht[:], AF.Square,
             reads=[(ht.name, k) for k in range(4)], writes=[sq])
        for kc in range(KC):
            c.op("pe", "matmul", ps_stat[:], ones_bf[:], sq[:, kc, :], start=(kc == 0),
                 stop=(kc == KC - 1), reads=[sq, ones_bf], writes=[ps_stat])
        c.op("act", "activation", rt[:], ps_stat[:], AF.Sqrt, bias=eps_t[:, 0:1], scale=1.0 / D,
             reads=[ps_stat, eps_t], writes=[rt])
        c.op("dve", "reciprocal", rstd[:], rt[:], reads=[rt], writes=[rstd])
        for kc in range(KC):
            c.op("dve", "scalar_tensor_tensor", aT[:, kc, ts], ht[:, kc, :], g_col[:, kc:kc + 1],
                 rstd[:], ALU.mult, ALU.mult,
                 reads=[(ht.name, kc // 4), g_col, rstd], writes=[(aT.name, tt)])


def load_w(c, q, wt, src3, ncol, tag):
    for k4 in range(4):
        c.dma(q, wt[:, k4 * 4:(k4 + 1) * 4, 0:ncol], src3[:, k4 * 4:(k4 + 1) * 4, :],
              writes=[(wt.name, k4)])


HROWS16 = 2304
HROWS32 = 268
N_FM = 8
N_TM = 4


def _default_dr(nc):
    return lambda n, s, d, k="ExternalInput": nc.dram_tensor(n, s, d, kind=k).ap()


def build_A(nc=None, dr=None, pfx="", semstack=None):
    standalone = nc is None
    if standalone:
        nc = bass.Bass("TRN2", target_bir_lowering=False)
        dr = _default_dr(nc)
    dt = dr
    hT = dt("hT", [D, TOK], F32, "ExternalInput")
    g1 = dt("g1", [128, KC], F32, "ExternalInput")
    wall = dt("wall", [D, (N_FM + N_TM) * 512], F32, "ExternalInput")
    wgate = dt("wgate", [D, 24], F32, "ExternalInput")
    glng = dt("glng", [1, 512], F32, "ExternalInput")
    glnb = dt("glnb", [1, 512], F32, "ExternalInput")
    wsT = dt("wsT", [128, 4, 128], F32, "ExternalInput")
    bsf = dt("bsf", [1, 512], F32, "ExternalInput")
    tril = dt("tril", [128, 128], F32, "ExternalInput")
    a16 = dt("a16", [2 * HROWS16, TOK], BF16, "ExternalOutput")
    a32 = dt("a32", [2 * HROWS32, TOK], F32, "ExternalOutput")
    ogm = dt("ogm", [512, TOK], BF16, "ExternalOutput")

    with contextlib.ExitStack() as st:
        c = Ctx(nc, st, pfx, semstack)
        aT = c.sb("aT", [128, KC, TOK], BF16)
        ht = c.sb("ht", [128, KC, 512], F32)
        sq = c.sb("sq", [128, KC, 512], BF16)
        wb = [c.sb("wb%d" % i, [128, KC, 512], BF16) for i in range(2)]
        wg = c.sb("wg", [128, KC, 24], BF16)
        uT = c.sb("uT", [128, 4, TOK], BF16)
        ones_bf = c.sb("ones_bf", [128, 128], BF16)
        eps_t = c.sb("eps_t", [128, 1], F32)
        g_col = c.sb("g_col", [128, KC], F32)
        rt = c.sb("rt", [128, 512], F32)
        rstd = c.sb("rstd", [128, 512], F32)
        lng = c.sb("lng", [128, 512], F32)
        lnb = c.sb("lnb", [128, 512], F32)
        bsb = c.sb("bsb", [128, 512], F32)
        wsf = c.sb("wsf", [128, 4, 128], F32)
        trl = c.sb("trl", [128, 128], F32)
        wsm = c.sb("wsm", [128, 4, 128], BF16)
        stg = [c.sb("stg%d" % i, [128, 4, 512], BF16) for i in range(2)]
        stgf = [c.sb("stgf%d" % i, [128, 4, 512], F32) for i in range(1)]
        stgt = [c.sb("stgt%d" % i, [128, 512], BF16) for i in range(2)]
        gst = c.sb("gst", [24, 512], F32)
        vg = c.sb("vg", [128, 512], F32)
        vn = c.sb("vn", [128, 512], F32)
        vln = c.sb("vln", [128, 512], BF16)
        bst = c.sb("bst", [128, 6], F32)
        mv = c.sb("mv", [128, 2], F32)
        sd = c.sb("sd", [128, 1], F32)
        rs1 = c.sb("rs1", [128, 1], F32)
        tmpg = c.sb("tmpg", [128, 512], F32)
        ogs = [c.sb("ogs%d" % i, [128, 4, 512], BF16) for i in range(1)]
        pst = [c.ps("pst%d" % i, [128, 512]) for i in range(6)]
        ps_stat = c.ps("ps_stat", [128, 512])
        psg = c.ps("psg", [128, 512])

        c.op("pool", "memset", ones_bf[:], 1.0, writes=[ones_bf])
        c.op("pool", "memset", eps_t[:], EPS, writes=[eps_t])
        c.dma("sp", g_col[:], g1, writes=[g_col])
        c.dma("sp", lng[:], bcast_rows(glng, 128), writes=[lng])
        c.dma("sp", lnb[:], bcast_rows(glnb, 128), writes=[lnb])
        c.dma("sp", bsb[:], bcast_rows(bsf, 128), writes=[bsb])
        c.dma("sp", wsf[:], wsT, writes=[wsf])
        c.dma("sp", trl[:], tril, writes=[trl])
        for g in range(4):
            c.op("dve", "tensor_tensor", wsm[:, g, :], wsf[:, g, :], trl[:], ALU.mult,
                 reads=[wsf, trl], writes=[wsm])
        c.dma("pool", wg[:], wgate.rearrange("(kc p) n -> p kc n", p=128), writes=[wg])

        wv = wall.rearrange("(kc p) n -> p kc n", p=128)
        load_w(c, "pool", wb[0], wv[:, :, 0:512], 512, 0)

        rmsnorm_tiles(c, hT, g_col, aT, ones_bf, eps_t, ht, sq, ps_stat, rt, rstd, 4)
        aT_all = [(aT.name, tt) for tt in range(4)]

        pcount = [0]

        def next_ps():
            p = pst[pcount[0] % len(pst)]
            pcount[0] += 1
            return p

        for tt in range(4):
            ts = slice(tt * 512, (tt + 1) * 512)
            p = next_ps()
            for kc in range(KC):
                c.op("pe", "matmul", p[0:24, :], wg[:, kc, :], aT[:, kc, ts], start=(kc == 0),
                     stop=(kc == KC - 1), reads=[wg, (aT.name, tt)], writes=[p])
            c.op("act", "activation", gst[:], p[0:24, :], AF.Sigmoid, reads=[p], writes=[gst])
            for hf in range(2):
                c.dma("sp", a32[hf * HROWS32 + 256:hf * HROWS32 + 268, ts], gst[hf * 12:(hf + 1) * 12, :], reads=[gst])

        ev = 0
        for g in range(N_FM + N_TM):
            w = wb[g % 2]
            if g + 1 < N_FM + N_TM:
                load_w(c, "pool", wb[(g + 1) % 2], wv[:, :, (g + 1) * 512:(g + 2) * 512], 512, g + 1)
            wkeys = [(w.name, k) for k in range(4)]
            if g < N_FM:
                for tt in range(4):
                    ts = slice(tt * 512, (tt + 1) * 512)
                    if g == 6:
                        s_t = stgf[0]
                    elif g == 7:
                        s_t = None
                    else:
                        s_t = stg[(g * 4 + tt) % 2]
                    for blk in range(4):
                        p = next_ps()
                        for kc in range(KC):
                            c.op("pe", "matmul", p[:], w[:, kc, blk * 128:(blk + 1) * 128], aT[:, kc, ts],
                                 start=(kc == 0), stop=(kc == KC - 1),
                                 reads=[(w.name, kc // 4), (aT.name, tt)], writes=[p])
                        if g == 7:
                            c.op("act", "activation", uT[:, blk, ts], p[:], AF.Gelu_apprx_tanh,
                                 reads=[p], writes=[(uT.name, tt, blk)])
                        elif g == 6:
                            c.op("act", "activation", s_t[:, blk, :], p[:], AF.Silu,
                                 reads=[p], writes=[(s_t.name, blk)])
                        else:
                            sc = SCALE if g in (0, 3) else 1.0
                            if ev % 2 == 0:
                                c.op("act", "activation", s_t[:, blk, :], p[:], AF.Copy, scale=sc,
                                     reads=[p], writes=[(s_t.name, blk)])
                            else:
                                c.op("dve", "tensor_scalar", s_t[:, blk, :], p[:], sc, None, ALU.mult,
                                     reads=[p], writes=[(s_t.name, blk)])
                            ev += 1
                    if g == 6:
                        for hf in range(2):
                            dst = a32[hf * HROWS32:hf * HROWS32 + 256, :].rearrange("(b p) t -> p b t", p=128)[:, :, ts]
                            c.dma("sp", dst, s_t[:, hf * 2:(hf + 1) * 2, :], reads=[(s_t.name, b_) for b_ in range(4)])
                    elif g < 6:
                        r0 = (g // 3) * HROWS16 + (g % 3) * 512
                        dst = a16[r0:r0 + 512, :].rearrange("(b p) t -> p b t", p=128)[:, :, ts]
                        c.dma("sp", dst, s_t[:], reads=[(s_t.name, b_) for b_ in range(4)])
            else:
                gi = g - N_FM
                for tb in range(16):
                    tt = tb // 4
                    tks = slice(tb * 128, (tb + 1) * 128)
                    p = next_ps()
                    for kc in range(KC):
                        c.op("pe", "matmul", p[:], aT[:, kc, tks], w[:, kc, :], start=(kc == 0),
                             stop=(kc == KC - 1), reads=[(w.name, kc // 4), (aT.name, tt)], writes=[p])
                    if gi < 3:
                        s_t = stgt[tb % 2]
                        if tb % 2 == 0:
                            c.op("act", "activation", s_t[:], p[:], AF.Copy, reads=[p], writes=[s_t])
                        else:
                            c.op("dve", "tensor_copy", s_t[:], p[:], reads=[p], writes=[s_t])
                        for hf in range(2):
                            off = (hf * HROWS16 + 1536) * TOK + tb * 128 * 768 + gi * 256
                            c.dma("sp", bass.AP(a16.tensor, off, [[768, 128], [1, 256]]),
                                  s_t[:, hf * 256:(hf + 1) * 256], reads=[s_t])
                    else:
                        c.op("act", "activation", vg[:], p[:], AF.Gelu_apprx_tanh, reads=[p], writes=[vg])
                        c.op("dve", "bn_stats", bst[:], vg[:], reads=[vg], writes=[bst])
                        c.op("dve", "bn_aggr", mv[:], bst[:], reads=[bst], writes=[mv])
                        c.op("act", "activation", sd[:], mv[:, 1:2], AF.Sqrt, bias=eps_t[:, 0:1], scale=1.0,
                             reads=[mv, eps_t], writes=[sd])
                        c.op("dve", "reciprocal", rs1[:], sd[:], reads=[sd], writes=[rs1])
                        c.op("dve", "tensor_scalar", vn[:], vg[:], mv[:, 0:1], rs1[:, 0:1], ALU.subtract,
                             ALU.mult, reads=[vg, mv, rs1], writes=[vn])
                        c.op("pool", "tensor_tensor", vn[:], vn[:], lng[:], ALU.mult,
                             reads=[vn, lng], writes=[vn])
                        c.op("pool", "tensor_tensor", vln[:], vn[:], lnb[:], ALU.add,
                             reads=[vn, lnb], writes=[vln])
                        for gg in range(4):
                            c.op("pe", "matmul", psg[:, gg * 128:(gg + 1) * 128], vln[:, gg * 128:(gg + 1) * 128],
                                 wsm[:, gg, :], start=True, stop=True, reads=[vln, wsm], writes=[psg])
                        c.op("dve", "tensor_tensor", tmpg[:], psg[:], bsb[:], ALU.add,
                             reads=[psg, bsb], writes=[tmpg])
                        og = ogs[0]
                        c.op("pool", "tensor_tensor", og[:, :, (tb % 4) * 128:(tb % 4 + 1) * 128],
                             tmpg[:].rearrange("p (g t) -> p g t", g=4), uT[:, :, tks], ALU.mult,
                             reads=[tmpg] + [(uT.name, tt, b_) for b_ in range(4)],
                             writes=[(og.name, tb % 4)])
                        if tb % 4 == 3:
                            dst = ogm.rearrange("(g p) t -> p g t", p=128)[:, :, tt * 512:(tt + 1) * 512]
                            c.dma("sp", dst, og[:], reads=[(og.name, b_) for b_ in range(4)])
        c.finish()
        if not standalone:
            c.release()
        print("A: ins", c.n_ins, "waits", c.n_wait)
    return nc


_OFF = {}
_o = 0
for _n, _w in (("q", 1024), ("kcmp", 256), ("vcmp", 256), ("kslc", 256), ("vslc", 256), ("kwin", 256),
               ("vwin", 256), ("gate", 24), ("u", 512), ("v", 512), ("rq", 512), ("rk", 512),
               ("rv", 512), ("rg", 512)):
    _OFF[_n] = (_o, _o + _w)
    _o += _w


def pack_A_weights(w_in_l):
    cols = lambda n: w_in_l[:, _OFF[n][0]:_OFF[n][1]]
    hcol = lambda n, hf, w: w_in_l[:, _OFF[n][0] + hf * w:_OFF[n][0] + (hf + 1) * w]
    fm = []
    for hf in range(2):
        fm += [hcol("q", hf, 512), hcol("kcmp", hf, 128), hcol("vcmp", hf, 128), hcol("kslc", hf, 128),
               hcol("kwin", hf, 128), hcol("rq", hf, 256), hcol("rk", hf, 256)]
    fm += [cols("rg"), cols("u")]
    tm = [hcol("vslc", 0, 128), hcol("vwin", 0, 128), hcol("vslc", 1, 128), hcol("vwin", 1, 128),
          cols("rk"), cols("rv"), cols("v")]
    wall = np.ascontiguousarray(np.concatenate(fm + tm, axis=1))
    assert wall.shape[1] == (N_FM + N_TM) * 512
    return wall, np.ascontiguousarray(cols("gate"))


NEG = -30000.0
NCMP = 255


def nsa_consts():
    p = np.arange(128)[:, None]
    f = np.arange(512)[None, :]
    nbc = np.stack([np.where(f - p - 128 * r >= 0, 0.0, NEG) for r in range(4)])
    nbw = np.stack([np.where(f - p - 128 * r < 512, 0.0, NEG) for r in (-4, -3, -2, -1)])
    nbm = np.stack([np.where(f - 16 * p + 512 * r - 31 >= 0, 0.0, NEG) for r in range(5)])
    nbm1 = nbm[0:4].copy()
    nbm1[:, 127, :] = NEG
    masks = np.concatenate([nbc, nbw, nbm, nbm1], 0).astype(np.float32)
    masks = np.ascontiguousarray(masks.transpose(1, 0, 2))
    pk = np.arange(128)
    posk = np.stack([pk, np.ones(128), np.ones(128)]).astype(np.float32)
    poskc = np.stack([16 * pk, np.ones(128), np.ones(128)]).astype(np.float32)
    ff = np.arange(512)
    e30 = np.zeros((64, 4096), np.float32)
    for j in range(64):
        e30[j, j * 64:(j + 1) * 64] = 30000.0
    c0 = np.arange(256)[:, None] * 16
    s0 = np.arange(64)[None, :] * 64
    ov = np.minimum(c0 + 32, s0 + 64) - np.maximum(c0, s0)
    share = (np.clip(ov, 0, 32) / 32.0).astype(np.float32)
    share[255] = 0
    share = np.ascontiguousarray(share.reshape(2, 128, 64).transpose(1, 0, 2))
    t = np.arange(4096)
    cur = (t // 64)[:, None]
    j = np.arange(64)[None, :]
    forced = (j == 0) | (j == cur) | (j == cur - 1)
    btab = np.where(forced, 1e4, 0.0)
    btab = np.where(j > cur, -1e30, btab).astype(np.float32)
    btab = np.ascontiguousarray(btab.reshape(32, 128, 64).transpose(1, 0, 2))
    selg = np.zeros((128, 12, 128), np.float32)
    for n in range(12):
        selg[n, n, :] = 1.0
        selg[32 + n, n, :] = 1.0
    selg = selg.reshape(128, 12 * 128)
    ek = np.zeros((128, 4096), np.float32)
    ek[0:64] = e30
    ek[64:67] = np.tile(posk, (1, 32))
    ekc = np.zeros((128, 128), np.float32)
    ekc[64:67] = poskc
    ekw = np.zeros((128, 128), np.float32)
    ekw[64:67] = posk
    return dict(masks=masks, ek=ek, ekc=ekc, ekw=ekw, share=share, btab=btab, selg=selg,
                ident=np.eye(128, dtype=np.float32))


def nsa_posq(hf):
    ff = np.arange(512)
    out = np.zeros((4, 3, 512), np.float32)
    for r in range(4):
        h = 4 * hf + r
        slope = 2.0 ** (-8.0 * (h + 1) / 8)
        out[r, 0] = slope
        out[r, 1] = -slope * 64 * (ff // 64)
        out[r, 2] = -slope * (ff % 64)
    return out


def nsa_bias_table(hf):
    tb = np.zeros((192,), np.float32)
    for r in range(4):
        slope = 2.0 ** (-(4 * hf + r + 1))
        for cb in range(2):
            for tc in range(8):
                tb[r * 16 + cb * 8 + tc] = slope * (2048 * cb + 31 - 512 * tc)
        for rel in range(-28, 4):
            tb[64 + r * 32 + rel + 28] = slope * 128 * rel
    return np.ascontiguousarray(np.broadcast_to(tb[None, :], (128, 192)))


def build_B1(nc=None, dr=None, pfx="", semstack=None, pre_hook=None):
    standalone = nc is None
    if standalone:
        nc = bass.Bass("TRN2", target_bir_lowering=False)
        dr = _default_dr(nc)
    dt = dr
    b16 = dt("b16", [2 * HROWS16, TOK], BF16)
    b32 = dt("b32", [2 * HROWS32, TOK], F32)
    w1_d = dt("d_w1", [2, 128, 32, 128], F32)
    pe_d = dt("d_peT", [2, 128, 32], F32)
    w2_d = dt("d_w2", [2, 128, 128], F32)
    masks_d = dt("d_masks", [128, 17, 512], F32)
    ekc_d = dt("d_ekc", [128, 128], F32)
    ekw_d = dt("d_ekw", [128, 128], F32)
    posq_d = dt("posq", [4, 3, 512], F32)
    ek_d = dt("d_ek", [128, 4096], F32)
    share_d = dt("d_share", [128, 2, 64], F32)
    btab_d = dt("d_btab", [128, 32, 64], F32)
    selg_d = dt("d_selg", [128, 12 * 128], F32)
    ident_d = dt("d_ident", [128, 128], F32)
    btl_d = dt("d_btl", [128, 192], F32)
    out_d = dt("bout", [768, T], BF16, "ExternalOutput")

    with contextlib.ExitStack() as st:
        c = Ctx(nc, st, pfx, semstack)
        qT = c.sb("qT", [128, 4, T], BF16)
        kT = c.sb("kT", [128, 4, T], BF16)
        vs = c.sb("vs", [128, 32, 128], BF16)
        vw = c.sb("vw", [128, 32, 128], BF16)
        masks = c.sb("masks", [128, 17, 512], BF16)
        ekc = c.sb("ekc", [128, 128], BF16)
        ekw = c.sb("ekw", [128, 128], BF16)
        ek = c.sb("ek", [128, 4096], BF16)
        nq = [c.sb("nq%d" % r, [128, 512], BF16) for r in range(4)]
        gst32 = [c.sb("gst32_%d" % i, [128, 512], F32) for i in range(2)]
        gh = [c.sb("gh%d" % i, [128, 512], BF16) for i in range(2)]
        hb = c.sb("hb", [128, 512], BF16)
        share = c.sb("share", [128, 2, 64], BF16)
        btab = c.sb("btab", [128, 32, 64], F32)
        selg = c.sb("selg", [128, 12 * 128], BF16)
        ident = c.sb("ident", [128, 128], BF16)
        identf = c.sb("identf", [128, 128], F32)
        ones = c.sb("ones", [128, 128], BF16)
        w1 = [c.sb("w1_%d" % i, [128, 32, 128], BF16) for i in range(2)]
        peT = c.sb("peT", [128, 2, 32], BF16)
        w2 = c.sb("w2", [128, 2, 128], BF16)
        b1 = c.sb("b1", [128, 2], F32)
        g1 = c.sb("g1", [128, 2, 256], BF16)
        kccT = c.sb("kccT", [128, 256], BF16)
        vcc = c.sb("vcc", [128, 2, 128], BF16)
        Et = [c.sb("Et%d" % i, [128, 512], BF16) for i in range(3)]
        Pn = c.sb("Pn", [128, 4, 2, 512], BF16)
        rd = c.sb("rd", [128, 512], F32)
        lnd = c.sb("lnd", [128, 512], F32)
        tiny = c.sb("tiny", [128, 1], F32)
        wgt = c.sb("wgt", [128, 512], F32)
        tmp = c.sb("tmp", [128, 512], F32)
        acc = c.sb("acc", [128, 512], F32)
        caccs = [c.sb("cacc%d" % i, [128, 4, 512], F32) for i in range(2)]
        ost = [c.sb("ost%d" % i, [128, 512], BF16) for i in range(2)]
        scr = c.sb("scr", [128, 4, 64], F32)
        scr2 = c.sb("scr2", [128, 4, 64], F32)
        m8 = c.sb("m8", [128, 4, 16], F32)
        thr = c.sb("thr", [128, 4], F32)
        psS = [c.ps("psS%d" % i, [128, 512]) for i in range(2)]
        psOs = [c.ps("psO%d" % i, [128, 512]) for i in range(2)]
        psDs = [c.ps("psD%d" % i, [128, 512]) for i in range(2)]
        psG = c.ps("psG", [128, 512])
        psI = c.ps("psIX", [128, 512])
        psX = psI
        sm1 = c.sb("sm1b", [128, 4, 64], F32)

        btl = c.sb("btl", [128, 192], F32)
        c.dma("sp", btl[:], btl_d, writes=[btl])
        c.op("pool", "memset", ones[:], 1.0, writes=[ones])
        c.op("pool", "memset", tiny[:], 1e-18, writes=[tiny])
        for th in range(2):
            tsl = slice(th * TOK, (th + 1) * TOK)
            for r in range(4):
                c.dma("sp", qT[:, r, tsl], b16[th * HROWS16 + r * 128:th * HROWS16 + (r + 1) * 128, :],
                      writes=[(qT.name, r)])
                c.dma("sp", kT[:, r, tsl], b16[th * HROWS16 + 512 + r * 128:th * HROWS16 + 512 + (r + 1) * 128, :],
                      writes=[(kT.name, r)])
            toff = (th * HROWS16 + 1536) * TOK
            c.dma("sp", vs[:, th * 16:(th + 1) * 16, :],
                  bass.AP(b16.tensor, toff, [[768, 128], [768 * 128, 16], [1, 128]]), writes=[vs])
            c.dma("sp", vw[:, th * 16:(th + 1) * 16, :],
                  bass.AP(b16.tensor, toff + 128, [[768, 128], [768 * 128, 16], [1, 128]]), writes=[vw])
        c.dma("pool", masks[:], masks_d, writes=[masks])
        c.dma("pool", ekc[:], ekc_d, writes=[ekc])
        c.dma("pool", ekw[:], ekw_d, writes=[ekw])
        for r in range(4):
            c.op("dve", "memset", nq[r][:], 0.0, writes=[nq[r]])
            c.dma("pool", nq[r][64:67, :], posq_d[r], writes=[nq[r]])
        for i in range(2):
            c.op("dve", "memset", gst32[i][:], 0.0, writes=[gst32[i]])
            c.op("dve", "memset", gh[i][:], 0.0, writes=[gh[i]])
        c.op("dve", "memset", g1[:], 0.0, writes=[(g1.name, 0), (g1.name, 1)])
        c.op("dve", "memset", kccT[:], 0.0, writes=[kccT])
        c.op("dve", "memset", vcc[:], 0.0, writes=[(vcc.name, 0), (vcc.name, 1)])
        c.dma("pool", ek[:], ek_d, writes=[ek])
        c.dma("pool", share[:], share_d, writes=[share])
        c.dma("sp", btab[:], btab_d, writes=[btab])
        c.dma("pool", selg[:], selg_d, writes=[selg])
        c.dma("pool", ident[:], ident_d, writes=[ident])
        c.dma("sp", identf[:], ident_d, writes=[identf])
        for i in range(2):
            c.dma("pool", w1[i][:], w1_d[i], writes=[w1[i]])
            c.dma("pool", peT[:, i, :], pe_d[i], writes=[(peT.name, i)])
            c.dma("pool", w2[:, i, :], w2_d[i], writes=[(w2.name, i)])

        bg = list(pre_hook()) if pre_hook is not None else []
        for i in range(2):
            for j in range(32):
                c.op("pe", "matmul", psX[:, 0:1], w1[i][:, j, :], peT[:, i, j:j + 1], start=(j == 0),
                     stop=(j == 31), reads=[w1[i], (peT.name, i)], writes=[psX])
            c.op("dve", "tensor_copy", b1[:, i:i + 1], psX[:, 0:1], reads=[psX], writes=[(b1.name, i)])
            for j in range(32):
                c.op("pe", "matmul", psI[:, 0:NCMP], w1[i][:, j, :], kT[:, i, j:j + 16 * (NCMP - 1) + 1:16],
                     start=(j == 0), stop=(j == 31), reads=[w1[i], (kT.name, i)], writes=[psI])
            c.op("act", "activation", g1[:, i, 0:NCMP], psI[:, 0:NCMP], AF.Gelu_apprx_tanh,
                 bias=b1[:, i:i + 1], scale=1.0, reads=[psI, (b1.name, i)], writes=[(g1.name, i)])
        c.op("pe", "matmul", psI[:, 0:NCMP], w2[:, 0, :], g1[:, 0, 0:NCMP], start=True, stop=True,
             reads=[(w2.name, 0), (g1.name, 0)], writes=[psI])
        c.op("dve", "tensor_copy", kccT[:, 0:NCMP], psI[:, 0:NCMP], reads=[psI], writes=[kccT])
        for cb in range(2):
            M = 128
            c.op("pe", "matmul", psX[0:M, 0:128], g1[:, 1, cb * 128:cb * 128 + M], w2[:, 1, :], start=True,
                 stop=True, reads=[(w2.name, 1), (g1.name, 1)], writes=[psX])
            c.op("dve", "tensor_copy", vcc[0:M, cb, :], psX[0:M, 0:128], reads=[psX], writes=[(vcc.name, cb)])

        sctr = [0]
        octr = [0]

        def next_S():
            p = psS[sctr[0] % 2]
            e = Et[sctr[0] % 3]
            sctr[0] += 1
            return p, e

        def run_blocks(blocks):
            psO = psOs[octr[0] % 2]
            psD = psDs[octr[0] % 2]
            octr[0] += 1
            n = len(blocks)
            pend = None

            def pv(b, e, i):
                M = b["M"]
                c.op("pe", "matmul", psO[:], b["v"], e[0:M, :], start=(i == 0), stop=(i == n - 1),
                     reads=[b["vkey"], e], writes=[psO])
                c.op("pe", "matmul", psD[:], ones[0:M, :], e[0:M, :], start=(i == 0), stop=(i == n - 1),
                     reads=[ones, e], writes=[psD])
                if b.get("keep") is not None:
                    dst, key = b["keep"]
                    c.op("pool", "tensor_copy", dst, e[0:M, :], reads=[e], writes=[key])

            for i, b in enumerate(blocks):
                p, e = next_S()
                M = b["M"]
                ns = len(b["s_ops"])
                for j, (lh, rh, rd) in enumerate(b["s_ops"]):
                    c.op("pe", "matmul", p[0:M, :], lh, rh, start=(j == 0), stop=(j == ns - 1), reads=rd, writes=[p])
                c.op("act", "activation", e[0:M, :], p[0:M, :], AF.Exp, bias=b["bias"], scale=1.0,
                     reads=[p, btl], writes=[e])
                if pend is not None:
                    pv(*pend)
                pend = (b, e, i)
            pv(*pend)
            return psO, psD

        def branch_finish(tc, r, br, first, psO, psD):
            g = gh[tc % 2]
            c.op("act", "activation", lnd[:], psD[:], AF.Ln, bias=tiny[:, 0:1], scale=1.0, reads=[psD, tiny], writes=[lnd])
            c.op("act", "activation", rd[:], lnd[:], AF.Exp, scale=-1.0, reads=[lnd], writes=[rd])
            n = r * 3 + br
            c.op("pe", "matmul", psG[:], selg[:, n * 128:(n + 1) * 128], g[:], start=True, stop=True,
                 reads=[selg, g], writes=[psG])
            c.op("dve", "tensor_tensor", wgt[:], psG[:], rd[:], ALU.mult, reads=[psG, rd], writes=[wgt])
            if first:
                c.op("dve", "tensor_tensor", acc[:], psO[:], wgt[:], ALU.mult, reads=[psO, wgt], writes=[acc])
            else:
                c.op("dve", "tensor_tensor", tmp[:], psO[:], wgt[:], ALU.mult, reads=[psO, wgt], writes=[tmp])
                c.op("pool", "tensor_tensor", acc[:], acc[:], tmp[:], ALU.add, reads=[acc, tmp], writes=[acc])

        def front(tc):
            ts = slice(tc * 512, (tc + 1) * 512)
            g = gh[tc % 2]
            g32 = gst32[tc % 2]
            cacc = caccs[tc % 2]

            def load_gates(tcn):
                gr0 = (tcn // 4) * HROWS32 + 256
                gsrc = b32[gr0:gr0 + 12, (tcn % 4) * 512:(tcn % 4 + 1) * 512]
                c.dma("sp", gst32[tcn % 2][0:12, :], gsrc, writes=[gst32[tcn % 2]])
                c.dma("sp", gst32[tcn % 2][32:44, :], gsrc, writes=[gst32[tcn % 2]])

            if tc == 0:
                load_gates(0)
            c.op("act", "activation", g[0:12, :], g32[0:12, :], AF.Copy, reads=[g32], writes=[g])
            c.op("act", "activation", hb[32:44, :], g32[32:44, :], AF.Copy, reads=[g32], writes=[hb])
            c.op("dve", "tensor_tensor", g[32:44, :], g32[32:44, :], hb[32:44, :], ALU.subtract,
                 reads=[g32, hb], writes=[g])
            if tc + 1 < 8:
                load_gates(tc + 1)
            cbs = [0] if tc < 4 else [0, 1]
            for r in range(4):
                blocks = []
                for cb in cbs:
                    M = 128
                    rel = tc - 4 * cb
                    ops = [(kccT[:, cb * 128:cb * 128 + M], qT[:, r, ts], [kccT, (qT.name, r)]),
                           (ekc[:], nq[r][:], [ekc, nq[r]])]
                    if cb == 1:
                        ops.append((ident[:], masks[:, 13 + rel, :], [ident, masks]))
                    elif rel <= 4:
                        ops.append((ident[:], masks[:, 8 + rel, :], [ident, masks]))
                    bi = r * 16 + cb * 8 + tc
                    blocks.append(dict(M=M, s_ops=ops, bias=btl[0:M, bi:bi + 1], v=vcc[0:M, cb, :],
                                       vkey=(vcc.name, cb), keep=(Pn[0:M, r, cb, :], (Pn.name, r, cb))))
                psO, psD = run_blocks(blocks)
                branch_finish(tc, r, 0, True, psO, psD)
                for cb in cbs:
                    M = 128
                    c.op("dve", "tensor_tensor", Pn[0:M, r, cb, :], Pn[0:M, r, cb, :], rd[0:M, :], ALU.mult,
                         reads=[(Pn.name, r, cb), rd], writes=[(Pn.name, r, cb)])
                c.op("dve", "tensor_copy", cacc[:, r, :], acc[:], reads=[acc], writes=[(cacc.name, r)])
            for tb in range(4):
                gtb = tc * 4 + tb
                n = 0
                tot = 4 * len(cbs)
                for r in range(4):
                    for cb in cbs:
                        M = 128
                        c.op("pe", "matmul", psI[:, tb * 64:(tb + 1) * 64], Pn[0:M, r, cb, tb * 128:(tb + 1) * 128],
                             share[0:M, cb, :], start=(n == 0), stop=(n == tot - 1),
                             reads=[(Pn.name, r, cb), share], writes=[psI])
                        n += 1
            K4 = range(4)
            for tb in K4:
                c.op("dve", "tensor_tensor", scr[:, tb, :], psI[:, tb * 64:(tb + 1) * 64], btab[:, tc * 4 + tb, :],
                     ALU.add, reads=[psI, btab], writes=[(scr.name, tb)])
            for tb in K4:
                c.op("dve", "max", m8[:, tb, 0:8], scr[:, tb, :], reads=[(scr.name, tb)], writes=[(m8.name, tb)])
            for tb in K4:
                c.op("dve", "match_replace", scr2[:, tb, :], m8[:, tb, 0:8], scr[:, tb, :], -1e30,
                     reads=[(scr.name, tb), (m8.name, tb)], writes=[(scr2.name, tb)])
            for tb in K4:
                c.op("dve", "max", m8[:, tb, 8:16], scr2[:, tb, :], reads=[(scr2.name, tb)], writes=[(m8.name, tb)])
            for tb in K4:
                c.op("dve", "tensor_scalar", thr[:, tb:tb + 1], m8[:, tb, 15:16], -1e29, None, ALU.max,
                     reads=[(m8.name, tb)], writes=[(thr.name, tb)])
            for tb in K4:
                c.op("dve", "tensor_scalar", sm1[:, tb, :], scr[:, tb, :], thr[:, tb:tb + 1], 1.0, ALU.is_ge,
                     ALU.subtract, reads=[(scr.name, tb), (thr.name, tb)], writes=[(sm1.name, tb)])

        def mid(tc):
            ts = slice(tc * 512, (tc + 1) * 512)
            cbs = [0] if tc < 4 else [0, 1]
            cacc = caccs[tc % 2]
            for tb in range(4):
                c.op("pe", "transpose", psX[0:64, tb * 128:(tb + 1) * 128], sm1[:, tb, :], identf[:],
                     reads=[(sm1.name, tb), identf], writes=[psX])
            c.op("act", "activation", nq[0][0:64, :], psX[0:64, :], AF.Copy, reads=[psX], writes=[nq[0]])
            for r in range(1, 4):
                c.op("pool", "tensor_copy", nq[r][0:64, :], nq[0][0:64, :], reads=[nq[0]], writes=[nq[r]])

        def sw(tc):
            ts = slice(tc * 512, (tc + 1) * 512)
            cbs = [0] if tc < 4 else [0, 1]
            cacc = caccs[tc % 2]
            for r in range(4):
                for _ in range(2):
                    if bg:
                        d_, s_ = bg.pop(0)
                        c.dma("pool", d_, s_)
                c.op("dve", "tensor_copy", acc[:], cacc[:, r, :], reads=[(cacc.name, r)], writes=[acc])
                blocks = []
                for kb in range(4 * tc + 4):
                    rel = kb - 4 * tc
                    ks = slice(kb * 128, (kb + 1) * 128)
                    ops = [(kT[:, 2, ks], qT[:, r, ts], [(kT.name, 2), (qT.name, r)]),
                           (ek[:, ks], nq[r][:], [ek, nq[r]])]
                    if rel >= 0:
                        ops.append((ident[:], masks[:, rel, :], [ident, masks]))
                    bi = 64 + r * 32 + rel + 28
                    blocks.append(dict(M=128, s_ops=ops, bias=btl[:, bi:bi + 1], v=vs[:, kb, :], vkey=vs))
                psO, psD = run_blocks(blocks)
                branch_finish(tc, r, 1, False, psO, psD)
                blocks = []
                for kb in range(max(0, 4 * tc - 4), 4 * tc + 4):
                    rel = kb - 4 * tc
                    ks = slice(kb * 128, (kb + 1) * 128)
                    mi = rel if rel >= 0 else 4 + (rel + 4)
                    ops = [(kT[:, 3, ks], qT[:, r, ts], [(kT.name, 3), (qT.name, r)]),
                           (ekw[:], nq[r][:], [ekw, nq[r]]),
                           (ident[:], masks[:, mi, :], [ident, masks])]
                    bi = 64 + r * 32 + rel + 28
                    blocks.append(dict(M=128, s_ops=ops, bias=btl[:, bi:bi + 1], v=vw[:, kb, :], vkey=vw))
                psO, psD = run_blocks(blocks)
                branch_finish(tc, r, 2, False, psO, psD)
                o = ost[r % 2]
                c.op("act", "activation", o[:], acc[:], AF.Copy, reads=[acc], writes=[o])
                c.dma("sp", out_d[r * 128:(r + 1) * 128, ts], o[:], reads=[o])

        front(0)
        mid(0)
        for tc in range(8):
            if tc + 1 < 8:
                front(tc + 1)
            sw(tc)
            if tc + 1 < 8:
                mid(tc + 1)
        while bg:
            d_, s_ = bg.pop(0)
            c.dma("pool", d_, s_)
        c.finish()
        if not standalone:
            c.release()
        print("B1: ins", c.n_ins, "waits", c.n_wait)
    return nc


def ret_consts(hf):
    n = np.arange(128, dtype=np.float64)
    dec = np.zeros((2, 128, 128), np.float32)
    zeta = np.zeros((128, 2), np.float32)
    xi = np.zeros((2, 1, 128), np.float32)
    dc = []
    for i in range(2):
        h = 2 * hf + i
        lg = np.log1p(-2.0 ** (-5.0 - h))
        rel = n[None, :] - n[:, None]
        dec[i] = np.where(rel >= 0, np.exp(lg * np.maximum(rel, 0)), 0.0) * SCALE
        zeta[:, i] = np.exp(lg * (127.0 - n)) * SCALE
        xi[i, 0] = np.exp(lg * (n + 1.0))
        dc.append(float(np.exp(lg * 128.0)))
    dct = np.ascontiguousarray(np.broadcast_to(np.asarray(dc, np.float32)[None, :], (128, 2)))
    return dict(d_dec=np.ascontiguousarray(dec.transpose(1, 0, 2)), d_zeta=zeta, d_xi=xi, d_dc=dct)


def build_B2(nc=None, dr=None, pfx="", semstack=None):
    standalone = nc is None
    if standalone:
        nc = bass.Bass("TRN2", target_bir_lowering=False)
        dr = _default_dr(nc)
    dt = dr
    b16 = dt("b16", [2 * HROWS16, TOK], BF16)
    b32 = dt("b32", [2 * HROWS32, TOK], F32)
    gn_d = dt("d_gn", [128, 2], F32)
    dec_d = dt("d_dec", [128, 2, 128], F32)
    zeta_d = dt("d_zeta", [128, 2], F32)
    xi_d = dt("d_xi", [2, 1, 128], F32)
    dc_d = dt("d_dc", [128, 2], F32)
    out_d = dt("bout", [768, T], BF16, "ExternalOutput")
    with contextlib.ExitStack() as st:
        c = Ctx(nc, st, pfx, semstack)
        rq = c.sb("rq", [128, 2, T], BF16)
        rk = c.sb("rk", [128, 2, T], BF16)
        qx = c.sb("qx", [128, 2, T], BF16)
        rktm = c.sb("rktm", [128, 32, 256], BF16)
        rvtm = c.sb("rvtm", [128, 32, 256], BF16)
        kz = c.sb("kz", [128, 32, 256], BF16)
        gn = c.sb("gn", [128, 2], F32)
        dec = c.sb("dec", [128, 2, 128], F32)
        zeta = c.sb("zeta", [128, 2], F32)
        xib = c.sb("xib", [128, 2, 128], F32)
        onesf = c.sb("onesf", [128, 128], F32)
        eps_t = c.sb("eps_t", [128, 1], F32)
        state = [c.sb("state%d" % i, [128, 128], F32) for i in range(2)]
        prevb = [c.sb("prevb%d" % i, [128, 128], BF16) for i in range(2)]
        scb = [c.sb("scb%d" % i, [128, 128], BF16) for i in range(2)]
        ysb = c.sb("ysb", [128, 512], F32)
        ysq = c.sb("ysq", [128, 512], F32)
        m2 = c.sb("m2", [128, 512], F32)
        var = c.sb("var", [128, 512], F32)
        sd = c.sb("sd", [128, 512], F32)
        yc = c.sb("yc", [128, 512], F32)
        rgt = [c.sb("rgt%d" % i, [128, 512], F32) for i in range(2)]
        ost = [c.sb("ost%d" % i, [128, 512], BF16) for i in range(2)]
        psSc = [c.ps("psSc%d" % i, [128, 512]) for i in range(2)]
        psY = [c.ps("psY%d" % i, [128, 512]) for i in range(2)]
        psKVs = [c.ps("psKV%d" % i, [128, 512]) for i in range(2)]
        psM = c.ps("psM", [128, 512])
        psE = c.ps("psE", [128, 512])

        c.op("pool", "memset", onesf[:], 1.0 / 128.0, writes=[onesf])
        c.op("pool", "memset", eps_t[:], EPS, writes=[eps_t])
        for i in range(2):
            for th in range(2):
                tsl = slice(th * TOK, (th + 1) * TOK)
                c.dma("sp", rq[:, i, tsl], b16[th * HROWS16 + 1024 + i * 128:th * HROWS16 + 1024 + (i + 1) * 128, :],
                      writes=[(rq.name, i)])
                c.dma("sp", rk[:, i, tsl], b16[th * HROWS16 + 1280 + i * 128:th * HROWS16 + 1280 + (i + 1) * 128, :],
                      writes=[(rk.name, i)])
            c.dma("sp", xib[:, i, :], bcast_rows(xi_d[i], 128), writes=[(xib.name, i)])
            c.op("pool", "memset", state[i][:], 0.0, writes=[state[i]])
            c.op("pool", "memset", prevb[i][:], 0.0, writes=[prevb[i]])
        for th in range(2):
            toff = (th * HROWS16 + 1536) * TOK
            c.dma("sp", rktm[:, th * 16:(th + 1) * 16, :],
                  bass.AP(b16.tensor, toff + 256, [[768, 128], [768 * 128, 16], [1, 256]]), writes=[rktm])
            c.dma("sp", rvtm[:, th * 16:(th + 1) * 16, :],
                  bass.AP(b16.tensor, toff + 512, [[768, 128], [768 * 128, 16], [1, 256]]), writes=[rvtm])
        c.dma("sp", gn[:], gn_d, writes=[gn])
        c.dma("sp", dec[:], dec_d, writes=[dec])
        c.dma("sp", zeta[:], zeta_d, writes=[zeta])
        dct = c.sb("dct", [128, 2], F32)
        c.dma("sp", dct[:], dc_d, writes=[dct])
        for i in range(2):
            c.op("dve", "tensor_scalar", kz[:, :, i * 128:(i + 1) * 128], rktm[:, :, i * 128:(i + 1) * 128],
                 zeta[:, i:i + 1], None, ALU.mult, reads=[rktm, zeta], writes=[(kz.name, i)])
            xb = xib[:, i, :]
            xbb = bass.AP(xb.tensor, xb.offset, [list(xb.ap[0]), [0, 32], list(xb.ap[-1])])
            c.op("pool", "tensor_tensor", qx[:, i, :].rearrange("p (c n) -> p c n", n=128),
                 rq[:, i, :].rearrange("p (c n) -> p c n", n=128), xbb, ALU.mult,
                 reads=[(rq.name, i), (xib.name, i)], writes=[(qx.name, i)])
        k = 0
        for grp in range(8):
            for cc in range(4):
                for i in range(2):
                    pY = psY[i]
                    ch = grp * 4 + cc
                    cs = slice(ch * 128, (ch + 1) * 128)
                    hs = slice(i * 128, (i + 1) * 128)
                    pS = psSc[k % 2]
                    sb_ = scb[k % 2]
                    k += 1
                    c.op("pe", "matmul", pS[:, 0:128], rk[:, i, cs], rq[:, i, cs], start=True, stop=True,
                         reads=[(rk.name, i), (rq.name, i)], writes=[pS])
                    c.op("dve", "tensor_tensor", sb_[:], pS[:, 0:128], dec[:, i, :], ALU.mult,
                         reads=[pS, dec], writes=[sb_])
                    c.op("pe", "matmul", pY[:, cc * 128:(cc + 1) * 128], rvtm[:, ch, hs], sb_[:], start=True,
                         stop=False, reads=[rvtm, sb_], writes=[pY])
                    c.op("pe", "matmul", pY[:, cc * 128:(cc + 1) * 128], prevb[i][:], qx[:, i, cs], start=False,
                         stop=True, reads=[prevb[i], (qx.name, i)], writes=[pY])
                    psKV = psKVs[i]
                    c.op("pe", "matmul", psKV[:, 0:128], kz[:, ch, hs], rvtm[:, ch, hs],
                         start=True, stop=True, reads=[(kz.name, i), rvtm], writes=[psKV])
                    c.op("dve", "scalar_tensor_tensor", state[i][:], state[i][:], dct[:, i:i + 1],
                         psKV[:, 0:128], ALU.mult, ALU.add,
                         reads=[state[i], psKV, dct], writes=[state[i]])
                    c.op("act", "activation", prevb[i][:], state[i][:], AF.Copy, reads=[state[i]],
                         writes=[prevb[i]])
            for i in range(2):
                pY = psY[i]
                ts = slice(grp * 512, (grp + 1) * 512)
                rg_t = rgt[i]
                rr0 = (grp // 4) * HROWS32 + i * 128
                c.dma("sp", rg_t[:], b32[rr0:rr0 + 128, (grp % 4) * 512:(grp % 4 + 1) * 512], writes=[rg_t])
                c.op("act", "activation", ysb[:], pY[:], AF.Copy, reads=[pY], writes=[ysb])
                c.op("act", "activation", ysq[:], pY[:], AF.Square, reads=[pY], writes=[ysq])
                c.op("pe", "matmul", psM[:], onesf[:], ysb[:], start=True, stop=True, reads=[onesf, ysb],
                     writes=[psM])
                c.op("pe", "matmul", psE[:], onesf[:], ysq[:], start=True, stop=True, reads=[onesf, ysq],
                     writes=[psE])
                c.op("act", "activation", m2[:], psM[:], AF.Square, reads=[psM], writes=[m2])
                c.op("dve", "tensor_tensor", var[:], psE[:], m2[:], ALU.subtract, reads=[psE, m2], writes=[var])
                c.op("act", "activation", sd[:], var[:], AF.Sqrt, bias=eps_t[:, 0:1], scale=1.0,
                     reads=[var, eps_t], writes=[sd])
                c.op("dve", "reciprocal", sd[:], sd[:], reads=[sd], writes=[sd])
                c.op("dve", "tensor_tensor", yc[:], ysb[:], psM[:], ALU.subtract, reads=[ysb, psM], writes=[yc])
                c.op("pool", "tensor_tensor", yc[:], yc[:], sd[:], ALU.mult, reads=[yc, sd], writes=[yc])
                o = ost[i]
                c.op("dve", "scalar_tensor_tensor", o[:], yc[:], gn[:, i:i + 1], rg_t[:], ALU.mult, ALU.mult,
                     reads=[yc, gn, rg_t], writes=[o])
                c.dma("sp", out_d[512 + i * 128:512 + (i + 1) * 128, ts], o[:], reads=[o])
        c.finish()
        if not standalone:
            c.release()
        print("B2: ins", c.n_ins, "waits", c.n_wait)
    return nc


FFE = 2816
NFB = 22


def build_C(n_exp, moe, final, nc=None, dr=None, pfx="", semstack=None, wbf=None):
    standalone = nc is None
    if standalone:
        nc = bass.Bass("TRN2", target_bir_lowering=False)
        dr = _default_dr(nc)
    dt = dr
    hT_d = dt("d_hT", [D, TOK], F32)
    cin = dt("cin", [2 * 768, TOK], BF16)
    ogm_d = dt("ogm", [512, TOK], BF16)
    wo_d = dt("d_wo", [D, D], F32)
    g2_d = dt("d_g2", [128, KC], F32)
    w1_d = dt("d_w1", [n_exp, D, FFE], F32)
    w3_d = dt("d_w3", [n_exp, D, FFE], F32)
    w2_d = dt("d_w2", [n_exp, FFE, D], F32)
    if moe:
        wr_d = dt("d_wr", [128, KC, 8], F32)
        br_d = dt("d_br", [8, 1], F32)
        identf_d = dt("d_identf", [128, 128], F32)
        sel8_d = dt("d_sel8", [8, 8 * 128], F32)
    if final:
        gf_d = dt("d_gf", [128, KC], F32)
    out_d = dt("oh", [D, TOK], F32, "ExternalOutput")

    with contextlib.ExitStack() as st:
        c = Ctx(nc, st, pfx, semstack)
        h1t = c.sb("h1t", [128, KC, 512], F32)
        mx = c.sb("mx", [128, KC, 512], BF16)
        fT = c.sb("fT", [128, KC, 512], BF16)
        gT = c.sb("gT", [128, NFB, 512], BF16)
        NWB = 2
        wA = [c.sb("wA%d" % i, [128, KC, 512], BF16) for i in range(NWB)]
        wB = [c.sb("wB%d" % i, [128, NFB, 256], BF16) for i in range(NWB)]
        ones_bf = c.sb("ones_bf", [128, 128], BF16)
        eps_t = c.sb("eps_t", [128, 1], F32)
        g_col = c.sb("g_col", [128, KC], F32)
        rt = c.sb("rt", [128, 512], F32)
        rstd = c.sb("rstd", [128, 512], F32)
        st1 = [c.sb("st1_%d" % i, [128, 512], F32) for i in range(2)]
        st2 = [c.sb("st2_%d" % i, [128, 512], F32) for i in range(2)]
        ps1 = [c.ps("ps1_%d" % i, [128, 512]) for i in range(2)]
        ps3 = [c.ps("ps3_%d" % i, [128, 512]) for i in range(2)]
        pso = [c.ps("pso_%d" % i, [128, 512]) for i in range(2)]
        ps_stat = c.ps("ps_stat", [128, 512])
        psr = c.ps("psr", [128, 512])
        if moe:
            wr = c.sb("wr", [128, KC, 8], F32)
            wrg = c.sb("wrg", [128, KC, 8], F32)
            br = c.sb("br", [8, 1], F32)
            identf = c.sb("identf", [128, 128], F32)
            sel8 = c.sb("sel8", [8, 8 * 128], F32)
            lgT = c.sb("lgT", [8, 512], F32)
            lg = c.sb("lg", [128, 4, 8], F32)
            m8 = c.sb("m8", [128, 4, 8], F32)
            dlt = c.sb("dlt", [128, 4], F32)
            gg1 = c.sb("gg1", [128, 4], F32)
            gg2 = c.sb("gg2", [128, 4], F32)
            cm1 = c.sb("cm1", [128, 4, 8], F32)
            cm2 = c.sb("cm2", [128, 4, 8], F32)
            combT = c.sb("combT", [8, 512], F32)
            cbc = c.sb("cbc", [128, 8, 512], F32)
        if final:
            gf_col = c.sb("gf_col", [128, KC], F32)

        c.op("pool", "memset", ones_bf[:], 1.0, writes=[ones_bf])
        c.op("pool", "memset", eps_t[:], EPS, writes=[eps_t])
        c.dma("sp", g_col[:], g2_d, writes=[g_col])
        if final:
            c.dma("sp", gf_col[:], gf_d, writes=[gf_col])
        if moe:
            c.dma("sp", wr[:], wr_d, writes=[wr])
            c.dma("sp", br[:], br_d, writes=[br])
            c.dma("sp", identf[:], identf_d, writes=[identf])
            c.dma("sp", sel8[:], sel8_d, writes=[sel8])
            for kc in range(KC):
                c.op("dve", "tensor_scalar", wrg[:, kc, :], wr[:, kc, :], g_col[:, kc:kc + 1], None, ALU.mult,
                     reads=[wr, g_col], writes=[wrg])

        hv = hT_d.rearrange("(kc p) t -> p kc t", p=128)
        ov = out_d.rearrange("(kc p) t -> p kc t", p=128)
        wov = wo_d.rearrange("(kc p) n -> p kc n", p=128)
        wa_i = [0]
        wb_i = [0]
        pp = [0]

        def rms(src, gcol, dst_fn):
            c.op("act", "activation", mx[:], src[:], AF.Square,
                 reads=[(src.name, k) for k in range(KC)], writes=[mx])
            for kc in range(KC):
                c.op("pe", "matmul", ps_stat[:], ones_bf[:], mx[:, kc, :], start=(kc == 0), stop=(kc == KC - 1),
                     reads=[mx, ones_bf], writes=[ps_stat])
            c.op("act", "activation", rt[:], ps_stat[:], AF.Sqrt, bias=eps_t[:, 0:1], scale=1.0 / D,
                 reads=[ps_stat, eps_t], writes=[rt])
            c.op("dve", "reciprocal", rstd[:], rt[:], reads=[rt], writes=[rstd])
            for kc in range(KC):
                o, wk = dst_fn(kc)
                c.op("dve", "scalar_tensor_tensor", o, src[:, kc, :], gcol[:, kc:kc + 1], rstd[:], ALU.mult,
                     ALU.mult, reads=[(src.name, kc), gcol, rstd], writes=[wk])

        for tt in range(4):
            ts = slice(tt * 512, (tt + 1) * 512)
            for k4 in range(4):
                c.dma("sp", h1t[:, k4 * 4:(k4 + 1) * 4, :], hv[:, k4 * 4:(k4 + 1) * 4, ts],
                      writes=[(h1t.name, k) for k in range(k4 * 4, k4 * 4 + 4)])
            for k0, nk, src in ((0, 4, cin[0:512, :]), (4, 4, cin[768:1280, :]), (8, 4, ogm_d),
                                (12, 2, cin[512:768, :]), (14, 2, cin[1280:1536, :])):
                c.dma("sp", mx[:, k0:k0 + nk, :], src.rearrange("(kc p) t -> p kc t", p=128)[:, :, ts], writes=[mx])
            for og in range(4):
                w = wA[wa_i[0] % NWB]
                wa_i[0] += 1
                if wbf is not None:
                    c.dma("sp", w[:], wbf["wob"].ap()[og], writes=[(w.name, k) for k in range(4)])
                else:
                    for k4 in range(4):
                        c.dma("pool", w[:, k4 * 4:(k4 + 1) * 4, :],
                              wov[:, k4 * 4:(k4 + 1) * 4, og * 512:(og + 1) * 512], writes=[(w.name, k4)])
                for blk in range(4):
                    dmb = og * 4 + blk
                    p = pso[pp[0] % 2]
                    pp[0] += 1
                    for kc in range(KC):
                        c.op("pe", "matmul", p[:], w[:, kc, blk * 128:(blk + 1) * 128], mx[:, kc, :],
                             start=(kc == 0), stop=(kc == KC - 1), reads=[(w.name, kc // 4), mx], writes=[p])
                    c.op("dve", "tensor_tensor", h1t[:, dmb, :], p[:], h1t[:, dmb, :], ALU.add,
                         reads=[p, (h1t.name, dmb)], writes=[(h1t.name, dmb)])
            rms(h1t, g_col, lambda kc: (fT[:, kc, :], (fT.name, kc)))
            fkeys = [(fT.name, k) for k in range(KC)]
            if moe:
                for kc in range(KC):
                    c.op("pe", "matmul", psr[0:8, :], wrg[:, kc, :], h1t[:, kc, :], start=(kc == 0),
                         stop=(kc == KC - 1), reads=[wrg, (h1t.name, kc)], writes=[psr])
                c.op("dve", "tensor_tensor", lgT[:], psr[0:8, :], rstd[0:8, :], ALU.mult, reads=[psr, rstd],
                     writes=[lgT])
                c.op("dve", "tensor_scalar", lgT[:], lgT[:], br[:, 0:1], None, ALU.add, reads=[lgT, br],
                     writes=[lgT])
                for tb in range(4):
                    c.op("pe", "transpose", psr[:, 64 + tb * 8:64 + (tb + 1) * 8], lgT[:, tb * 128:(tb + 1) * 128],
                         identf[0:8, 0:8], reads=[lgT, identf], writes=[psr])
                c.op("dve", "tensor_copy", lg[:], psr[:, 64:96].rearrange("p (a e) -> p a e", e=8),
                     reads=[psr], writes=[lg])
                for tb in range(4):
                    c.op("dve", "max", m8[:, tb, :], lg[:, tb, :], reads=[lg], writes=[m8])
                c.op("dve", "tensor_tensor", dlt[:], m8[:, :, 1], m8[:, :, 0], ALU.subtract, reads=[m8], writes=[dlt])
                c.op("act", "activation", dlt[:], dlt[:], AF.Exp, reads=[dlt], writes=[dlt])
                c.op("dve", "tensor_scalar", gg1[:], dlt[:], 1.0, None, ALU.add, reads=[dlt], writes=[gg1])
                c.op("dve", "reciprocal", gg1[:], gg1[:], reads=[gg1], writes=[gg1])
                c.op("dve", "tensor_scalar", gg2[:], gg1[:], -1.0, 1.0, ALU.mult, ALU.add, reads=[gg1], writes=[gg2])
                for tb in range(4):
                    c.op("dve", "tensor_scalar", cm1[:, tb, :], lg[:, tb, :], m8[:, tb, 0:1], gg1[:, tb:tb + 1],
                         ALU.is_ge, ALU.mult, reads=[lg, m8, gg1], writes=[cm1])
                    c.op("dve", "tensor_scalar", cm2[:, tb, :], lg[:, tb, :], m8[:, tb, 1:2], gg2[:, tb:tb + 1],
                         ALU.is_equal, ALU.mult, reads=[lg, m8, gg2], writes=[cm2])
                c.op("dve", "tensor_tensor", cm1[:], cm1[:], cm2[:], ALU.add, reads=[cm1, cm2], writes=[cm1])
                for tb in range(4):
                    c.op("pe", "transpose", psr[0:8, tb * 128:(tb + 1) * 128], cm1[:, tb, :], identf[:],
                         reads=[cm1, identf], writes=[psr])
                c.op("dve", "tensor_copy", combT[:], psr[0:8, :], reads=[psr], writes=[combT])
                for e in range(8):
                    c.op("pe", "matmul", psr[:], sel8[:, e * 128:(e + 1) * 128], combT[:], start=True, stop=True,
                         reads=[sel8, combT], writes=[psr])
                    c.op("act", "activation", cbc[:, e, :], psr[:], AF.Copy, reads=[psr], writes=[(cbc.name, e)])
            for e in range(n_exp):
                w1v = w1_d[e].rearrange("(kc p) n -> p kc n", p=128)
                w3v = w3_d[e].rearrange("(kc p) n -> p kc n", p=128)
                w2v = w2_d[e].rearrange("(ch p) n -> p ch n", p=128)
                for j in range(NFB // 2):
                    w = wA[wa_i[0] % NWB]
                    wa_i[0] += 1
                    cs = slice(j * 256, (j + 1) * 256)
                    if wbf is not None:
                        c.dma("sp", w[:], wbf["f13b"].ap()[e, j], writes=[(w.name, k) for k in range(4)])
                    else:
                        for k4 in range(4):
                            c.dma("pool", w[:, k4 * 4:(k4 + 1) * 4, 0:256], w1v[:, k4 * 4:(k4 + 1) * 4, cs],
                                  writes=[(w.name, k4)])
                        for k4 in range(4):
                            c.dma("pool", w[:, k4 * 4:(k4 + 1) * 4, 256:512], w3v[:, k4 * 4:(k4 + 1) * 4, cs],
                                  writes=[(w.name, k4)])
                    for bb in range(2):
                        ffb = j * 2 + bb
                        i2 = pp[0] % 2
                        pp[0] += 1
                        p1, p3 = ps1[i2], ps3[i2]
                        for kc in range(KC):
                            c.op("pe", "matmul", p1[:], w[:, kc, bb * 128:(bb + 1) * 128], fT[:, kc, :],
                                 start=(kc == 0), stop=(kc == KC - 1), reads=[(w.name, kc // 4), (fT.name, kc)],
                                 writes=[p1])
                        for kc in range(KC):
                            c.op("pe", "matmul", p3[:], w[:, kc, 256 + bb * 128:256 + (bb + 1) * 128], fT[:, kc, :],
                                 start=(kc == 0), stop=(kc == KC - 1), reads=[(w.name, kc // 4), (fT.name, kc)],
                                 writes=[p3])
                        s1 = st1[i2]
                        c.op("act", "activation", s1[:], p1[:], AF.Silu, reads=[p1], writes=[s1])
                        if moe:
                            s2 = st2[i2]
                            c.op("dve", "tensor_tensor", s2[:], s1[:], p3[:], ALU.mult, reads=[s1, p3], writes=[s2])
                            c.op("dve", "tensor_tensor", gT[:, ffb, :], s2[:], cbc[:, e, :], ALU.mult,
                                 reads=[s2, (cbc.name, e)], writes=[(gT.name, ffb)])
                        else:
                            c.op("dve", "tensor_tensor", gT[:, ffb, :], s1[:], p3[:], ALU.mult, reads=[s1, p3],
                                 writes=[(gT.name, ffb)])
                gkeys = [(gT.name, k) for k in range(NFB)]
                for dg in range(8):
                    w = wB[wb_i[0] % NWB]
                    wb_i[0] += 1
                    if wbf is not None:
                        c.dma("sp", w[:], wbf["f2b"].ap()[e, dg], writes=[(w.name, 0), (w.name, 1)])
                    else:
                        for c2 in range(2):
                            c.dma("pool", w[:, c2 * 11:(c2 + 1) * 11, :],
                                  w2v[:, c2 * 11:(c2 + 1) * 11, dg * 256:(dg + 1) * 256], writes=[(w.name, c2)])
                    for bb in range(2):
                        dmb = dg * 2 + bb
                        p = pso[pp[0] % 2]
                        pp[0] += 1
                        for ch in range(NFB):
                            c.op("pe", "matmul", p[:], w[:, ch, bb * 128:(bb + 1) * 128], gT[:, ch, :],
                                 start=(ch == 0), stop=(ch == NFB - 1), reads=[(w.name, ch // 11), (gT.name, ch)],
                                 writes=[p])
                        c.op("dve", "tensor_tensor", h1t[:, dmb, :], p[:], h1t[:, dmb, :], ALU.add,
                             reads=[p, (h1t.name, dmb)], writes=[(h1t.name, dmb)])
            if final:
                rms(h1t, gf_col, lambda kc: (h1t[:, kc, :], (h1t.name, kc)))
            for k4 in range(4):
                c.dma("sp", ov[:, k4 * 4:(k4 + 1) * 4, ts], h1t[:, k4 * 4:(k4 + 1) * 4, :],
                      reads=[(h1t.name, k) for k in range(k4 * 4, k4 * 4 + 4)])
        c.finish()
        if not standalone:
            c.release()
        print("C: ins", c.n_ins, "waits", c.n_wait)
    return nc


I32 = mybir.dt.int32
PAIRS = [[0, 1], [2, 3], [4, 5], [6, 7]]


def build_fused():
    nc = bass.Bass("TRN2", target_bir_lowering=False)
    ext = lambda n, sh, d: nc.dram_tensor(n, sh, d, kind="ExternalInput")
    loc = lambda n, sh, d: nc.dram_tensor(n, sh, d)
    shr = lambda n, sh, d: nc.dram_tensor(n, sh, d, addr_space="Shared")
    E = {}

    def e(n, sh, d=F32):
        E[n] = ext(n, sh, d)
        return E[n]

    e("hT0", [D, TOK]); e("half", [1, 1], I32); e("tril", [128, 128])
    for L in range(2):
        e("g1_%d" % L, [128, KC]); e("wall_%d" % L, [D, (N_FM + N_TM) * 512]); e("wgate_%d" % L, [D, 24])
        e("glng_%d" % L, [1, 512]); e("glnb_%d" % L, [1, 512]); e("wsT_%d" % L, [128, 4, 128]); e("bsf_%d" % L, [1, 512])
        e("cw1_%d" % L, [2, 128, 32, 128]); e("cpe_%d" % L, [2, 128, 32]); e("cw2_%d" % L, [2, 128, 128])
        e("gn_%d" % L, [128, 2]); e("wo_%d" % L, [D, D]); e("g2_%d" % L, [128, KC])
    e("d_masks", [128, 17, 512]); e("d_ekc", [128, 128]); e("d_ekw", [128, 128]); e("posq", [4, 3, 512])
    e("d_ek", [128, 4096]); e("d_share", [128, 2, 64]); e("d_btab", [128, 32, 64]); e("d_selg", [128, 12 * 128])
    e("d_ident", [128, 128]); e("d_btl", [128, 192])
    e("d_dec", [128, 2, 128]); e("d_zeta", [128, 2]); e("d_xi", [2, 1, 128]); e("d_dc", [128, 2])
    e("fw1", [2, D, FFE]); e("fw3", [2, D, FFE]); e("fw2", [2, FFE, D])
    e("mw1", [8, D, FFE]); e("mw3", [8, D, FFE]); e("mw2", [8, FFE, D])
    e("d_wr", [128, KC, 8]); e("d_br", [8, 1]); e("d_sel8", [8, 8 * 128]); e("d_gf", [128, KC])
    out = nc.dram_tensor("oh", [D, TOK], F32, kind="ExternalOutput")

    Lc = dict(a16=loc("a16", [2 * HROWS16, TOK], BF16), a32=loc("a32", [2 * HROWS32, TOK], F32),
              ogm=loc("ogm", [512, TOK], BF16),
              b16=loc("b16", [2 * HROWS16, TOK], BF16), b32=loc("b32", [2 * HROWS32, TOK], F32),
              bout=loc("bout", [768, T], BF16), cin=loc("cin", [2 * 768, TOK], BF16),
              oh0=loc("oh0", [D, TOK], F32), bi=loc("bi", [1, 64], F32), bo=loc("bo", [2, 64], F32))
    H16 = HROWS16 * TOK
    H32 = HROWS32 * TOK
    XA16 = shr("XA16", [2 * 2 * HROWS16, TOK], BF16)
    XA32 = shr("XA32", [2 * 2 * HROWS32, TOK], F32)
    XB = shr("XB", [2 * 768, T], BF16)

    WB = dict(f13b=loc("f13b", [2, NFB // 2, 128, KC, 512], BF16), f2b=loc("f2b", [2, 8, 128, NFB, 256], BF16),
              wob=loc("wob", [4, 128, KC, 512], BF16))

    def convert_dense():
        out_ = []
        for og in range(4):
            out_.append((WB["wob"].ap()[og], bass.AP(E["wo_0"], og * 512, [[D, 128], [128 * D, KC], [1, 512]])))
        for e_ in range(2):
            for j in range(NFB // 2):
                for wi, wn in enumerate(("fw1", "fw3")):
                    dst = WB["f13b"].ap()[e_, j][:, :, wi * 256:(wi + 1) * 256]
                    out_.append((dst, bass.AP(E[wn], e_ * D * FFE + j * 256, [[FFE, 128], [128 * FFE, KC], [1, 256]])))
            for dg in range(8):
                out_.append((WB["f2b"].ap()[e_, dg],
                             bass.AP(E["fw2"], e_ * FFE * D + dg * 256, [[D, 128], [128 * D, NFB], [1, 256]])))
        return out_

    def mk_dr(mapping):
        def dr(n, sh, d, k="ExternalInput"):
            t = mapping[n]
            assert list(t.shape) == list(sh), (n, list(t.shape), sh)
            return t.ap()
        return dr

    with contextlib.ExitStack() as top:
        g = nc.gpsimd
        cc = top.enter_context(nc.semaphore("cc"))
        r_half = top.enter_context(g.register("r_half"))
        r_tmp = top.enter_context(g.register("r_tmp"))
        halft = top.enter_context(nc.sbuf_tensor("halft", [1, 1], I32))
        hsem = top.enter_context(nc.semaphore("hsem"))
        g.dma_start(out=halft[:], in_=E["half"].ap()).then_inc(hsem, 16)
        g.wait_ge(hsem, 16)
        g.reg_load(r_half, halft[0:1, 0:1])
        ccn = [0]

        def dyn(t, base, stride, pat):
            g.reg_mul(r_tmp, r_half, stride)
            g.reg_add(r_tmp, r_tmp, base)
            return bass.AP(t, r_tmp, pat)

        def stat(t, base, pat):
            return bass.AP(t, base, pat)

        def barrier(xc):
            xc.finish()
            g.collective_compute("AllGather", ALU.bypass, replica_groups=PAIRS, ins=[Lc["bi"].ap().opt()],
                                 outs=[Lc["bo"].ap().opt()]).then_inc(cc, 1)
            ccn[0] += 1
            g.wait_ge(cc, ccn[0])

        def xchg_A(pfx):
            with contextlib.ExitStack() as st:
                xc = Ctx(nc, st, pfx, top)
                CH = 32768
                xc.dma("pool", dyn(XA16, 0, 2 * H16, [[CH, 2 * H16 // CH], [1, CH]]),
                       stat(Lc["a16"], 0, [[CH, 2 * H16 // CH], [1, CH]]))
                xc.dma("pool", dyn(XA32, 0, 2 * H32, [[TOK, 2 * HROWS32], [1, TOK]]),
                       stat(Lc["a32"], 0, [[TOK, 2 * HROWS32], [1, TOK]]))
                barrier(xc)
                for th in range(2):
                    xc.dma("pool", stat(Lc["b16"], th * H16, [[CH, H16 // CH], [1, CH]]),
                           dyn(XA16, th * 2 * H16, H16, [[CH, H16 // CH], [1, CH]]))
                    xc.dma("pool", stat(Lc["b32"], th * H32, [[TOK, HROWS32], [1, TOK]]),
                           dyn(XA32, th * 2 * H32, H32, [[TOK, HROWS32], [1, TOK]]))
                xc.finish()
                xc.release()

        def xchg_B(pfx):
            with contextlib.ExitStack() as st:
                xc = Ctx(nc, st, pfx, top)
                CH = 32768
                n = 768 * T
                xc.dma("pool", dyn(XB, 0, n, [[CH, n // CH], [1, CH]]), stat(Lc["bout"], 0, [[CH, n // CH], [1, CH]]))
                barrier(xc)
                for hh in range(2):
                    xc.dma("pool", stat(Lc["cin"], hh * 768 * TOK, [[TOK, 768], [1, TOK]]),
                           dyn(XB, hh * n, TOK, [[T, 768], [1, TOK]]))
                xc.finish()
                xc.release()

        for L in range(2):
            hsrc = E["hT0"] if L == 0 else Lc["oh0"]
            mA = dict(hT=hsrc, g1=E["g1_%d" % L], wall=E["wall_%d" % L], wgate=E["wgate_%d" % L],
                      glng=E["glng_%d" % L], glnb=E["glnb_%d" % L], wsT=E["wsT_%d" % L], bsf=E["bsf_%d" % L],
                      tril=E["tril"], a16=Lc["a16"], a32=Lc["a32"], ogm=Lc["ogm"])
            build_A(nc, mk_dr(mA), "A%d_" % L, top)
            xchg_A("XA%d_" % L)
            mB1 = dict(E)
            mB1.update(b16=Lc["b16"], b32=Lc["b32"], d_w1=E["cw1_%d" % L], d_peT=E["cpe_%d" % L],
                       d_w2=E["cw2_%d" % L], bout=Lc["bout"])
            build_B1(nc, mk_dr(mB1), "B1%d_" % L, top, pre_hook=convert_dense if L == 0 else None)
            mB2 = dict(E)
            mB2.update(b16=Lc["b16"], b32=Lc["b32"], d_gn=E["gn_%d" % L], bout=Lc["bout"])
            build_B2(nc, mk_dr(mB2), "B2%d_" % L, top)
            xchg_B("XB%d_" % L)
            moe = (L == 1)
            mC = dict(E)
            mC.update(d_hT=hsrc, cin=Lc["cin"], ogm=Lc["ogm"], d_wo=E["wo_%d" % L], d_g2=E["g2_%d" % L],
                      d_w1=E["mw1"] if moe else E["fw1"], d_w3=E["mw3"] if moe else E["fw3"],
                      d_w2=E["mw2"] if moe else E["fw2"], d_identf=E["d_ident"], oh=out if moe else Lc["oh0"])
            build_C(8 if moe else 2, moe, moe, nc, mk_dr(mC), "C%d_" % L, top, wbf=None if moe else WB)
    return nc


_PROGS = {}


def _col(g):
    return np.ascontiguousarray(np.asarray(g, np.float32).reshape(KC, 128).T)


def kernel(x, ln1_g, w_in, cmp_pe_k, cmp_w1_k, cmp_w2_k, cmp_pe_v, cmp_w1_v, cmp_w2_v,
           gmlp_ln_g, gmlp_ln_b, gmlp_ws, gmlp_bs, ret_gn_g, w_out, ln2_g,
           ffn_w1, ffn_w3, ffn_w2, moe_wr, moe_br, moe_w1, moe_w3, moe_w2, final_g):
    f32 = lambda a: np.asarray(a, np.float32)
    xf = f32(x).reshape(B * T, D)
    if "F" not in _PROGS:
        _PROGS["F"] = build_fused()
    nc = _PROGS["F"]
    sh = {"d_" + k: v for k, v in nsa_consts().items()}
    sh["tril"] = np.triu(np.ones((128, 128), np.float32))
    for L in range(2):
        wall, wgate = pack_A_weights(f32(w_in[L]))
        sh["g1_%d" % L] = _col(ln1_g[L]); sh["wall_%d" % L] = wall; sh["wgate_%d" % L] = wgate
        sh["glng_%d" % L] = f32(gmlp_ln_g[L]).reshape(1, 512); sh["glnb_%d" % L] = f32(gmlp_ln_b[L]).reshape(1, 512)
        sh["wsT_%d" % L] = np.ascontiguousarray(f32(gmlp_ws[L]).transpose(2, 0, 1))
        sh["bsf_%d" % L] = f32(gmlp_bs[L]).reshape(1, 512)
        sh["cw1_%d" % L] = np.ascontiguousarray(np.stack(
            [f32(cmp_w1_k[L]).reshape(32, 128, 128).transpose(1, 0, 2),
             f32(cmp_w1_v[L]).reshape(32, 128, 128).transpose(1, 0, 2)]))
        sh["cpe_%d" % L] = np.ascontiguousarray(np.stack([f32(cmp_pe_k[L]).T, f32(cmp_pe_v[L]).T]))
        sh["cw2_%d" % L] = np.ascontiguousarray(np.stack([f32(cmp_w2_k[L]), f32(cmp_w2_v[L])]))
        sh["wo_%d" % L] = f32(w_out[L]); sh["g2_%d" % L] = _col(ln2_g[L])
    sh["fw1"] = np.ascontiguousarray(f32(ffn_w1[0]).reshape(D, 2, FFE).transpose(1, 0, 2))
    sh["fw3"] = np.ascontiguousarray(f32(ffn_w3[0]).reshape(D, 2, FFE).transpose(1, 0, 2))
    sh["fw2"] = np.ascontiguousarray(f32(ffn_w2[0]).reshape(2, FFE, D))
    sh["mw1"] = f32(moe_w1[0]); sh["mw3"] = f32(moe_w3[0]); sh["mw2"] = f32(moe_w2[0])
    sh["d_wr"] = np.ascontiguousarray(f32(moe_wr[0]).reshape(KC, 128, 8).transpose(1, 0, 2))
    sh["d_br"] = f32(moe_br[0]).reshape(8, 1)
    sh["d_sel8"] = np.repeat(np.eye(8, dtype=np.float32), 128, axis=1)
    sh["d_gf"] = _col(final_g)
    maps = []
    for c in range(NCORES):
        hf = c % 2
        m = dict(sh)
        m["hT0"] = np.ascontiguousarray(xf[c * TOK:(c + 1) * TOK].T)
        m["half"] = np.array([[hf]], np.int32)
        m["posq"] = nsa_posq(hf)
        m["d_btl"] = nsa_bias_table(hf)
        m.update(ret_consts(hf))
        for L in range(2):
            m["gn_%d" % L] = np.ascontiguousarray(f32(ret_gn_g[L])[hf * 256:(hf + 1) * 256].reshape(2, 128).T)
        maps.append(m)
    res = run_bass_kernel_spmd(nc, maps, core_ids=list(range(NCORES))).results
    out = np.concatenate([np.asarray(res[c]["oh"], np.float32).T for c in range(NCORES)], axis=0)
    return np.ascontiguousarray(out.reshape(B, T, D))
```

```python
import contextlib
import numpy as np
import ml_dtypes
import concourse.bass as bass
import concourse.mybir as mybir
from concourse.bass_utils import run_bass_kernel_spmd

F32 = mybir.dt.float32
BF16 = mybir.dt.bfloat16
AF = mybir.ActivationFunctionType
ALU = mybir.AluOpType
AX = mybir.AxisListType
NPBF = ml_dtypes.bfloat16

NCORES = 8
D = 2048
KC = 16
B, T = 4, 4096
TOK = 2048
EPS = 1e-6
NRING = 4
SCALE = 128.0 ** -0.5


class Ctx:
    def __init__(self, nc, stack, pfx="", semstack=None):
        self.nc = nc
        self.stack = stack
        self.pfx = pfx
        self.fused = semstack is not None
        self.all_sems = []

        def mksem(name):
            if self.fused:
                h = nc.alloc_semaphore(name=name)
                self.all_sems.append(h)
                return h
            return stack.enter_context(nc.semaphore(name))
        self.E = {"pe": nc.tensor, "act": nc.scalar, "dve": nc.vector,
                  "pool": nc.gpsimd, "sp": nc.sync}
        self.csem = {}
        self.ccnt = {}
        for e in ("pe", "act", "dve", "pool"):
            self.csem[e] = mksem(pfx + "c_" + e)
            self.ccnt[e] = 0
        self.dsem = {}
        self.dcnt = {}
        for q in ("sp", "pool"):
            self.dsem[q] = [mksem(pfx + "d_%s%d" % (q, i)) for i in range(NRING)]
            self.dcnt[q] = 0
        self.waited = {e: {} for e in self.E}
        self.writer = {}
        self.readers = {}
        self.n_wait = 0
        self.n_ins = 0
        self.psum_names = set()
        self.psum_last = {}

    def sb(self, name, shape, dt):
        return self.stack.enter_context(self.nc.sbuf_tensor(self.pfx + name, shape, dt))

    def ps(self, name, shape, dt=F32):
        t = self.stack.enter_context(self.nc.psum_tensor(self.pfx + name, shape, dt))
        self.psum_names.add(t.name)
        return t

    @staticmethod
    def key(x):
        if isinstance(x, (str, tuple)):
            return x
        t = getattr(x, "tensor", None)
        if t is not None:
            return t.name
        return x.name

    def _need(self, e, ev, skip_same=None):
        if ev is None:
            return
        sem, val, src = ev
        if src == skip_same:
            return
        w = self.waited[e]
        k = id(sem)
        if w.get(k, 0) >= val:
            return
        self.E[e].wait_ge(sem, val)
        self.n_wait += 1
        w[k] = val

    def _base(self, b):
        k = self.key(b)
        return k[0] if isinstance(k, tuple) else k

    def _sync(self, e, reads, writes, same_ok=False):
        skip = e if same_ok else None
        for b in list(reads) + list(writes):
            base = self._base(b)
            if base in self.psum_names:
                for e2, ev in self.psum_last.get(base, {}).items():
                    if e2 != e:
                        self._need(e, ev, skip)
        for b in reads:
            self._need(e, self.writer.get(self.key(b)), skip)
        for b in writes:
            k = self.key(b)
            self._need(e, self.writer.get(k), skip)
            for ev in self.readers.get(k, {}).values():
                self._need(e, ev, skip)

    def _record(self, ev, reads, writes):
        for b in list(reads) + list(writes):
            base = self._base(b)
            if base in self.psum_names:
                self.psum_last.setdefault(base, {})[ev[2]] = ev
        for b in reads:
            self.readers.setdefault(self.key(b), {})[id(ev[0])] = ev
        for b in writes:
            k = self.key(b)
            self.writer[k] = ev
            self.readers[k] = {}

    def op(self, e, name, *args, reads=(), writes=(), **kw):
        self._sync(e, reads, writes, same_ok=(e == "pe"))
        ins = getattr(self.E[e], name)(*args, **kw)
        self.ccnt[e] += 1
        ins.then_inc(self.csem[e], 1)
        ev = (self.csem[e], self.ccnt[e], e)
        self._record(ev, reads, writes)
        self.n_ins += 1
        return ins

    def dma(self, q, out, in_, reads=(), writes=(), **kw):
        self._sync(q, reads, writes)
        i = self.dcnt[q]
        self.dcnt[q] += 1
        sem = self.dsem[q][i % NRING]
        val = 16 * (i // NRING + 1)
        self.E[q].dma_start(out=out, in_=in_, **kw).then_inc(sem, 16)
        ev = (sem, val, "dma_" + q)
        self._record(ev, reads, writes)
        self.n_ins += 1
        return ev

    def release(self):
        self.nc.all_engine_barrier()
        self.nc.clear_and_free_semaphores(self.all_sems)
        self.nc.all_engine_barrier()

    def finish(self):
        for q in ("sp", "pool"):
            n = self.dcnt[q]
            for s in range(NRING):
                cnt = (n - s + NRING - 1) // NRING if n > s else 0
                if cnt > 0:
                    self.E[q].wait_ge(self.dsem[q][s], 16 * cnt)


def bcast_rows(ap, nparts):
    return bass.AP(ap.tensor, ap.offset, [[0, nparts]] + [list(x) for x in ap.ap[1:]])


def rmsnorm_tiles(c, hT_dram, g_col, aT, ones_bf, eps_t, ht, sq, ps_stat, rt, rstd, ntt, q="sp"):
    hv = hT_dram.rearrange("(kc p) t -> p kc t", p=128)
    for tt in range(ntt):
        ts = slice(tt * 512, (tt + 1) * 512)
        for k4 in range(4):
            c.dma(q, ht[:, k4 * 4:(k4 + 1) * 4, :], hv[:, k4 * 4:(k4 + 1) * 4, ts],
                  writes=[(ht.name, k4)])
        c.op("act", "activation", sq[:], ht[:], AF.Square,
             reads=[(ht.name, k) for k in range(4)], writes=[sq])
        for kc in range(KC):
            c.op("pe", "matmul", ps_stat[:], ones_bf[:], sq[:, kc, :], start=(kc == 0),
                 stop=(kc == KC - 1), reads=[sq, ones_bf], writes=[ps_stat])
        c.op("act", "activation", rt[:], ps_stat[:], AF.Sqrt, bias=eps_t[:, 0:1], scale=1.0 / D,
             reads=[ps_stat, eps_t], writes=[rt])
        c.op("dve", "reciprocal", rstd[:], rt[:], reads=[rt], writes=[rstd])
        for kc in range(KC):
            c.op("dve", "scalar_tensor_tensor", aT[:, kc, ts], ht[:, kc, :], g_col[:, kc:kc + 1],
                 rstd[:], ALU.mult, ALU.mult,
                 reads=[(ht.name, kc // 4), g_col, rstd], writes=[(aT.name, tt)])


def load_w(c, q, wt, src3, ncol, tag):
    for k4 in range(4):
        c.dma(q, wt[:, k4 * 4:(k4 + 1) * 4, 0:ncol], src3[:, k4 * 4:(k4 + 1) * 4, :],
              writes=[(wt.name, k4)])


HROWS16 = 2304
HROWS32 = 268
N_FM = 8
N_TM = 4


def _default_dr(nc):
    return lambda n, s, d, k="ExternalInput": nc.dram_tensor(n, s, d, kind=k).ap()


def build_A(nc=None, dr=None, pfx="", semstack=None):
    standalone = nc is None
    if standalone:
        nc = bass.Bass("TRN2", target_bir_lowering=False)
        dr = _default_dr(nc)
    dt = dr
    hT = dt("hT", [D, TOK], F32, "ExternalInput")
    g1 = dt("g1", [128, KC], F32, "ExternalInput")
    wall = dt("wall", [D, (N_FM + N_TM) * 512], F32, "ExternalInput")
    wgate = dt("wgate", [D, 24], F32, "ExternalInput")
    glng = dt("glng", [1, 512], F32, "ExternalInput")
    glnb = dt("glnb", [1, 512], F32, "ExternalInput")
    wsT = dt("wsT", [128, 4, 128], F32, "ExternalInput")
    bsf = dt("bsf", [1, 512], F32, "ExternalInput")
    tril = dt("tril", [128, 128], F32, "ExternalInput")
    a16 = dt("a16", [2 * HROWS16, TOK], BF16, "ExternalOutput")
    a32 = dt("a32", [2 * HROWS32, TOK], F32, "ExternalOutput")
    ogm = dt("ogm", [512, TOK], BF16, "ExternalOutput")

    with contextlib.ExitStack() as st:
        c = Ctx(nc, st, pfx, semstack)
        aT = c.sb("aT", [128, KC, TOK], BF16)
        ht = c.sb("ht", [128, KC, 512], F32)
        sq = c.sb("sq", [128, KC, 512], BF16)
        wb = [c.sb("wb%d" % i, [128, KC, 512], BF16) for i in range(2)]
        wg = c.sb("wg", [128, KC, 24], BF16)
        uT = c.sb("uT", [128, 4, TOK], BF16)
        ones_bf = c.sb("ones_bf", [128, 128], BF16)
        eps_t = c.sb("eps_t", [128, 1], F32)
        g_col = c.sb("g_col", [128, KC], F32)
        rt = c.sb("rt", [128, 512], F32)
        rstd = c.sb("rstd", [128, 512], F32)
        lng = c.sb("lng", [128, 512], F32)
        lnb = c.sb("lnb", [128, 512], F32)
        bsb = c.sb("bsb", [128, 512], F32)
        wsf = c.sb("wsf", [128, 4, 128], F32)
        trl = c.sb("trl", [128, 128], F32)
        wsm = c.sb("wsm", [128, 4, 128], BF16)
        stg = [c.sb("stg%d" % i, [128, 4, 512], BF16) for i in range(2)]
        stgf = [c.sb("stgf%d" % i, [128, 4, 512], F32) for i in range(1)]
        stgt = [c.sb("stgt%d" % i, [128, 512], BF16) for i in range(2)]
        gst = c.sb("gst", [24, 512], F32)
        vg = c.sb("vg", [128, 512], F32)
        vn = c.sb("vn", [128, 512], F32)
        vln = c.sb("vln", [128, 512], BF16)
        bst = c.sb("bst", [128, 6], F32)
        mv = c.sb("mv", [128, 2], F32)
        sd = c.sb("sd", [128, 1], F32)
        rs1 = c.sb("rs1", [128, 1], F32)
        tmpg = c.sb("tmpg", [128, 512], F32)
        ogs = [c.sb("ogs%d" % i, [128, 4, 512], BF16) for i in range(1)]
        pst = [c.ps("pst%d" % i, [128, 512]) for i in range(6)]
        ps_stat = c.ps("ps_stat", [128, 512])
        psg = c.ps("psg", [128, 512])

        c.op("pool", "memset", ones_bf[:], 1.0, writes=[ones_bf])
        c.op("pool", "memset", eps_t[:], EPS, writes=[eps_t])
        c.dma("sp", g_col[:], g1, writes=[g_col])
        c.dma("sp", lng[:], bcast_rows(glng, 128), writes=[lng])
        c.dma("sp", lnb[:], bcast_rows(glnb, 128), writes=[lnb])
        c.dma("sp", bsb[:], bcast_rows(bsf, 128), writes=[bsb])
        c.dma("sp", wsf[:], wsT, writes=[wsf])
        c.dma("sp", trl[:], tril, writes=[trl])
        for g in range(4):
            c.op("dve", "tensor_tensor", wsm[:, g, :], wsf[:, g, :], trl[:], ALU.mult,
                 reads=[wsf, trl], writes=[wsm])
        c.dma("pool", wg[:], wgate.rearrange("(kc p) n -> p kc n", p=128), writes=[wg])

        wv = wall.rearrange("(kc p) n -> p kc n", p=128)
        load_w(c, "pool", wb[0], wv[:, :, 0:512], 512, 0)

        rmsnorm_tiles(c, hT, g_col, aT, ones_bf, eps_t, ht, sq, ps_stat, rt, rstd, 4)
        aT_all = [(aT.name, tt) for tt in range(4)]

        pcount = [0]

        def next_ps():
            p = pst[pcount[0] % len(pst)]
            pcount[0] += 1
            return p

        for tt in range(4):
            ts = slice(tt * 512, (tt + 1) * 512)
            p = next_ps()
            for kc in range(KC):
                c.op("pe", "matmul", p[0:24, :], wg[:, kc, :], aT[:, kc, ts], start=(kc == 0),
                     stop=(kc == KC - 1), reads=[wg, (aT.name, tt)], writes=[p])
            c.op("act", "activation", gst[:], p[0:24, :], AF.Sigmoid, reads=[p], writes=[gst])
            for hf in range(2):
                c.dma("sp", a32[hf * HROWS32 + 256:hf * HROWS32 + 268, ts], gst[hf * 12:(hf + 1) * 12, :], reads=[gst])

        ev = 0
        for g in range(N_FM + N_TM):
            w = wb[g % 2]
            if g + 1 < N_FM + N_TM:
                load_w(c, "pool", wb[(g + 1) % 2], wv[:, :, (g + 1) * 512:(g + 2) * 512], 512, g + 1)
            wkeys = [(w.name, k) for k in range(4)]
            if g < N_FM:
                for tt in range(4):
                    ts = slice(tt * 512, (tt + 1) * 512)
                    if g == 6:
                        s_t = stgf[0]
                    elif g == 7:
                        s_t = None
                    else:
                        s_t = stg[(g * 4 + tt) % 2]
                    for blk in range(4):
                        p = next_ps()
                        for kc in range(KC):
                            c.op("pe", "matmul", p[:], w[:, kc, blk * 128:(blk + 1) * 128], aT[:, kc, ts],
                                 start=(kc == 0), stop=(kc == KC - 1),
                                 reads=[(w.name, kc // 4), (aT.name, tt)], writes=[p])
                        if g == 7:
                            c.op("act", "activation", uT[:, blk, ts], p[:], AF.Gelu_apprx_tanh,
                                 reads=[p], writes=[(uT.name, tt, blk)])
                        elif g == 6:
                            c.op("act", "activation", s_t[:, blk, :], p[:], AF.Silu,
                                 reads=[p], writes=[(s_t.name, blk)])
                        else:
                            sc = SCALE if g in (0, 3) else 1.0
                            if ev % 2 == 0:
                                c.op("act", "activation", s_t[:, blk, :], p[:], AF.Copy, scale=sc,
                                     reads=[p], writes=[(s_t.name, blk)])
                            else:
                                c.op("dve", "tensor_scalar", s_t[:, blk, :], p[:], sc, None, ALU.mult,
                                     reads=[p], writes=[(s_t.name, blk)])
                            ev += 1
                    if g == 6:
                        for hf in range(2):
                            dst = a32[hf * HROWS32:hf * HROWS32 + 256, :].rearrange("(b p) t -> p b t", p=128)[:, :, ts]
                            c.dma("sp", dst, s_t[:, hf * 2:(hf + 1) * 2, :], reads=[(s_t.name, b_) for b_ in range(4)])
                    elif g < 6:
                        r0 = (g // 3) * HROWS16 + (g % 3) * 512
                        dst = a16[r0:r0 + 512, :].rearrange("(b p) t -> p b t", p=128)[:, :, ts]
                        c.dma("sp", dst, s_t[:], reads=[(s_t.name, b_) for b_ in range(4)])
            else:
                gi = g - N_FM
                for tb in range(16):
                    tt = tb // 4
                    tks = slice(tb * 128, (tb + 1) * 128)
                    p = next_ps()
                    for kc in range(KC):
                        c.op("pe", "matmul", p[:], aT[:, kc, tks], w[:, kc, :], start=(kc == 0),
                             stop=(kc == KC - 1), reads=[(w.name, kc // 4), (aT.name, tt)], writes=[p])
                    if gi < 3:
                        s_t = stgt[tb % 2]
                        if tb % 2 == 0:
                            c.op("act", "activation", s_t[:], p[:], AF.Copy, reads=[p], writes=[s_t])
                        else:
                            c.op("dve", "tensor_copy", s_t[:], p[:], reads=[p], writes=[s_t])
                        for hf in range(2):
                            off = (hf * HROWS16 + 1536) * TOK + tb * 128 * 768 + gi * 256
                            c.dma("sp", bass.AP(a16.tensor, off, [[768, 128], [1, 256]]),
                                  s_t[:, hf * 256:(hf + 1) * 256], reads=[s_t])
                    else:
                        c.op("act", "activation", vg[:], p[:], AF.Gelu_apprx_tanh, reads=[p], writes=[vg])
                        c.op("dve", "bn_stats", bst[:], vg[:], reads=[vg], writes=[bst])
                        c.op("dve", "bn_aggr", mv[:], bst[:], reads=[bst], writes=[mv])
                        c.op("act", "activation", sd[:], mv[:, 1:2], AF.Sqrt, bias=eps_t[:, 0:1], scale=1.0,
                             reads=[mv, eps_t], writes=[sd])
                        c.op("dve", "reciprocal", rs1[:], sd[:], reads=[sd], writes=[rs1])
                        c.op("dve", "tensor_scalar", vn[:], vg[:], mv[:, 0:1], rs1[:, 0:1], ALU.subtract,
                             ALU.mult, reads=[vg, mv, rs1], writes=[vn])
                        c.op("pool", "tensor_tensor", vn[:], vn[:], lng[:], ALU.mult,
                             reads=[vn, lng], writes=[vn])
                        c.op("pool", "tensor_tensor", vln[:], vn[:], lnb[:], ALU.add,
                             reads=[vn, lnb], writes=[vln])
                        for gg in range(4):
                            c.op("pe", "matmul", psg[:, gg * 128:(gg + 1) * 128], vln[:, gg * 128:(gg + 1) * 128],
                                 wsm[:, gg, :], start=True, stop=True, reads=[vln, wsm], writes=[psg])
                        c.op("dve", "tensor_tensor", tmpg[:], psg[:], bsb[:], ALU.add,
                             reads=[psg, bsb], writes=[tmpg])
                        og = ogs[0]
                        c.op("pool", "tensor_tensor", og[:, :, (tb % 4) * 128:(tb % 4 + 1) * 128],
                             tmpg[:].rearrange("p (g t) -> p g t", g=4), uT[:, :, tks], ALU.mult,
                             reads=[tmpg] + [(uT.name, tt, b_) for b_ in range(4)],
                             writes=[(og.name, tb % 4)])
                        if tb % 4 == 3:
                            dst = ogm.rearrange("(g p) t -> p g t", p=128)[:, :, tt * 512:(tt + 1) * 512]
                            c.dma("sp", dst, og[:], reads=[(og.name, b_) for b_ in range(4)])
        c.finish()
        if not standalone:
            c.release()
        print("A: ins", c.n_ins, "waits", c.n_wait)
    return nc


_OFF = {}
_o = 0
for _n, _w in (("q", 1024), ("kcmp", 256), ("vcmp", 256), ("kslc", 256), ("vslc", 256), ("kwin", 256),
               ("vwin", 256), ("gate", 24), ("u", 512), ("v", 512), ("rq", 512), ("rk", 512),
               ("rv", 512), ("rg", 512)):
    _OFF[_n] = (_o, _o + _w)
    _o += _w


def pack_A_weights(w_in_l):
    cols = lambda n: w_in_l[:, _OFF[n][0]:_OFF[n][1]]
    hcol = lambda n, hf, w: w_in_l[:, _OFF[n][0] + hf * w:_OFF[n][0] + (hf + 1) * w]
    fm = []
    for hf in range(2):
        fm += [hcol("q", hf, 512), hcol("kcmp", hf, 128), hcol("vcmp", hf, 128), hcol("kslc", hf, 128),
               hcol("kwin", hf, 128), hcol("rq", hf, 256), hcol("rk", hf, 256)]
    fm += [cols("rg"), cols("u")]
    tm = [hcol("vslc", 0, 128), hcol("vwin", 0, 128), hcol("vslc", 1, 128), hcol("vwin", 1, 128),
          cols("rk"), cols("rv"), cols("v")]
    wall = np.ascontiguousarray(np.concatenate(fm + tm, axis=1))
    assert wall.shape[1] == (N_FM + N_TM) * 512
    return wall, np.ascontiguousarray(cols("gate"))


NEG = -30000.0
NCMP = 255


def nsa_consts():
    p = np.arange(128)[:, None]
    f = np.arange(512)[None, :]
    nbc = np.stack([np.where(f - p - 128 * r >= 0, 0.0, NEG) for r in range(4)])
    nbw = np.stack([np.where(f - p - 128 * r < 512, 0.0, NEG) for r in (-4, -3, -2, -1)])
    nbm = np.stack([np.where(f - 16 * p + 512 * r - 31 >= 0, 0.0, NEG) for r in range(5)])
    nbm1 = nbm[0:4].copy()
    nbm1[:, 127, :] = NEG
    masks = np.concatenate([nbc, nbw, nbm, nbm1], 0).astype(np.float32)
    masks = np.ascontiguousarray(masks.transpose(1, 0, 2))
    pk = np.arange(128)
    posk = np.stack([pk, np.ones(128), np.ones(128)]).astype(np.float32)
    poskc = np.stack([16 * pk, np.ones(128), np.ones(128)]).astype(np.float32)
    ff = np.arange(512)
    e30 = np.zeros((64, 4096), np.float32)
    for j in range(64):
        e30[j, j * 64:(j + 1) * 64] = 30000.0
    c0 = np.arange(256)[:, None] * 16
    s0 = np.arange(64)[None, :] * 64
    ov = np.minimum(c0 + 32, s0 + 64) - np.maximum(c0, s0)
    share = (np.clip(ov, 0, 32) / 32.0).astype(np.float32)
    share[255] = 0
    share = np.ascontiguousarray(share.reshape(2, 128, 64).transpose(1, 0, 2))
    t = np.arange(4096)
    cur = (t // 64)[:, None]
    j = np.arange(64)[None, :]
    forced = (j == 0) | (j == cur) | (j == cur - 1)
    btab = np.where(forced, 1e4, 0.0)
    btab = np.where(j > cur, -1e30, btab).astype(np.float32)
    btab = np.ascontiguousarray(btab.reshape(32, 128, 64).transpose(1, 0, 2))
    selg = np.zeros((128, 12, 128), np.float32)
    for n in range(12):
        selg[n, n, :] = 1.0
        selg[32 + n, n, :] = 1.0
    selg = selg.reshape(128, 12 * 128)
    ek = np.zeros((128, 4096), np.float32)
    ek[0:64] = e30
    ek[64:67] = np.tile(posk, (1, 32))
    ekc = np.zeros((128, 128), np.float32)
    ekc[64:67] = poskc
    ekw = np.zeros((128, 128), np.float32)
    ekw[64:67] = posk
    return dict(masks=masks, ek=ek, ekc=ekc, ekw=ekw, share=share, btab=btab, selg=selg,
                ident=np.eye(128, dtype=np.float32))


def nsa_posq(hf):
    ff = np.arange(512)
    out = np.zeros((4, 3, 512), np.float32)
    for r in range(4):
        h = 4 * hf + r
        slope = 2.0 ** (-8.0 * (h + 1) / 8)
        out[r, 0] = slope
        out[r, 1] = -slope * 64 * (ff // 64)
        out[r, 2] = -slope * (ff % 64)
    return out


def nsa_bias_table(hf):
    tb = np.zeros((192,), np.float32)
    for r in range(4):
        slope = 2.0 ** (-(4 * hf + r + 1))
        for cb in range(2):
            for tc in range(8):
                tb[r * 16 + cb * 8 + tc] = slope * (2048 * cb + 31 - 512 * tc)
        for rel in range(-28, 4):
            tb[64 + r * 32 + rel + 28] = slope * 128 * rel
    return np.ascontiguousarray(np.broadcast_to(tb[None, :], (128, 192)))


def build_B1(nc=None, dr=None, pfx="", semstack=None, pre_hook=None):
    standalone = nc is None
    if standalone:
        nc = bass.Bass("TRN2", target_bir_lowering=False)
        dr = _default_dr(nc)
    dt = dr
    b16 = dt("b16", [2 * HROWS16, TOK], BF16)
    b32 = dt("b32", [2 * HROWS32, TOK], F32)
    w1_d = dt("d_w1", [2, 128, 32, 128], F32)
    pe_d = dt("d_peT", [2, 128, 32], F32)
    w2_d = dt("d_w2", [2, 128, 128], F32)
    masks_d = dt("d_masks", [128, 17, 512], F32)
    ekc_d = dt("d_ekc", [128, 128], F32)
    ekw_d = dt("d_ekw", [128, 128], F32)
    posq_d = dt("posq", [4, 3, 512], F32)
    ek_d = dt("d_ek", [128, 4096], F32)
    share_d = dt("d_share", [128, 2, 64], F32)
    btab_d = dt("d_btab", [128, 32, 64], F32)
    selg_d = dt("d_selg", [128, 12 * 128], F32)
    ident_d = dt("d_ident", [128, 128], F32)
    btl_d = dt("d_btl", [128, 192], F32)
    out_d = dt("bout", [768, T], BF16, "ExternalOutput")

    with contextlib.ExitStack() as st:
        c = Ctx(nc, st, pfx, semstack)
        qT = c.sb("qT", [128, 4, T], BF16)
        kT = c.sb("kT", [128, 4, T], BF16)
        vs = c.sb("vs", [128, 32, 128], BF16)
        vw = c.sb("vw", [128, 32, 128], BF16)
        masks = c.sb("masks", [128, 17, 512], BF16)
        ekc = c.sb("ekc", [128, 128], BF16)
        ekw = c.sb("ekw", [128, 128], BF16)
        ek = c.sb("ek", [128, 4096], BF16)
        nq = [c.sb("nq%d" % r, [128, 512], BF16) for r in range(4)]
        gst32 = [c.sb("gst32_%d" % i, [128, 512], F32) for i in range(2)]
        gh = [c.sb("gh%d" % i, [128, 512], BF16) for i in range(2)]
        hb = c.sb("hb", [128, 512], BF16)
        share = c.sb("share", [128, 2, 64], BF16)
        btab = c.sb("btab", [128, 32, 64], F32)
        selg = c.sb("selg", [128, 12 * 128], BF16)
        ident = c.sb("ident", [128, 128], BF16)
        identf = c.sb("identf", [128, 128], F32)
        ones = c.sb("ones", [128, 128], BF16)
        w1 = [c.sb("w1_%d" % i, [128, 32, 128], BF16) for i in range(2)]
        peT = c.sb("peT", [128, 2, 32], BF16)
        w2 = c.sb("w2", [128, 2, 128], BF16)
        b1 = c.sb("b1", [128, 2], F32)
        g1 = c.sb("g1", [128, 2, 256], BF16)
        kccT = c.sb("kccT", [128, 256], BF16)
        vcc = c.sb("vcc", [128, 2, 128], BF16)
        Et = [c.sb("Et%d" % i, [128, 512], BF16) for i in range(3)]
        Pn = c.sb("Pn", [128, 4, 2, 512], BF16)
        rd = c.sb("rd", [128, 512], F32)
        lnd = c.sb("lnd", [128, 512], F32)
        tiny = c.sb("tiny", [128, 1], F32)
        wgt = c.sb("wgt", [128, 512], F32)
        tmp = c.sb("tmp", [128, 512], F32)
        acc = c.sb("acc", [128, 512], F32)
        caccs = [c.sb("cacc%d" % i, [128, 4, 512], F32) for i in range(2)]
        ost = [c.sb("ost%d" % i, [128, 512], BF16) for i in range(2)]
        scr = c.sb("scr", [128, 4, 64], F32)
        scr2 = c.sb("scr2", [128, 4, 64], F32)
        m8 = c.sb("m8", [128, 4, 16], F32)
        thr = c.sb("thr", [128, 4], F32)
        psS = [c.ps("psS%d" % i, [128, 512]) for i in range(2)]
        psOs = [c.ps("psO%d" % i, [128, 512]) for i in range(2)]
        psDs = [c.ps("psD%d" % i, [128, 512]) for i in range(2)]
        psG = c.ps("psG", [128, 512])
        psI = c.ps("psIX", [128, 512])
        psX = psI
        sm1 = c.sb("sm1b", [128, 4, 64], F32)

        btl = c.sb("btl", [128, 192], F32)
        c.dma("sp", btl[:], btl_d, writes=[btl])
        c.op("pool", "memset", ones[:], 1.0, writes=[ones])
        c.op("pool", "memset", tiny[:], 1e-18, writes=[tiny])
        for th in range(2):
            tsl = slice(th * TOK, (th + 1) * TOK)
            for r in range(4):
                c.dma("sp", qT[:, r, tsl], b16[th * HROWS16 + r * 128:th * HROWS16 + (r + 1) * 128, :],
                      writes=[(qT.name, r)])
                c.dma("sp", kT[:, r, tsl], b16[th * HROWS16 + 512 + r * 128:th * HROWS16 + 512 + (r + 1) * 128, :],
                      writes=[(kT.name, r)])
            toff = (th * HROWS16 + 1536) * TOK
            c.dma("sp", vs[:, th * 16:(th + 1) * 16, :],
                  bass.AP(b16.tensor, toff, [[768, 128], [768 * 128, 16], [1, 128]]), writes=[vs])
            c.dma("sp", vw[:, th * 16:(th + 1) * 16, :],
                  bass.AP(b16.tensor, toff + 128, [[768, 128], [768 * 128, 16], [1, 128]]), writes=[vw])
        c.dma("pool", masks[:], masks_d, writes=[masks])
        c.dma("pool", ekc[:], ekc_d, writes=[ekc])
        c.dma("pool", ekw[:], ekw_d, writes=[ekw])
        for r in range(4):
            c.op("dve", "memset", nq[r][:], 0.0, writes=[nq[r]])
            c.dma("pool", nq[r][64:67, :], posq_d[r], writes=[nq[r]])
        for i in range(2):
            c.op("dve", "memset", gst32[i][:], 0.0, writes=[gst32[i]])
            c.op("dve", "memset", gh[i][:], 0.0, writes=[gh[i]])
        c.op("dve", "memset", g1[:], 0.0, writes=[(g1.name, 0), (g1.name, 1)])
        c.op("dve", "memset", kccT[:], 0.0, writes=[kccT])
        c.op("dve", "memset", vcc[:], 0.0, writes=[(vcc.name, 0), (vcc.name, 1)])
        c.dma("pool", ek[:], ek_d, writes=[ek])
        c.dma("pool", share[:], share_d, writes=[share])
        c.dma("sp", btab[:], btab_d, writes=[btab])
        c.dma("pool", selg[:], selg_d, writes=[selg])
        c.dma("pool", ident[:], ident_d, writes=[ident])
        c.dma("sp", identf[:], ident_d, writes=[identf])
        for i in range(2):
            c.dma("pool", w1[i][:], w1_d[i], writes=[w1[i]])
            c.dma("pool", peT[:, i, :], pe_d[i], writes=[(peT.name, i)])
            c.dma("pool", w2[:, i, :], w2_d[i], writes=[(w2.name, i)])

        bg = list(pre_hook()) if pre_hook is not None else []
        for i in range(2):
            for j in range(32):
                c.op("pe", "matmul", psX[:, 0:1], w1[i][:, j, :], peT[:, i, j:j + 1], start=(j == 0),
                     stop=(j == 31), reads=[w1[i], (peT.name, i)], writes=[psX])
            c.op("dve", "tensor_copy", b1[:, i:i + 1], psX[:, 0:1], reads=[psX], writes=[(b1.name, i)])
            for j in range(32):
                c.op("pe", "matmul", psI[:, 0:NCMP], w1[i][:, j, :], kT[:, i, j:j + 16 * (NCMP - 1) + 1:16],
                     start=(j == 0), stop=(j == 31), reads=[w1[i], (kT.name, i)], writes=[psI])
            c.op("act", "activation", g1[:, i, 0:NCMP], psI[:, 0:NCMP], AF.Gelu_apprx_tanh,
                 bias=b1[:, i:i + 1], scale=1.0, reads=[psI, (b1.name, i)], writes=[(g1.name, i)])
        c.op("pe", "matmul", psI[:, 0:NCMP], w2[:, 0, :], g1[:, 0, 0:NCMP], start=True, stop=True,
             reads=[(w2.name, 0), (g1.name, 0)], writes=[psI])
        c.op("dve", "tensor_copy", kccT[:, 0:NCMP], psI[:, 0:NCMP], reads=[psI], writes=[kccT])
        for cb in range(2):
            M = 128
            c.op("pe", "matmul", psX[0:M, 0:128], g1[:, 1, cb * 128:cb * 128 + M], w2[:, 1, :], start=True,
                 stop=True, reads=[(w2.name, 1), (g1.name, 1)], writes=[psX])
            c.op("dve", "tensor_copy", vcc[0:M, cb, :], psX[0:M, 0:128], reads=[psX], writes=[(vcc.name, cb)])

        sctr = [0]
        octr = [0]

        def next_S():
            p = psS[sctr[0] % 2]
            e = Et[sctr[0] % 3]
            sctr[0] += 1
            return p, e

        def run_blocks(blocks):
            psO = psOs[octr[0] % 2]
            psD = psDs[octr[0] % 2]
            octr[0] += 1
            n = len(blocks)
            pend = None

            def pv(b, e, i):
                M = b["M"]
                c.op("pe", "matmul", psO[:], b["v"], e[0:M, :], start=(i == 0), stop=(i == n - 1),
                     reads=[b["vkey"], e], writes=[psO])
                c.op("pe", "matmul", psD[:], ones[0:M, :], e[0:M, :], start=(i == 0), stop=(i == n - 1),
                     reads=[ones, e], writes=[psD])
                if b.get("keep") is not None:
                    dst, key = b["keep"]
                    c.op("pool", "tensor_copy", dst, e[0:M, :], reads=[e], writes=[key])

            for i, b in enumerate(blocks):
                p, e = next_S()
                M = b["M"]
                ns = len(b["s_ops"])
                for j, (lh, rh, rd) in enumerate(b["s_ops"]):
                    c.op("pe", "matmul", p[0:M, :], lh, rh, start=(j == 0), stop=(j == ns - 1), reads=rd, writes=[p])
                c.op("act", "activation", e[0:M, :], p[0:M, :], AF.Exp, bias=b["bias"], scale=1.0,
                     reads=[p, btl], writes=[e])
                if pend is not None:
                    pv(*pend)
                pend = (b, e, i)
            pv(*pend)
            return psO, psD

        def branch_finish(tc, r, br, first, psO, psD):
            g = gh[tc % 2]
            c.op("act", "activation", lnd[:], psD[:], AF.Ln, bias=tiny[:, 0:1], scale=1.0, reads=[psD, tiny], writes=[lnd])
            c.op("act", "activation", rd[:], lnd[:], AF.Exp, scale=-1.0, reads=[lnd], writes=[rd])
            n = r * 3 + br
            c.op("pe", "matmul", psG[:], selg[:, n * 128:(n + 1) * 128], g[:], start=True, stop=True,
                 reads=[selg, g], writes=[psG])
            c.op("dve", "tensor_tensor", wgt[:], psG[:], rd[:], ALU.mult, reads=[psG, rd], writes=[wgt])
            if first:
                c.op("dve", "tensor_tensor", acc[:], psO[:], wgt[:], ALU.mult, reads=[psO, wgt], writes=[acc])
            else:
                c.op("dve", "tensor_tensor", tmp[:], psO[:], wgt[:], ALU.mult, reads=[psO, wgt], writes=[tmp])
                c.op("pool", "tensor_tensor", acc[:], acc[:], tmp[:], ALU.add, reads=[acc, tmp], writes=[acc])

        def front(tc):
            ts = slice(tc * 512, (tc + 1) * 512)
            g = gh[tc % 2]
            g32 = gst32[tc % 2]
            cacc = caccs[tc % 2]

            def load_gates(tcn):
                gr0 = (tcn // 4) * HROWS32 + 256
                gsrc = b32[gr0:gr0 + 12, (tcn % 4) * 512:(tcn % 4 + 1) * 512]
                c.dma("sp", gst32[tcn % 2][0:12, :], gsrc, writes=[gst32[tcn % 2]])
                c.dma("sp", gst32[tcn % 2][32:44, :], gsrc, writes=[gst32[tcn % 2]])

            if tc == 0:
                load_gates(0)
            c.op("act", "activation", g[0:12, :], g32[0:12, :], AF.Copy, reads=[g32], writes=[g])
            c.op("act", "activation", hb[32:44, :], g32[32:44, :], AF.Copy, reads=[g32], writes=[hb])
            c.op("dve", "tensor_tensor", g[32:44, :], g32[32:44, :], hb[32:44, :], ALU.subtract,
                 reads=[g32, hb], writes=[g])
            if tc + 1 < 8:
                load_gates(tc + 1)
            cbs = [0] if tc < 4 else [0, 1]
            for r in range(4):
                blocks = []
                for cb in cbs:
                    M = 128
                    rel = tc - 4 * cb
                    ops = [(kccT[:, cb * 128:cb * 128 + M], qT[:, r, ts], [kccT, (qT.name, r)]),
                           (ekc[:], nq[r][:], [ekc, nq[r]])]
                    if cb == 1:
                        ops.append((ident[:], masks[:, 13 + rel, :], [ident, masks]))
                    elif rel <= 4:
                        ops.append((ident[:], masks[:, 8 + rel, :], [ident, masks]))
                    bi = r * 16 + cb * 8 + tc
                    blocks.append(dict(M=M, s_ops=ops, bias=btl[0:M, bi:bi + 1], v=vcc[0:M, cb, :],
                                       vkey=(vcc.name, cb), keep=(Pn[0:M, r, cb, :], (Pn.name, r, cb))))
                psO, psD = run_blocks(blocks)
                branch_finish(tc, r, 0, True, psO, psD)
                for cb in cbs:
                    M = 128
                    c.op("dve", "tensor_tensor", Pn[0:M, r, cb, :], Pn[0:M, r, cb, :], rd[0:M, :], ALU.mult,
                         reads=[(Pn.name, r, cb), rd], writes=[(Pn.name, r, cb)])
                c.op("dve", "tensor_copy", cacc[:, r, :], acc[:], reads=[acc], writes=[(cacc.name, r)])
            for tb in range(4):
                gtb = tc * 4 + tb
                n = 0
                tot = 4 * len(cbs)
                for r in range(4):
                    for cb in cbs:
                        M = 128
                        c.op("pe", "matmul", psI[:, tb * 64:(tb + 1) * 64], Pn[0:M, r, cb, tb * 128:(tb + 1) * 128],
                             share[0:M, cb, :], start=(n == 0), stop=(n == tot - 1),
                             reads=[(Pn.name, r, cb), share], writes=[psI])
                        n += 1
            K4 = range(4)
            for tb in K4:
                c.op("dve", "tensor_tensor", scr[:, tb, :], psI[:, tb * 64:(tb + 1) * 64], btab[:, tc * 4 + tb, :],
                     ALU.add, reads=[psI, btab], writes=[(scr.name, tb)])
            for tb in K4:
                c.op("dve", "max", m8[:, tb, 0:8], scr[:, tb, :], reads=[(scr.name, tb)], writes=[(m8.name, tb)])
            for tb in K4:
                c.op("dve", "match_replace", scr2[:, tb, :], m8[:, tb, 0:8], scr[:, tb, :], -1e30,
                     reads=[(scr.name, tb), (m8.name, tb)], writes=[(scr2.name, tb)])
            for tb in K4:
                c.op("dve", "max", m8[:, tb, 8:16], scr2[:, tb, :], reads=[(scr2.name, tb)], writes=[(m8.name, tb)])
            for tb in K4:
                c.op("dve", "tensor_scalar", thr[:, tb:tb + 1], m8[:, tb, 15:16], -1e29, None, ALU.max,
                     reads=[(m8.name, tb)], writes=[(thr.name, tb)])
            for tb in K4:
                c.op("dve", "tensor_scalar", sm1[:, tb, :], scr[:, tb, :], thr[:, tb:tb + 1], 1.0, ALU.is_ge,
                     ALU.subtract, reads=[(scr.name, tb), (thr.name, tb)], writes=[(sm1.name, tb)])

        def mid(tc):
            ts = slice(tc * 512, (tc + 1) * 512)
            cbs = [0] if tc < 4 else [0, 1]
            cacc = caccs[tc % 2]
            for tb in range(4):
                c.op("pe", "transpose", psX[0:64, tb * 128:(tb + 1) * 128], sm1[:, tb, :], identf[:],
                     reads=[(sm1.name, tb), identf], writes=[psX])
            c.op("act", "activation", nq[0][0:64, :], psX[0:64, :], AF.Copy, reads=[psX], writes=[nq[0]])
            for r in range(1, 4):
                c.op("pool", "tensor_copy", nq[r][0:64, :], nq[0][0:64, :], reads=[nq[0]], writes=[nq[r]])

        def sw(tc):
            ts = slice(tc * 512, (tc + 1) * 512)
            cbs = [0] if tc < 4 else [0, 1]
            cacc = caccs[tc % 2]
            for r in range(4):
                for _ in range(2):
                    if bg:
                        d_, s_ = bg.pop(0)
                        c.dma("pool", d_, s_)
                c.op("dve", "tensor_copy", acc[:], cacc[:, r, :], reads=[(cacc.name, r)], writes=[acc])
                blocks = []
                for kb in range(4 * tc + 4):
                    rel = kb - 4 * tc
                    ks = slice(kb * 128, (kb + 1) * 128)
                    ops = [(kT[:, 2, ks], qT[:, r, ts], [(kT.name, 2), (qT.name, r)]),
                           (ek[:, ks], nq[r][:], [ek, nq[r]])]
                    if rel >= 0:
                        ops.append((ident[:], masks[:, rel, :], [ident, masks]))
                    bi = 64 + r * 32 + rel + 28
                    blocks.append(dict(M=128, s_ops=ops, bias=btl[:, bi:bi + 1], v=vs[:, kb, :], vkey=vs))
                psO, psD = run_blocks(blocks)
                branch_finish(tc, r, 1, False, psO, psD)
                blocks = []
                for kb in range(max(0, 4 * tc - 4), 4 * tc + 4):
                    rel = kb - 4 * tc
                    ks = slice(kb * 128, (kb + 1) * 128)
                    mi = rel if rel >= 0 else 4 + (rel + 4)
                    ops = [(kT[:, 3, ks], qT[:, r, ts], [(kT.name, 3), (qT.name, r)]),
                           (ekw[:], nq[r][:], [ekw, nq[r]]),
                           (ident[:], masks[:, mi, :], [ident, masks])]
                    bi = 64 + r * 32 + rel + 28
                    blocks.append(dict(M=128, s_ops=ops, bias=btl[:, bi:bi + 1], v=vw[:, kb, :], vkey=vw))
                psO, psD = run_blocks(blocks)
                branch_finish(tc, r, 2, False, psO, psD)
                o = ost[r % 2]
                c.op("act", "activation", o[:], acc[:], AF.Copy, reads=[acc], writes=[o])
                c.dma("sp", out_d[r * 128:(r + 1) * 128, ts], o[:], reads=[o])

        front(0)
        mid(0)
        for tc in range(8):
            if tc + 1 < 8:
                front(tc + 1)
            sw(tc)
            if tc + 1 < 8:
                mid(tc + 1)
        while bg:
            d_, s_ = bg.pop(0)
            c.dma("pool", d_, s_)
        c.finish()
        if not standalone:
            c.release()
        print("B1: ins", c.n_ins, "waits", c.n_wait)
    return nc


def ret_consts(hf):
    n = np.arange(128, dtype=np.float64)
    dec = np.zeros((2, 128, 128), np.float32)
    zeta = np.zeros((128, 2), np.float32)
    xi = np.zeros((2, 1, 128), np.float32)
    dc = []
    for i in range(2):
        h = 2 * hf + i
        lg = np.log1p(-2.0 ** (-5.0 - h))
        rel = n[None, :] - n[:, None]
        dec[i] = np.where(rel >= 0, np.exp(lg * np.maximum(rel, 0)), 0.0) * SCALE
        zeta[:, i] = np.exp(lg * (127.0 - n)) * SCALE
        xi[i, 0] = np.exp(lg * (n + 1.0))
        dc.append(float(np.exp(lg * 128.0)))
    dct = np.ascontiguousarray(np.broadcast_to(np.asarray(dc, np.float32)[None, :], (128, 2)))
    return dict(d_dec=np.ascontiguousarray(dec.transpose(1, 0, 2)), d_zeta=zeta, d_xi=xi, d_dc=dct)


def build_B2(nc=None, dr=None, pfx="", semstack=None):
    standalone = nc is None
    if standalone:
        nc = bass.Bass("TRN2", target_bir_lowering=False)
        dr = _default_dr(nc)
    dt = dr
    b16 = dt("b16", [2 * HROWS16, TOK], BF16)
    b32 = dt("b32", [2 * HROWS32, TOK], F32)
    gn_d = dt("d_gn", [128, 2], F32)
    dec_d = dt("d_dec", [128, 2, 128], F32)
    zeta_d = dt("d_zeta", [128, 2], F32)
    xi_d = dt("d_xi", [2, 1, 128], F32)
    dc_d = dt("d_dc", [128, 2], F32)
    out_d = dt("bout", [768, T], BF16, "ExternalOutput")
    with contextlib.ExitStack() as st:
        c = Ctx(nc, st, pfx, semstack)
        rq = c.sb("rq", [128, 2, T], BF16)
        rk = c.sb("rk", [128, 2, T], BF16)
        qx = c.sb("qx", [128, 2, T], BF16)
        rktm = c.sb("rktm", [128, 32, 256], BF16)
        rvtm = c.sb("rvtm", [128, 32, 256], BF16)
        kz = c.sb("kz", [128, 32, 256], BF16)
        gn = c.sb("gn", [128, 2], F32)
        dec = c.sb("dec", [128, 2, 128], F32)
        zeta = c.sb("zeta", [128, 2], F32)
        xib = c.sb("xib", [128, 2, 128], F32)
        onesf = c.sb("onesf", [128, 128], F32)
        eps_t = c.sb("eps_t", [128, 1], F32)
        state = [c.sb("state%d" % i, [128, 128], F32) for i in range(2)]
        prevb = [c.sb("prevb%d" % i, [128, 128], BF16) for i in range(2)]
        scb = [c.sb("scb%d" % i, [128, 128], BF16) for i in range(2)]
        ysb = c.sb("ysb", [128, 512], F32)
        ysq = c.sb("ysq", [128, 512], F32)
        m2 = c.sb("m2", [128, 512], F32)
        var = c.sb("var", [128, 512], F32)
        sd = c.sb("sd", [128, 512], F32)
        yc = c.sb("yc", [128, 512], F32)
        rgt = [c.sb("rgt%d" % i, [128, 512], F32) for i in range(2)]
        ost = [c.sb("ost%d" % i, [128, 512], BF16) for i in range(2)]
        psSc = [c.ps("psSc%d" % i, [128, 512]) for i in range(2)]
        psY = [c.ps("psY%d" % i, [128, 512]) for i in range(2)]
        psKVs = [c.ps("psKV%d" % i, [128, 512]) for i in range(2)]
        psM = c.ps("psM", [128, 512])
        psE = c.ps("psE", [128, 512])

        c.op("pool", "memset", onesf[:], 1.0 / 128.0, writes=[onesf])
        c.op("pool", "memset", eps_t[:], EPS, writes=[eps_t])
        for i in range(2):
            for th in range(2):
                tsl = slice(th * TOK, (th + 1) * TOK)
                c.dma("sp", rq[:, i, tsl], b16[th * HROWS16 + 1024 + i * 128:th * HROWS16 + 1024 + (i + 1) * 128, :],
                      writes=[(rq.name, i)])
                c.dma("sp", rk[:, i, tsl], b16[th * HROWS16 + 1280 + i * 128:th * HROWS16 + 1280 + (i + 1) * 128, :],
                      writes=[(rk.name, i)])
            c.dma("sp", xib[:, i, :], bcast_rows(xi_d[i], 128), writes=[(xib.name, i)])
            c.op("pool", "memset", state[i][:], 0.0, writes=[state[i]])
            c.op("pool", "memset", prevb[i][:], 0.0, writes=[prevb[i]])
        for th in range(2):
            toff = (th * HROWS16 + 1536) * TOK
            c.dma("sp", rktm[:, th * 16:(th + 1) * 16, :],
                  bass.AP(b16.tensor, toff + 256, [[768, 128], [768 * 128, 16], [1, 256]]), writes=[rktm])
            c.dma("sp", rvtm[:, th * 16:(th + 1) * 16, :],
                  bass.AP(b16.tensor, toff + 512, [[768, 128], [768 * 128, 16], [1, 256]]), writes=[rvtm])
        c.dma("sp", gn[:], gn_d, writes=[gn])
        c.dma("sp", dec[:], dec_d, writes=[dec])
        c.dma("sp", zeta[:], zeta_d, writes=[zeta])
        dct = c.sb("dct", [128, 2], F32)
        c.dma("sp", dct[:], dc_d, writes=[dct])
        for i in range(2):
            c.op("dve", "tensor_scalar", kz[:, :, i * 128:(i + 1) * 128], rktm[:, :, i * 128:(i + 1) * 128],
                 zeta[:, i:i + 1], None, ALU.mult, reads=[rktm, zeta], writes=[(kz.name, i)])
            xb = xib[:, i, :]
            xbb = bass.AP(xb.tensor, xb.offset, [list(xb.ap[0]), [0, 32], list(xb.ap[-1])])
            c.op("pool", "tensor_tensor", qx[:, i, :].rearrange("p (c n) -> p c n", n=128),
                 rq[:, i, :].rearrange("p (c n) -> p c n", n=128), xbb, ALU.mult,
                 reads=[(rq.name, i), (xib.name, i)], writes=[(qx.name, i)])
        k = 0
        for grp in range(8):
            for i in range(2):
                pY = psY[i]
                for cc in range(4):
                    ch = grp * 4 + cc
                    cs = slice(ch * 128, (ch + 1) * 128)
                    hs = slice(i * 128, (i + 1) * 128)
                    pS = psSc[k % 2]
                    sb_ = scb[k % 2]
                    k += 1
                    c.op("pe", "matmul", pS[:, 0:128], rk[:, i, cs], rq[:, i, cs], start=True, stop=True,
                         reads=[(rk.name, i), (rq.name, i)], writes=[pS])
                    c.op("dve", "tensor_tensor", sb_[:], pS[:, 0:128], dec[:, i, :], ALU.mult,
                         reads=[pS, dec], writes=[sb_])
                    c.op("pe", "matmul", pY[:, cc * 128:(cc + 1) * 128], rvtm[:, ch, hs], sb_[:], start=True,
                         stop=False, reads=[rvtm, sb_], writes=[pY])
                    c.op("pe", "matmul", pY[:, cc * 128:(cc + 1) * 128], prevb[i][:], qx[:, i, cs], start=False,
                         stop=True, reads=[prevb[i], (qx.name, i)], writes=[pY])
                    psKV = psKVs[i]
                    c.op("pe", "matmul", psKV[:, 0:128], kz[:, ch, hs], rvtm[:, ch, hs],
                         start=True, stop=True, reads=[(kz.name, i), rvtm], writes=[psKV])
                    c.op("dve", "scalar_tensor_tensor", state[i][:], state[i][:], dct[:, i:i + 1],
                         psKV[:, 0:128], ALU.mult, ALU.add,
                         reads=[state[i], psKV, dct], writes=[state[i]])
                    c.op("act", "activation", prevb[i][:], state[i][:], AF.Copy, reads=[state[i]],
                         writes=[prevb[i]])
                ts = slice(grp * 512, (grp + 1) * 512)
                rg_t = rgt[i]
                rr0 = (grp // 4) * HROWS32 + i * 128
                c.dma("sp", rg_t[:], b32[rr0:rr0 + 128, (grp % 4) * 512:(grp % 4 + 1) * 512], writes=[rg_t])
                c.op("act", "activation", ysb[:], pY[:], AF.Copy, reads=[pY], writes=[ysb])
                c.op("act", "activation", ysq[:], pY[:], AF.Square, reads=[pY], writes=[ysq])
                c.op("pe", "matmul", psM[:], onesf[:], ysb[:], start=True, stop=True, reads=[onesf, ysb],
                     writes=[psM])
                c.op("pe", "matmul", psE[:], onesf[:], ysq[:], start=True, stop=True, reads=[onesf, ysq],
                     writes=[psE])
                c.op("act", "activation", m2[:], psM[:], AF.Square, reads=[psM], writes=[m2])
                c.op("dve", "tensor_tensor", var[:], psE[:], m2[:], ALU.subtract, reads=[psE, m2], writes=[var])
                c.op("act", "activation", sd[:], var[:], AF.Sqrt, bias=eps_t[:, 0:1], scale=1.0,
                     reads=[var, eps_t], writes=[sd])
                c.op("dve", "reciprocal", sd[:], sd[:], reads=[sd], writes=[sd])
                c.op("dve", "tensor_tensor", yc[:], ysb[:], psM[:], ALU.subtract, reads=[ysb, psM], writes=[yc])
                c.op("pool", "tensor_tensor", yc[:], yc[:], sd[:], ALU.mult, reads=[yc, sd], writes=[yc])
                o = ost[i]
                c.op("dve", "scalar_tensor_tensor", o[:], yc[:], gn[:, i:i + 1], rg_t[:], ALU.mult, ALU.mult,
                     reads=[yc, gn, rg_t], writes=[o])
                c.dma("sp", out_d[512 + i * 128:512 + (i + 1) * 128, ts], o[:], reads=[o])
        c.finish()
        if not standalone:
            c.release()
        print("B2: ins", c.n_ins, "waits", c.n_wait)
    return nc


FFE = 2816
NFB = 22


def build_C(n_exp, moe, final, nc=None, dr=None, pfx="", semstack=None, wbf=None):
    standalone = nc is None
    if standalone:
        nc = bass.Bass("TRN2", target_bir_lowering=False)
        dr = _default_dr(nc)
    dt = dr
    hT_d = dt("d_hT", [D, TOK], F32)
    cin = dt("cin", [2 * 768, TOK], BF16)
    ogm_d = dt("ogm", [512, TOK], BF16)
    wo_d = dt("d_wo", [D, D], F32)
    g2_d = dt("d_g2", [128, KC], F32)
    w1_d = dt("d_w1", [n_exp, D, FFE], F32)
    w3_d = dt("d_w3", [n_exp, D, FFE], F32)
    w2_d = dt("d_w2", [n_exp, FFE, D], F32)
    if moe:
        wr_d = dt("d_wr", [128, KC, 8], F32)
        br_d = dt("d_br", [8, 1], F32)
        identf_d = dt("d_identf", [128, 128], F32)
        sel8_d = dt("d_sel8", [8, 8 * 128], F32)
    if final:
        gf_d = dt("d_gf", [128, KC], F32)
    out_d = dt("oh", [D, TOK], F32, "ExternalOutput")

    with contextlib.ExitStack() as st:
        c = Ctx(nc, st, pfx, semstack)
        h1t = c.sb("h1t", [128, KC, 512], F32)
        mx = c.sb("mx", [128, KC, 512], BF16)
        fT = c.sb("fT", [128, KC, 512], BF16)
        gT = c.sb("gT", [128, NFB, 512], BF16)
        NWB = 2
        wA = [c.sb("wA%d" % i, [128, KC, 512], BF16) for i in range(NWB)]
        wB = [c.sb("wB%d" % i, [128, NFB, 256], BF16) for i in range(NWB)]
        ones_bf = c.sb("ones_bf", [128, 128], BF16)
        eps_t = c.sb("eps_t", [128, 1], F32)
        g_col = c.sb("g_col", [128, KC], F32)
        rt = c.sb("rt", [128, 512], F32)
        rstd = c.sb("rstd", [128, 512], F32)
        st1 = [c.sb("st1_%d" % i, [128, 512], F32) for i in range(2)]
        st2 = [c.sb("st2_%d" % i, [128, 512], F32) for i in range(2)]
        ps1 = [c.ps("ps1_%d" % i, [128, 512]) for i in range(2)]
        ps3 = [c.ps("ps3_%d" % i, [128, 512]) for i in range(2)]
        pso = [c.ps("pso_%d" % i, [128, 512]) for i in range(2)]
        ps_stat = c.ps("ps_stat", [128, 512])
        psr = c.ps("psr", [128, 512])
        if moe:
            wr = c.sb("wr", [128, KC, 8], F32)
            wrg = c.sb("wrg", [128, KC, 8], F32)
            br = c.sb("br", [8, 1], F32)
            identf = c.sb("identf", [128, 128], F32)
            sel8 = c.sb("sel8", [8, 8 * 128], F32)
            lgT = c.sb("lgT", [8, 512], F32)
            lg = c.sb("lg", [128, 4, 8], F32)
            m8 = c.sb("m8", [128, 4, 8], F32)
            dlt = c.sb("dlt", [128, 4], F32)
            gg1 = c.sb("gg1", [128, 4], F32)
            gg2 = c.sb("gg2", [128, 4], F32)
            cm1 = c.sb("cm1", [128, 4, 8], F32)
            cm2 = c.sb("cm2", [128, 4, 8], F32)
            combT = c.sb("combT", [8, 512], F32)
            cbc = c.sb("cbc", [128, 8, 512], F32)
        if final:
            gf_col = c.sb("gf_col", [128, KC], F32)

        c.op("pool", "memset", ones_bf[:], 1.0, writes=[ones_bf])
        c.op("pool", "memset", eps_t[:], EPS, writes=[eps_t])
        c.dma("sp", g_col[:], g2_d, writes=[g_col])
        if final:
            c.dma("sp", gf_col[:], gf_d, writes=[gf_col])
        if moe:
            c.dma("sp", wr[:], wr_d, writes=[wr])
            c.dma("sp", br[:], br_d, writes=[br])
            c.dma("sp", identf[:], identf_d, writes=[identf])
            c.dma("sp", sel8[:], sel8_d, writes=[sel8])
            for kc in range(KC):
                c.op("dve", "tensor_scalar", wrg[:, kc, :], wr[:, kc, :], g_col[:, kc:kc + 1], None, ALU.mult,
                     reads=[wr, g_col], writes=[wrg])

        hv = hT_d.rearrange("(kc p) t -> p kc t", p=128)
        ov = out_d.rearrange("(kc p) t -> p kc t", p=128)
        wov = wo_d.rearrange("(kc p) n -> p kc n", p=128)
        wa_i = [0]
        wb_i = [0]
        pp = [0]

        def rms(src, gcol, dst_fn):
            c.op("act", "activation", mx[:], src[:], AF.Square,
                 reads=[(src.name, k) for k in range(KC)], writes=[mx])
            for kc in range(KC):
                c.op("pe", "matmul", ps_stat[:], ones_bf[:], mx[:, kc, :], start=(kc == 0), stop=(kc == KC - 1),
                     reads=[mx, ones_bf], writes=[ps_stat])
            c.op("act", "activation", rt[:], ps_stat[:], AF.Sqrt, bias=eps_t[:, 0:1], scale=1.0 / D,
                 reads=[ps_stat, eps_t], writes=[rt])
            c.op("dve", "reciprocal", rstd[:], rt[:], reads=[rt], writes=[rstd])
            for kc in range(KC):
                o, wk = dst_fn(kc)
                c.op("dve", "scalar_tensor_tensor", o, src[:, kc, :], gcol[:, kc:kc + 1], rstd[:], ALU.mult,
                     ALU.mult, reads=[(src.name, kc), gcol, rstd], writes=[wk])

        for tt in range(4):
            ts = slice(tt * 512, (tt + 1) * 512)
            for k4 in range(4):
                c.dma("sp", h1t[:, k4 * 4:(k4 + 1) * 4, :], hv[:, k4 * 4:(k4 + 1) * 4, ts],
                      writes=[(h1t.name, k) for k in range(k4 * 4, k4 * 4 + 4)])
            for k0, nk, src in ((0, 4, cin[0:512, :]), (4, 4, cin[768:1280, :]), (8, 4, ogm_d),
                                (12, 2, cin[512:768, :]), (14, 2, cin[1280:1536, :])):
                c.dma("sp", mx[:, k0:k0 + nk, :], src.rearrange("(kc p) t -> p kc t", p=128)[:, :, ts], writes=[mx])
            for og in range(4):
                w = wA[wa_i[0] % NWB]
                wa_i[0] += 1
                if wbf is not None:
                    c.dma("sp", w[:], wbf["wob"].ap()[og], writes=[(w.name, k) for k in range(4)])
                else:
                    for k4 in range(4):
                        c.dma("pool", w[:, k4 * 4:(k4 + 1) * 4, :],
                              wov[:, k4 * 4:(k4 + 1) * 4, og * 512:(og + 1) * 512], writes=[(w.name, k4)])
                for blk in range(4):
                    dmb = og * 4 + blk
                    p = pso[pp[0] % 2]
                    pp[0] += 1
                    for kc in range(KC):
                        c.op("pe", "matmul", p[:], w[:, kc, blk * 128:(blk + 1) * 128], mx[:, kc, :],
                             start=(kc == 0), stop=(kc == KC - 1), reads=[(w.name, kc // 4), mx], writes=[p])
                    c.op("dve", "tensor_tensor", h1t[:, dmb, :], p[:], h1t[:, dmb, :], ALU.add,
                         reads=[p, (h1t.name, dmb)], writes=[(h1t.name, dmb)])
            rms(h1t, g_col, lambda kc: (fT[:, kc, :], (fT.name, kc)))
            fkeys = [(fT.name, k) for k in range(KC)]
            if moe:
                for kc in range(KC):
                    c.op("pe", "matmul", psr[0:8, :], wrg[:, kc, :], h1t[:, kc, :], start=(kc == 0),
                         stop=(kc == KC - 1), reads=[wrg, (h1t.name, kc)], writes=[psr])
                c.op("dve", "tensor_tensor", lgT[:], psr[0:8, :], rstd[0:8, :], ALU.mult, reads=[psr, rstd],
                     writes=[lgT])
                c.op("dve", "tensor_scalar", lgT[:], lgT[:], br[:, 0:1], None, ALU.add, reads=[lgT, br],
                     writes=[lgT])
                for tb in range(4):
                    c.op("pe", "transpose", psr[:, 64 + tb * 8:64 + (tb + 1) * 8], lgT[:, tb * 128:(tb + 1) * 128],
                         identf[0:8, 0:8], reads=[lgT, identf], writes=[psr])
                c.op("dve", "tensor_copy", lg[:], psr[:, 64:96].rearrange("p (a e) -> p a e", e=8),
                     reads=[psr], writes=[lg])
                for tb in range(4):
                    c.op("dve", "max", m8[:, tb, :], lg[:, tb, :], reads=[lg], writes=[m8])
                c.op("dve", "tensor_tensor", dlt[:], m8[:, :, 1], m8[:, :, 0], ALU.subtract, reads=[m8], writes=[dlt])
                c.op("act", "activation", dlt[:], dlt[:], AF.Exp, reads=[dlt], writes=[dlt])
                c.op("dve", "tensor_scalar", gg1[:], dlt[:], 1.0, None, ALU.add, reads=[dlt], writes=[gg1])
                c.op("dve", "reciprocal", gg1[:], gg1[:], reads=[gg1], writes=[gg1])
                c.op("dve", "tensor_scalar", gg2[:], gg1[:], -1.0, 1.0, ALU.mult, ALU.add, reads=[gg1], writes=[gg2])
                for tb in range(4):
                    c.op("dve", "tensor_scalar", cm1[:, tb, :], lg[:, tb, :], m8[:, tb, 0:1], gg1[:, tb:tb + 1],
                         ALU.is_ge, ALU.mult, reads=[lg, m8, gg1], writes=[cm1])
                    c.op("dve", "tensor_scalar", cm2[:, tb, :], lg[:, tb, :], m8[:, tb, 1:2], gg2[:, tb:tb + 1],
                         ALU.is_equal, ALU.mult, reads=[lg, m8, gg2], writes=[cm2])
                c.op("dve", "tensor_tensor", cm1[:], cm1[:], cm2[:], ALU.add, reads=[cm1, cm2], writes=[cm1])
                for tb in range(4):
                    c.op("pe", "transpose", psr[0:8, tb * 128:(tb + 1) * 128], cm1[:, tb, :], identf[:],
                         reads=[cm1, identf], writes=[psr])
                c.op("dve", "tensor_copy", combT[:], psr[0:8, :], reads=[psr], writes=[combT])
                for e in range(8):
                    c.op("pe", "matmul", psr[:], sel8[:, e * 128:(e + 1) * 128], combT[:], start=True, stop=True,
                         reads=[sel8, combT], writes=[psr])
                    c.op("act", "activation", cbc[:, e, :], psr[:], AF.Copy, reads=[psr], writes=[(cbc.name, e)])
            for e in range(n_exp):
                w1v = w1_d[e].rearrange("(kc p) n -> p kc n", p=128)
                w3v = w3_d[e].rearrange("(kc p) n -> p kc n", p=128)
                w2v = w2_d[e].rearrange("(ch p) n -> p ch n", p=128)
                for j in range(NFB // 2):
                    w = wA[wa_i[0] % NWB]
                    wa_i[0] += 1
                    cs = slice(j * 256, (j + 1) * 256)
                    if wbf is not None:
                        c.dma("sp", w[:], wbf["f13b"].ap()[e, j], writes=[(w.name, k) for k in range(4)])
                    else:
                        for k4 in range(4):
                            c.dma("pool", w[:, k4 * 4:(k4 + 1) * 4, 0:256], w1v[:, k4 * 4:(k4 + 1) * 4, cs],
                                  writes=[(w.name, k4)])
                        for k4 in range(4):
                            c.dma("pool", w[:, k4 * 4:(k4 + 1) * 4, 256:512], w3v[:, k4 * 4:(k4 + 1) * 4, cs],
                                  writes=[(w.name, k4)])
                    for bb in range(2):
                        ffb = j * 2 + bb
                        i2 = pp[0] % 2
                        pp[0] += 1
                        p1, p3 = ps1[i2], ps3[i2]
                        for kc in range(KC):
                            c.op("pe", "matmul", p1[:], w[:, kc, bb * 128:(bb + 1) * 128], fT[:, kc, :],
                                 start=(kc == 0), stop=(kc == KC - 1), reads=[(w.name, kc // 4), (fT.name, kc)],
                                 writes=[p1])
                        for kc in range(KC):
                            c.op("pe", "matmul", p3[:], w[:, kc, 256 + bb * 128:256 + (bb + 1) * 128], fT[:, kc, :],
                                 start=(kc == 0), stop=(kc == KC - 1), reads=[(w.name, kc // 4), (fT.name, kc)],
                                 writes=[p3])
                        s1 = st1[i2]
                        c.op("act", "activation", s1[:], p1[:], AF.Silu, reads=[p1], writes=[s1])
                        if moe:
                            s2 = st2[i2]
                            c.op("dve", "tensor_tensor", s2[:], s1[:], p3[:], ALU.mult, reads=[s1, p3], writes=[s2])
                            c.op("dve", "tensor_tensor", gT[:, ffb, :], s2[:], cbc[:, e, :], ALU.mult,
                                 reads=[s2, (cbc.name, e)], writes=[(gT.name, ffb)])
                        else:
                            c.op("dve", "tensor_tensor", gT[:, ffb, :], s1[:], p3[:], ALU.mult, reads=[s1, p3],
                                 writes=[(gT.name, ffb)])
                gkeys = [(gT.name, k) for k in range(NFB)]
                for dg in range(8):
                    w = wB[wb_i[0] % NWB]
                    wb_i[0] += 1
                    if wbf is not None:
                        c.dma("sp", w[:], wbf["f2b"].ap()[e, dg], writes=[(w.name, 0), (w.name, 1)])
                    else:
                        for c2 in range(2):
                            c.dma("pool", w[:, c2 * 11:(c2 + 1) * 11, :],
                                  w2v[:, c2 * 11:(c2 + 1) * 11, dg * 256:(dg + 1) * 256], writes=[(w.name, c2)])
                    for bb in range(2):
                        dmb = dg * 2 + bb
                        p = pso[pp[0] % 2]
                        pp[0] += 1
                        for ch in range(NFB):
                            c.op("pe", "matmul", p[:], w[:, ch, bb * 128:(bb + 1) * 128], gT[:, ch, :],
                                 start=(ch == 0), stop=(ch == NFB - 1), reads=[(w.name, ch // 11), (gT.name, ch)],
                                 writes=[p])
                        c.op("dve", "tensor_tensor", h1t[:, dmb, :], p[:], h1t[:, dmb, :], ALU.add,
                             reads=[p, (h1t.name, dmb)], writes=[(h1t.name, dmb)])
            if final:
                rms(h1t, gf_col, lambda kc: (h1t[:, kc, :], (h1t.name, kc)))
            for k4 in range(4):
                c.dma("sp", ov[:, k4 * 4:(k4 + 1) * 4, ts], h1t[:, k4 * 4:(k4 + 1) * 4, :],
                      reads=[(h1t.name, k) for k in range(k4 * 4, k4 * 4 + 4)])
        c.finish()
        if not standalone:
            c.release()
        print("C: ins", c.n_ins, "waits", c.n_wait)
    return nc


I32 = mybir.dt.int32
PAIRS = [[0, 1], [2, 3], [4, 5], [6, 7]]


def build_fused():
    nc = bass.Bass("TRN2", target_bir_lowering=False)
    ext = lambda n, sh, d: nc.dram_tensor(n, sh, d, kind="ExternalInput")
    loc = lambda n, sh, d: nc.dram_tensor(n, sh, d)
    shr = lambda n, sh, d: nc.dram_tensor(n, sh, d, addr_space="Shared")
    E = {}

    def e(n, sh, d=F32):
        E[n] = ext(n, sh, d)
        return E[n]

    e("hT0", [D, TOK]); e("half", [1, 1], I32); e("tril", [128, 128])
    for L in range(2):
        e("g1_%d" % L, [128, KC]); e("wall_%d" % L, [D, (N_FM + N_TM) * 512]); e("wgate_%d" % L, [D, 24])
        e("glng_%d" % L, [1, 512]); e("glnb_%d" % L, [1, 512]); e("wsT_%d" % L, [128, 4, 128]); e("bsf_%d" % L, [1, 512])
        e("cw1_%d" % L, [2, 128, 32, 128]); e("cpe_%d" % L, [2, 128, 32]); e("cw2_%d" % L, [2, 128, 128])
        e("gn_%d" % L, [128, 2]); e("wo_%d" % L, [D, D]); e("g2_%d" % L, [128, KC])
    e("d_masks", [128, 17, 512]); e("d_ekc", [128, 128]); e("d_ekw", [128, 128]); e("posq", [4, 3, 512])
    e("d_ek", [128, 4096]); e("d_share", [128, 2, 64]); e("d_btab", [128, 32, 64]); e("d_selg", [128, 12 * 128])
    e("d_ident", [128, 128]); e("d_btl", [128, 192])
    e("d_dec", [128, 2, 128]); e("d_zeta", [128, 2]); e("d_xi", [2, 1, 128]); e("d_dc", [128, 2])
    e("fw1", [2, D, FFE]); e("fw3", [2, D, FFE]); e("fw2", [2, FFE, D])
    e("mw1", [8, D, FFE]); e("mw3", [8, D, FFE]); e("mw2", [8, FFE, D])
    e("d_wr", [128, KC, 8]); e("d_br", [8, 1]); e("d_sel8", [8, 8 * 128]); e("d_gf", [128, KC])
    out = nc.dram_tensor("oh", [D, TOK], F32, kind="ExternalOutput")

    Lc = dict(a16=loc("a16", [2 * HROWS16, TOK], BF16), a32=loc("a32", [2 * HROWS32, TOK], F32),
              ogm=loc("ogm", [512, TOK], BF16),
              b16=loc("b16", [2 * HROWS16, TOK], BF16), b32=loc("b32", [2 * HROWS32, TOK], F32),
              bout=loc("bout", [768, T], BF16), cin=loc("cin", [2 * 768, TOK], BF16),
              oh0=loc("oh0", [D, TOK], F32), bi=loc("bi", [1, 64], F32), bo=loc("bo", [2, 64], F32))
    H16 = HROWS16 * TOK
    H32 = HROWS32 * TOK
    XA16 = shr("XA16", [2 * 2 * HROWS16, TOK], BF16)
    XA32 = shr("XA32", [2 * 2 * HROWS32, TOK], F32)
    XB = shr("XB", [2 * 768, T], BF16)

    WB = dict(f13b=loc("f13b", [2, NFB // 2, 128, KC, 512], BF16), f2b=loc("f2b", [2, 8, 128, NFB, 256], BF16),
              wob=loc("wob", [4, 128, KC, 512], BF16))

    def convert_dense():
        out_ = []
        for og in range(4):
            out_.append((WB["wob"].ap()[og], bass.AP(E["wo_0"], og * 512, [[D, 128], [128 * D, KC], [1, 512]])))
        for e_ in range(2):
            for j in range(NFB // 2):
                for wi, wn in enumerate(("fw1", "fw3")):
                    dst = WB["f13b"].ap()[e_, j][:, :, wi * 256:(wi + 1) * 256]
                    out_.append((dst, bass.AP(E[wn], e_ * D * FFE + j * 256, [[FFE, 128], [128 * FFE, KC], [1, 256]])))
            for dg in range(8):
                out_.append((WB["f2b"].ap()[e_, dg],
                             bass.AP(E["fw2"], e_ * FFE * D + dg * 256, [[D, 128], [128 * D, NFB], [1, 256]])))
        return out_

    def mk_dr(mapping):
        def dr(n, sh, d, k="ExternalInput"):
            t = mapping[n]
            assert list(t.shape) == list(sh), (n, list(t.shape), sh)
            return t.ap()
        return dr

    with contextlib.ExitStack() as top:
        g = nc.gpsimd
        cc = top.enter_context(nc.semaphore("cc"))
        r_half = top.enter_context(g.register("r_half"))
        r_tmp = top.enter_context(g.register("r_tmp"))
        halft = top.enter_context(nc.sbuf_tensor("halft", [1, 1], I32))
        hsem = top.enter_context(nc.semaphore("hsem"))
        g.dma_start(out=halft[:], in_=E["half"].ap()).then_inc(hsem, 16)
        g.wait_ge(hsem, 16)
        g.reg_load(r_half, halft[0:1, 0:1])
        ccn = [0]

        def dyn(t, base, stride, pat):
            g.reg_mul(r_tmp, r_half, stride)
            g.reg_add(r_tmp, r_tmp, base)
            return bass.AP(t, r_tmp, pat)

        def stat(t, base, pat):
            return bass.AP(t, base, pat)

        def barrier(xc):
            xc.finish()
            g.collective_compute("AllGather", ALU.bypass, replica_groups=PAIRS, ins=[Lc["bi"].ap().opt()],
                                 outs=[Lc["bo"].ap().opt()]).then_inc(cc, 1)
            ccn[0] += 1
            g.wait_ge(cc, ccn[0])

        def xchg_A(pfx):
            with contextlib.ExitStack() as st:
                xc = Ctx(nc, st, pfx, top)
                CH = 32768
                xc.dma("pool", dyn(XA16, 0, 2 * H16, [[CH, 2 * H16 // CH], [1, CH]]),
                       stat(Lc["a16"], 0, [[CH, 2 * H16 // CH], [1, CH]]))
                xc.dma("pool", dyn(XA32, 0, 2 * H32, [[TOK, 2 * HROWS32], [1, TOK]]),
                       stat(Lc["a32"], 0, [[TOK, 2 * HROWS32], [1, TOK]]))
                barrier(xc)
                for th in range(2):
                    xc.dma("pool", stat(Lc["b16"], th * H16, [[CH, H16 // CH], [1, CH]]),
                           dyn(XA16, th * 2 * H16, H16, [[CH, H16 // CH], [1, CH]]))
                    xc.dma("pool", stat(Lc["b32"], th * H32, [[TOK, HROWS32], [1, TOK]]),
                           dyn(XA32, th * 2 * H32, H32, [[TOK, HROWS32], [1, TOK]]))
                xc.finish()
                xc.release()

        def xchg_B(pfx):
            with contextlib.ExitStack() as st:
                xc = Ctx(nc, st, pfx, top)
                CH = 32768
                n = 768 * T
                xc.dma("pool", dyn(XB, 0, n, [[CH, n // CH], [1, CH]]), stat(Lc["bout"], 0, [[CH, n // CH], [1, CH]]))
                barrier(xc)
                for hh in range(2):
                    xc.dma("pool", stat(Lc["cin"], hh * 768 * TOK, [[TOK, 768], [1, TOK]]),
                           dyn(XB, hh * n, TOK, [[T, 768], [1, TOK]]))
                xc.finish()
                xc.release()

        for L in range(2):
            hsrc = E["hT0"] if L == 0 else Lc["oh0"]
            mA = dict(hT=hsrc, g1=E["g1_%d" % L], wall=E["wall_%d" % L], wgate=E["wgate_%d" % L],
                      glng=E["glng_%d" % L], glnb=E["glnb_%d" % L], wsT=E["wsT_%d" % L], bsf=E["bsf_%d" % L],
                      tril=E["tril"], a16=Lc["a16"], a32=Lc["a32"], ogm=Lc["ogm"])
            build_A(nc, mk_dr(mA), "A%d_" % L, top)
            xchg_A("XA%d_" % L)
            mB1 = dict(E)
            mB1.update(b16=Lc["b16"], b32=Lc["b32"], d_w1=E["cw1_%d" % L], d_peT=E["cpe_%d" % L],
                       d_w2=E["cw2_%d" % L], bout=Lc["bout"])
            build_B1(nc, mk_dr(mB1), "B1%d_" % L, top, pre_hook=convert_dense if L == 0 else None)
            mB2 = dict(E)
            mB2.update(b16=Lc["b16"], b32=Lc["b32"], d_gn=E["gn_%d" % L], bout=Lc["bout"])
            build_B2(nc, mk_dr(mB2), "B2%d_" % L, top)
            xchg_B("XB%d_" % L)
            moe = (L == 1)
            mC = dict(E)
            mC.update(d_hT=hsrc, cin=Lc["cin"], ogm=Lc["ogm"], d_wo=E["wo_%d" % L], d_g2=E["g2_%d" % L],
                      d_w1=E["mw1"] if moe else E["fw1"], d_w3=E["mw3"] if moe else E["fw3"],
                      d_w2=E["mw2"] if moe else E["fw2"], d_identf=E["d_ident"], oh=out if moe else Lc["oh0"])
            build_C(8 if moe else 2, moe, moe, nc, mk_dr(mC), "C%d_" % L, top, wbf=None if moe else WB)
    return nc


_PROGS = {}


def _col(g):
    return np.ascontiguousarray(np.asarray(g, np.float32).reshape(KC, 128).T)


def kernel(x, ln1_g, w_in, cmp_pe_k, cmp_w1_k, cmp_w2_k, cmp_pe_v, cmp_w1_v, cmp_w2_v,
           gmlp_ln_g, gmlp_ln_b, gmlp_ws, gmlp_bs, ret_gn_g, w_out, ln2_g,
           ffn_w1, ffn_w3, ffn_w2, moe_wr, moe_br, moe_w1, moe_w3, moe_w2, final_g):
    f32 = lambda a: np.asarray(a, np.float32)
    xf = f32(x).reshape(B * T, D)
    if "F" not in _PROGS:
        _PROGS["F"] = build_fused()
    nc = _PROGS["F"]
    sh = {"d_" + k: v for k, v in nsa_consts().items()}
    sh["tril"] = np.triu(np.ones((128, 128), np.float32))
    for L in range(2):
        wall, wgate = pack_A_weights(f32(w_in[L]))
        sh["g1_%d" % L] = _col(ln1_g[L]); sh["wall_%d" % L] = wall; sh["wgate_%d" % L] = wgate
        sh["glng_%d" % L] = f32(gmlp_ln_g[L]).reshape(1, 512); sh["glnb_%d" % L] = f32(gmlp_ln_b[L]).reshape(1, 512)
        sh["wsT_%d" % L] = np.ascontiguousarray(f32(gmlp_ws[L]).transpose(2, 0, 1))
        sh["bsf_%d" % L] = f32(gmlp_bs[L]).reshape(1, 512)
        sh["cw1_%d" % L] = np.ascontiguousarray(np.stack(
            [f32(cmp_w1_k[L]).reshape(32, 128, 128).transpose(1, 0, 2),
             f32(cmp_w1_v[L]).reshape(32, 128, 128).transpose(1, 0, 2)]))
        sh["cpe_%d" % L] = np.ascontiguousarray(np.stack([f32(cmp_pe_k[L]).T, f32(cmp_pe_v[L]).T]))
        sh["cw2_%d" % L] = np.ascontiguousarray(np.stack([f32(cmp_w2_k[L]), f32(cmp_w2_v[L])]))
        sh["wo_%d" % L] = f32(w_out[L]); sh["g2_%d" % L] = _col(ln2_g[L])
    sh["fw1"] = np.ascontiguousarray(f32(ffn_w1[0]).reshape(D, 2, FFE).transpose(1, 0, 2))
    sh["fw3"] = np.ascontiguousarray(f32(ffn_w3[0]).reshape(D, 2, FFE).transpose(1, 0, 2))
    sh["fw2"] = np.ascontiguousarray(f32(ffn_w2[0]).reshape(2, FFE, D))
    sh["mw1"] = f32(moe_w1[0]); sh["mw3"] = f32(moe_w3[0]); sh["mw2"] = f32(moe_w2[0])
    sh["d_wr"] = np.ascontiguousarray(f32(moe_wr[0]).reshape(KC, 128, 8).transpose(1, 0, 2))
    sh["d_br"] = f32(moe_br[0]).reshape(8, 1)
    sh["d_sel8"] = np.repeat(np.eye(8, dtype=np.float32), 128, axis=1)
    sh["d_gf"] = _col(final_g)
    maps = []
    for c in range(NCORES):
        hf = c % 2
        m = dict(sh)
        m["hT0"] = np.ascontiguousarray(xf[c * TOK:(c + 1) * TOK].T)
        m["half"] = np.array([[hf]], np.int32)
        m["posq"] = nsa_posq(hf)
        m["d_btl"] = nsa_bias_table(hf)
        m.update(ret_consts(hf))
        for L in range(2):
            m["gn_%d" % L] = np.ascontiguousarray(f32(ret_gn_g[L])[hf * 256:(hf + 1) * 256].reshape(2, 128).T)
        maps.append(m)
    res = run_bass_kernel_spmd(nc, maps, core_ids=list(range(NCORES))).results
    out = np.concatenate([np.asarray(res[c]["oh"], np.float32).T for c in range(NCORES)], axis=0)
    return np.ascontiguousarray(out.reshape(B, T, D))
```

```python
import contextlib
import numpy as np
import ml_dtypes
import concourse.bass as bass
import concourse.mybir as mybir
from concourse.bass_utils import run_bass_kernel_spmd

F32 = mybir.dt.float32
BF16 = mybir.dt.bfloat16
AF = mybir.ActivationFunctionType
ALU = mybir.AluOpType
AX = mybir.AxisListType
NPBF = ml_dtypes.bfloat16

NCORES = 8
D = 2048
KC = 16
B, T = 4, 4096
TOK = 2048
EPS = 1e-6
NRING = 4
SCALE = 128.0 ** -0.5


class Ctx:
    def __init__(self, nc, stack, pfx="", semstack=None):
        self.nc = nc
        self.stack = stack
        self.pfx = pfx
        self.fused = semstack is not None
        self.all_sems = []

        def mksem(name):
            if self.fused:
                h = nc.alloc_semaphore(name=name)
                self.all_sems.append(h)
                return h
            return stack.enter_context(nc.semaphore(name))
        self.E = {"pe": nc.tensor, "act": nc.scalar, "dve": nc.vector,
                  "pool": nc.gpsimd, "sp": nc.sync}
        self.csem = {}
        self.ccnt = {}
        for e in ("pe", "act", "dve", "pool"):
            self.csem[e] = mksem(pfx + "c_" + e)
            self.ccnt[e] = 0
        self.dsem = {}
        self.dcnt = {}
        for q in ("sp", "pool"):
            self.dsem[q] = [mksem(pfx + "d_%s%d" % (q, i)) for i in range(NRING)]
            self.dcnt[q] = 0
        self.waited = {e: {} for e in self.E}
        self.writer = {}
        self.readers = {}
        self.n_wait = 0
        self.n_ins = 0
        self.psum_names = set()
        self.psum_last = {}

    def sb(self, name, shape, dt):
        return self.stack.enter_context(self.nc.sbuf_tensor(self.pfx + name, shape, dt))

    def ps(self, name, shape, dt=F32):
        t = self.stack.enter_context(self.nc.psum_tensor(self.pfx + name, shape, dt))
        self.psum_names.add(t.name)
        return t

    @staticmethod
    def key(x):
        if isinstance(x, (str, tuple)):
            return x
        t = getattr(x, "tensor", None)
        if t is not None:
            return t.name
        return x.name

    def _need(self, e, ev, skip_same=None):
        if ev is None:
            return
        sem, val, src = ev
        if src == skip_same:
            return
        w = self.waited[e]
        k = id(sem)
        if w.get(k, 0) >= val:
            return
        self.E[e].wait_ge(sem, val)
        self.n_wait += 1
        w[k] = val

    def _base(self, b):
        k = self.key(b)
        return k[0] if isinstance(k, tuple) else k

    def _sync(self, e, reads, writes, same_ok=False):
        skip = e if same_ok else None
        for b in list(reads) + list(writes):
            base = self._base(b)
            if base in self.psum_names:
                for e2, ev in self.psum_last.get(base, {}).items():
                    if e2 != e:
                        self._need(e, ev, skip)
        for b in reads:
            self._need(e, self.writer.get(self.key(b)), skip)
        for b in writes:
            k = self.key(b)
            self._need(e, self.writer.get(k), skip)
            for ev in self.readers.get(k, {}).values():
                self._need(e, ev, skip)

    def _record(self, ev, reads, writes):
        for b in list(reads) + list(writes):
            base = self._base(b)
            if base in self.psum_names:
                self.psum_last.setdefault(base, {})[ev[2]] = ev
        for b in reads:
            self.readers.setdefault(self.key(b), {})[id(ev[0])] = ev
        for b in writes:
            k = self.key(b)
            self.writer[k] = ev
            self.readers[k] = {}

    def op(self, e, name, *args, reads=(), writes=(), **kw):
        self._sync(e, reads, writes, same_ok=(e == "pe"))
        ins = getattr(self.E[e], name)(*args, **kw)
        self.ccnt[e] += 1
        ins.then_inc(self.csem[e], 1)
        ev = (self.csem[e], self.ccnt[e], e)
        self._record(ev, reads, writes)
        self.n_ins += 1
        return ins

    def dma(self, q, out, in_, reads=(), writes=(), **kw):
        self._sync(q, reads, writes)
        i = self.dcnt[q]
        self.dcnt[q] += 1
        sem = self.dsem[q][i % NRING]
        val = 16 * (i // NRING + 1)
        self.E[q].dma_start(out=out, in_=in_, **kw).then_inc(sem, 16)
        ev = (sem, val, "dma_" + q)
        self._record(ev, reads, writes)
        self.n_ins += 1
        return ev

    def release(self):
        self.nc.all_engine_barrier()
        self.nc.clear_and_free_semaphores(self.all_sems)
        self.nc.all_engine_barrier()

    def finish(self):
        for q in ("sp", "pool"):
            n = self.dcnt[q]
            for s in range(NRING):
                cnt = (n - s + NRING - 1) // NRING if n > s else 0
                if cnt > 0:
                    self.E[q].wait_ge(self.dsem[q][s], 16 * cnt)


def bcast_rows(ap, nparts):
    return bass.AP(ap.tensor, ap.offset, [[0, nparts]] + [list(x) for x in ap.ap[1:]])


def rmsnorm_tiles(c, hT_dram, g_col, aT, ones_bf, eps_t, ht, sq, ps_stat, rt, rstd, ntt, q="sp"):
    hv = hT_dram.rearrange("(kc p) t -> p kc t", p=128)
    for tt in range(ntt):
        ts = slice(tt * 512, (tt + 1) * 512)
        for k4 in range(4):
            c.dma(q, ht[:, k4 * 4:(k4 + 1) * 4, :], hv[:, k4 * 4:(k4 + 1) * 4, ts],
                  writes=[(ht.name, k4)])
        c.op("act", "activation", sq[:], ht[:], AF.Square,
             reads=[(ht.name, k) for k in range(4)], writes=[sq])
        for kc in range(KC):
            c.op("pe", "matmul", ps_stat[:], ones_bf[:], sq[:, kc, :], start=(kc == 0),
                 stop=(kc == KC - 1), reads=[sq, ones_bf], writes=[ps_stat])
        c.op("act", "activation", rt[:], ps_stat[:], AF.Sqrt, bias=eps_t[:, 0:1], scale=1.0 / D,
             reads=[ps_stat, eps_t], writes=[rt])
        c.op("dve", "reciprocal", rstd[:], rt[:], reads=[rt], writes=[rstd])
        for kc in range(KC):
            c.op("dve", "scalar_tensor_tensor", aT[:, kc, ts], ht[:, kc, :], g_col[:, kc:kc + 1],
                 rstd[:], ALU.mult, ALU.mult,
                 reads=[(ht.name, kc // 4), g_col, rstd], writes=[(aT.name, tt)])


def load_w(c, q, wt, src3, ncol, tag):
    for k4 in range(4):
        c.dma(q, wt[:, k4 * 4:(k4 + 1) * 4, 0:ncol], src3[:, k4 * 4:(k4 + 1) * 4, :],
              writes=[(wt.name, k4)])


HROWS16 = 2304
HROWS32 = 268
N_FM = 8
N_TM = 4


def _default_dr(nc):
    return lambda n, s, d, k="ExternalInput": nc.dram_tensor(n, s, d, kind=k).ap()


def build_A(nc=None, dr=None, pfx="", semstack=None):
    standalone = nc is None
    if standalone:
        nc = bass.Bass("TRN2", target_bir_lowering=False)
        dr = _default_dr(nc)
    dt = dr
    hT = dt("hT", [D, TOK], F32, "ExternalInput")
    g1 = dt("g1", [128, KC], F32, "ExternalInput")
    wall = dt("wall", [D, (N_FM + N_TM) * 512], F32, "ExternalInput")
    wgate = dt("wgate", [D, 24], F32, "ExternalInput")
    glng = dt("glng", [1, 512], F32, "ExternalInput")
    glnb = dt("glnb", [1, 512], F32, "ExternalInput")
    wsT = dt("wsT", [128, 4, 128], F32, "ExternalInput")
    bsf = dt("bsf", [1, 512], F32, "ExternalInput")
    tril = dt("tril", [128, 128], F32, "ExternalInput")
    a16 = dt("a16", [2 * HROWS16, TOK], BF16, "ExternalOutput")
    a32 = dt("a32", [2 * HROWS32, TOK], F32, "ExternalOutput")
    ogm = dt("ogm", [512, TOK], BF16, "ExternalOutput")

    with contextlib.ExitStack() as st:
        c = Ctx(nc, st, pfx, semstack)
        aT = c.sb("aT", [128, KC, TOK], BF16)
        ht = c.sb("ht", [128, KC, 512], F32)
        sq = c.sb("sq", [128, KC, 512], BF16)
        wb = [c.sb("wb%d" % i, [128, KC, 512], BF16) for i in range(2)]
        wg = c.sb("wg", [128, KC, 24], BF16)
        uT = c.sb("uT", [128, 4, TOK], BF16)
        ones_bf = c.sb("ones_bf", [128, 128], BF16)
        eps_t = c.sb("eps_t", [128, 1], F32)
        g_col = c.sb("g_col", [128, KC], F32)
        rt = c.sb("rt", [128, 512], F32)
        rstd = c.sb("rstd", [128, 512], F32)
        lng = c.sb("lng", [128, 512], F32)
        lnb = c.sb("lnb", [128, 512], F32)
        bsb = c.sb("bsb", [128, 512], F32)
        wsf = c.sb("wsf", [128, 4, 128], F32)
        trl = c.sb("trl", [128, 128], F32)
        wsm = c.sb("wsm", [128, 4, 128], BF16)
        stg = [c.sb("stg%d" % i, [128, 4, 512], BF16) for i in range(2)]
        stgf = [c.sb("stgf%d" % i, [128, 4, 512], F32) for i in range(1)]
        stgt = [c.sb("stgt%d" % i, [128, 512], BF16) for i in range(2)]
        gst = c.sb("gst", [24, 512], F32)
        vg = c.sb("vg", [128, 512], F32)
        vn = c.sb("vn", [128, 512], F32)
        vln = c.sb("vln", [128, 512], BF16)
        bst = c.sb("bst", [128, 6], F32)
        mv = c.sb("mv", [128, 2], F32)
        sd = c.sb("sd", [128, 1], F32)
        rs1 = c.sb("rs1", [128, 1], F32)
        tmpg = c.sb("tmpg", [128, 512], F32)
        ogs = [c.sb("ogs%d" % i, [128, 4, 512], BF16) for i in range(1)]
        pst = [c.ps("pst%d" % i, [128, 512]) for i in range(6)]
        ps_stat = c.ps("ps_stat", [128, 512])
        psg = c.ps("psg", [128, 512])

        c.op("pool", "memset", ones_bf[:], 1.0, writes=[ones_bf])
        c.op("pool", "memset", eps_t[:], EPS, writes=[eps_t])
        c.dma("sp", g_col[:], g1, writes=[g_col])
        c.dma("sp", lng[:], bcast_rows(glng, 128), writes=[lng])
        c.dma("sp", lnb[:], bcast_rows(glnb, 128), writes=[lnb])
        c.dma("sp", bsb[:], bcast_rows(bsf, 128), writes=[bsb])
        c.dma("sp", wsf[:], wsT, writes=[wsf])
        c.dma("sp", trl[:], tril, writes=[trl])
        for g in range(4):
            c.op("dve", "tensor_tensor", wsm[:, g, :], wsf[:, g, :], trl[:], ALU.mult,
                 reads=[wsf, trl], writes=[wsm])
        c.dma("pool", wg[:], wgate.rearrange("(kc p) n -> p kc n", p=128), writes=[wg])

        wv = wall.rearrange("(kc p) n -> p kc n", p=128)
        load_w(c, "pool", wb[0], wv[:, :, 0:512], 512, 0)

        rmsnorm_tiles(c, hT, g_col, aT, ones_bf, eps_t, ht, sq, ps_stat, rt, rstd, 4)
        aT_all = [(aT.name, tt) for tt in range(4)]

        pcount = [0]

        def next_ps():
            p = pst[pcount[0] % len(pst)]
            pcount[0] += 1
            return p

        for tt in range(4):
            ts = slice(tt * 512, (tt + 1) * 512)
            p = next_ps()
            for kc in range(KC):
                c.op("pe", "matmul", p[0:24, :], wg[:, kc, :], aT[:, kc, ts], start=(kc == 0),
                     stop=(kc == KC - 1), reads=[wg, (aT.name, tt)], writes=[p])
            c.op("act", "activation", gst[:], p[0:24, :], AF.Sigmoid, reads=[p], writes=[gst])
            for hf in range(2):
                c.dma("sp", a32[hf * HROWS32 + 256:hf * HROWS32 + 268, ts], gst[hf * 12:(hf + 1) * 12, :], reads=[gst])

        ev = 0
        for g in range(N_FM + N_TM):
            w = wb[g % 2]
            if g + 1 < N_FM + N_TM:
                load_w(c, "pool", wb[(g + 1) % 2], wv[:, :, (g + 1) * 512:(g + 2) * 512], 512, g + 1)
            wkeys = [(w.name, k) for k in range(4)]
            if g < N_FM:
                for tt in range(4):
                    ts = slice(tt * 512, (tt + 1) * 512)
                    if g == 6:
                        s_t = stgf[0]
                    elif g == 7:
                        s_t = None
                    else:
                        s_t = stg[(g * 4 + tt) % 2]
                    for blk in range(4):
                        p = next_ps()
                        for kc in range(KC):
                            c.op("pe", "matmul", p[:], w[:, kc, blk * 128:(blk + 1) * 128], aT[:, kc, ts],
                                 start=(kc == 0), stop=(kc == KC - 1),
                                 reads=[(w.name, kc // 4), (aT.name, tt)], writes=[p])
                        if g == 7:
                            c.op("act", "activation", uT[:, blk, ts], p[:], AF.Gelu_apprx_tanh,
                                 reads=[p], writes=[(uT.name, tt, blk)])
                        elif g == 6:
                            c.op("act", "activation", s_t[:, blk, :], p[:], AF.Silu,
                                 reads=[p], writes=[(s_t.name, blk)])
                        else:
                            sc = SCALE if g in (0, 3) else 1.0
                            if ev % 2 == 0:
                                c.op("act", "activation", s_t[:, blk, :], p[:], AF.Copy, scale=sc,
                                     reads=[p], writes=[(s_t.name, blk)])
                            else:
                                c.op("dve", "tensor_scalar", s_t[:, blk, :], p[:], sc, None, ALU.mult,
                                     reads=[p], writes=[(s_t.name, blk)])
                            ev += 1
                    if g == 6:
                        for hf in range(2):
                            dst = a32[hf * HROWS32:hf * HROWS32 + 256, :].rearrange("(b p) t -> p b t", p=128)[:, :, ts]
                            c.dma("sp", dst, s_t[:, hf * 2:(hf + 1) * 2, :], reads=[(s_t.name, b_) for b_ in range(4)])
                    elif g < 6:
                        r0 = (g // 3) * HROWS16 + (g % 3) * 512
                        dst = a16[r0:r0 + 512, :].rearrange("(b p) t -> p b t", p=128)[:, :, ts]
                        c.dma("sp", dst, s_t[:], reads=[(s_t.name, b_) for b_ in range(4)])
            else:
                gi = g - N_FM
                for tb in range(16):
                    tt = tb // 4
                    tks = slice(tb * 128, (tb + 1) * 128)
                    p = next_ps()
                    for kc in range(KC):
                        c.op("pe", "matmul", p[:], aT[:, kc, tks], w[:, kc, :], start=(kc == 0),
                             stop=(kc == KC - 1), reads=[(w.name, kc // 4), (aT.name, tt)], writes=[p])
                    if gi < 3:
                        s_t = stgt[tb % 2]
                        if tb % 2 == 0:
                            c.op("act", "activation", s_t[:], p[:], AF.Copy, reads=[p], writes=[s_t])
                        else:
                            c.op("dve", "tensor_copy", s_t[:], p[:], reads=[p], writes=[s_t])
                        for hf in range(2):
                            off = (hf * HROWS16 + 1536) * TOK + tb * 128 * 768 + gi * 256
                            c.dma("sp", bass.AP(a16.tensor, off, [[768, 128], [1, 256]]),
                                  s_t[:, hf * 256:(hf + 1) * 256], reads=[s_t])
                    else:
                        c.op("act", "activation", vg[:], p[:], AF.Gelu_apprx_tanh, reads=[p], writes=[vg])
                        c.op("dve", "bn_stats", bst[:], vg[:], reads=[vg], writes=[bst])
                        c.op("dve", "bn_aggr", mv[:], bst[:], reads=[bst], writes=[mv])
                        c.op("act", "activation", sd[:], mv[:, 1:2], AF.Sqrt, bias=eps_t[:, 0:1], scale=1.0,
                             reads=[mv, eps_t], writes=[sd])
                        c.op("dve", "reciprocal", rs1[:], sd[:], reads=[sd], writes=[rs1])
                        c.op("dve", "tensor_scalar", vn[:], vg[:], mv[:, 0:1], rs1[:, 0:1], ALU.subtract,
                             ALU.mult, reads=[vg, mv, rs1], writes=[vn])
                        c.op("pool", "tensor_tensor", vn[:], vn[:], lng[:], ALU.mult,
                             reads=[vn, lng], writes=[vn])
                        c.op("pool", "tensor_tensor", vln[:], vn[:], lnb[:], ALU.add,
                             reads=[vn, lnb], writes=[vln])
                        for gg in range(4):
                            c.op("pe", "matmul", psg[:, gg * 128:(gg + 1) * 128], vln[:, gg * 128:(gg + 1) * 128],
                                 wsm[:, gg, :], start=True, stop=True, reads=[vln, wsm], writes=[psg])
                        c.op("dve", "tensor_tensor", tmpg[:], psg[:], bsb[:], ALU.add,
                             reads=[psg, bsb], writes=[tmpg])
                        og = ogs[0]
                        c.op("pool", "tensor_tensor", og[:, :, (tb % 4) * 128:(tb % 4 + 1) * 128],
                             tmpg[:].rearrange("p (g t) -> p g t", g=4), uT[:, :, tks], ALU.mult,
                             reads=[tmpg] + [(uT.name, tt, b_) for b_ in range(4)],
                             writes=[(og.name, tb % 4)])
                        if tb % 4 == 3:
                            dst = ogm.rearrange("(g p) t -> p g t", p=128)[:, :, tt * 512:(tt + 1) * 512]
                            c.dma("sp", dst, og[:], reads=[(og.name, b_) for b_ in range(4)])
        c.finish()
        if not standalone:
            c.release()
        print("A: ins", c.n_ins, "waits", c.n_wait)
    return nc


_OFF = {}
_o = 0
for _n, _w in (("q", 1024), ("kcmp", 256), ("vcmp", 256), ("kslc", 256), ("vslc", 256), ("kwin", 256),
               ("vwin", 256), ("gate", 24), ("u", 512), ("v", 512), ("rq", 512), ("rk", 512),
               ("rv", 512), ("rg", 512)):
    _OFF[_n] = (_o, _o + _w)
    _o += _w


def pack_A_weights(w_in_l):
    cols = lambda n: w_in_l[:, _OFF[n][0]:_OFF[n][1]]
    hcol = lambda n, hf, w: w_in_l[:, _OFF[n][0] + hf * w:_OFF[n][0] + (hf + 1) * w]
    fm = []
    for hf in range(2):
        fm += [hcol("q", hf, 512), hcol("kcmp", hf, 128), hcol("vcmp", hf, 128), hcol("kslc", hf, 128),
               hcol("kwin", hf, 128), hcol("rq", hf, 256), hcol("rk", hf, 256)]
    fm += [cols("rg"), cols("u")]
    tm = [hcol("vslc", 0, 128), hcol("vwin", 0, 128), hcol("vslc", 1, 128), hcol("vwin", 1, 128),
          cols("rk"), cols("rv"), cols("v")]
    wall = np.ascontiguousarray(np.concatenate(fm + tm, axis=1))
    assert wall.shape[1] == (N_FM + N_TM) * 512
    return wall, np.ascontiguousarray(cols("gate"))


NEG = -30000.0
NCMP = 255


def nsa_consts():
    p = np.arange(128)[:, None]
    f = np.arange(512)[None, :]
    nbc = np.stack([np.where(f - p - 128 * r >= 0, 0.0, NEG) for r in range(4)])
    nbw = np.stack([np.where(f - p - 128 * r < 512, 0.0, NEG) for r in (-4, -3, -2, -1)])
    nbm = np.stack([np.where(f - 16 * p + 512 * r - 31 >= 0, 0.0, NEG) for r in range(5)])
    nbm1 = nbm[0:4].copy()
    nbm1[:, 127, :] = NEG
    masks = np.concatenate([nbc, nbw, nbm, nbm1], 0).astype(np.float32)
    masks = np.ascontiguousarray(masks.transpose(1, 0, 2))
    pk = np.arange(128)
    posk = np.stack([pk, np.ones(128), np.ones(128)]).astype(np.float32)
    poskc = np.stack([16 * pk, np.ones(128), np.ones(128)]).astype(np.float32)
    ff = np.arange(512)
    e30 = np.zeros((64, 4096), np.float32)
    for j in range(64):
        e30[j, j * 64:(j + 1) * 64] = 30000.0
    c0 = np.arange(256)[:, None] * 16
    s0 = np.arange(64)[None, :] * 64
    ov = np.minimum(c0 + 32, s0 + 64) - np.maximum(c0, s0)
    share = (np.clip(ov, 0, 32) / 32.0).astype(np.float32)
    share[255] = 0
    share = np.ascontiguousarray(share.reshape(2, 128, 64).transpose(1, 0, 2))
    t = np.arange(4096)
    cur = (t // 64)[:, None]
    j = np.arange(64)[None, :]
    forced = (j == 0) | (j == cur) | (j == cur - 1)
    btab = np.where(forced, 1e4, 0.0)
    btab = np.where(j > cur, -1e30, btab).astype(np.float32)
    btab = np.ascontiguousarray(btab.reshape(32, 128, 64).transpose(1, 0, 2))
    selg = np.zeros((128, 12, 128), np.float32)
    for n in range(12):
        selg[n, n, :] = 1.0
        selg[32 + n, n, :] = 1.0
    selg = selg.reshape(128, 12 * 128)
    ek = np.zeros((128, 4096), np.float32)
    ek[0:64] = e30
    ek[64:67] = np.tile(posk, (1, 32))
    ekc = np.zeros((128, 128), np.float32)
    ekc[64:67] = poskc
    ekw = np.zeros((128, 128), np.float32)
    ekw[64:67] = posk
    return dict(masks=masks, ek=ek, ekc=ekc, ekw=ekw, share=share, btab=btab, selg=selg,
                ident=np.eye(128, dtype=np.float32))


def nsa_posq(hf):
    ff = np.arange(512)
    out = np.zeros((4, 3, 512), np.float32)
    for r in range(4):
        h = 4 * hf + r
        slope = 2.0 ** (-8.0 * (h + 1) / 8)
        out[r, 0] = slope
        out[r, 1] = -slope * 64 * (ff // 64)
        out[r, 2] = -slope * (ff % 64)
    return out


def nsa_bias_table(hf):
    tb = np.zeros((192,), np.float32)
    for r in range(4):
        slope = 2.0 ** (-(4 * hf + r + 1))
        for cb in range(2):
            for tc in range(8):
                tb[r * 16 + cb * 8 + tc] = slope * (2048 * cb + 31 - 512 * tc)
        for rel in range(-28, 4):
            tb[64 + r * 32 + rel + 28] = slope * 128 * rel
    return np.ascontiguousarray(np.broadcast_to(tb[None, :], (128, 192)))


def build_B1(nc=None, dr=None, pfx="", semstack=None, pre_hook=None):
    standalone = nc is None
    if standalone:
        nc = bass.Bass("TRN2", target_bir_lowering=False)
        dr = _default_dr(nc)
    dt = dr
    b16 = dt("b16", [2 * HROWS16, TOK], BF16)
    b32 = dt("b32", [2 * HROWS32, TOK], F32)
    w1_d = dt("d_w1", [2, 128, 32, 128], F32)
    pe_d = dt("d_peT", [2, 128, 32], F32)
    w2_d = dt("d_w2", [2, 128, 128], F32)
    masks_d = dt("d_masks", [128, 17, 512], F32)
    ekc_d = dt("d_ekc", [128, 128], F32)
    ekw_d = dt("d_ekw", [128, 128], F32)
    posq_d = dt("posq", [4, 3, 512], F32)
    ek_d = dt("d_ek", [128, 4096], F32)
    share_d = dt("d_share", [128, 2, 64], F32)
    btab_d = dt("d_btab", [128, 32, 64], F32)
    selg_d = dt("d_selg", [128, 12 * 128], F32)
    ident_d = dt("d_ident", [128, 128], F32)
    btl_d = dt("d_btl", [128, 192], F32)
    out_d = dt("bout", [768, T], BF16, "ExternalOutput")

    with contextlib.ExitStack() as st:
        c = Ctx(nc, st, pfx, semstack)
        qT = c.sb("qT", [128, 4, T], BF16)
        kT = c.sb("kT", [128, 4, T], BF16)
        vs = c.sb("vs", [128, 32, 128], BF16)
        vw = c.sb("vw", [128, 32, 128], BF16)
        masks = c.sb("masks", [128, 17, 512], BF16)
        ekc = c.sb("ekc", [128, 128], BF16)
        ekw = c.sb("ekw", [128, 128], BF16)
        ek = c.sb("ek", [128, 4096], BF16)
        nq = [c.sb("nq%d" % r, [128, 512], BF16) for r in range(4)]
        gst32 = [c.sb("gst32_%d" % i, [128, 512], F32) for i in range(2)]
        gh = [c.sb("gh%d" % i, [128, 512], BF16) for i in range(2)]
        hb = c.sb("hb", [128, 512], BF16)
        share = c.sb("share", [128, 2, 64], BF16)
        btab = c.sb("btab", [128, 32, 64], F32)
        selg = c.sb("selg", [128, 12 * 128], BF16)
        ident = c.sb("ident", [128, 128], BF16)
        identf = c.sb("identf", [128, 128], F32)
        ones = c.sb("ones", [128, 128], BF16)
        w1 = [c.sb("w1_%d" % i, [128, 32, 128], BF16) for i in range(2)]
        peT = c.sb("peT", [128, 2, 32], BF16)
        w2 = c.sb("w2", [128, 2, 128], BF16)
        b1 = c.sb("b1", [128, 2], F32)
        g1 = c.sb("g1", [128, 2, 256], BF16)
        kccT = c.sb("kccT", [128, 256], BF16)
        vcc = c.sb("vcc", [128, 2, 128], BF16)
        Et = [c.sb("Et%d" % i, [128, 512], BF16) for i in range(3)]
        Pn = c.sb("Pn", [128, 4, 2, 512], BF16)
        rd = c.sb("rd", [128, 512], F32)
        lnd = c.sb("lnd", [128, 512], F32)
        tiny = c.sb("tiny", [128, 1], F32)
        wgt = c.sb("wgt", [128, 512], F32)
        tmp = c.sb("tmp", [128, 512], F32)
        acc = c.sb("acc", [128, 512], F32)
        caccs = [c.sb("cacc%d" % i, [128, 4, 512], F32) for i in range(2)]
        ost = [c.sb("ost%d" % i, [128, 512], BF16) for i in range(2)]
        scr = c.sb("scr", [128, 4, 64], F32)
        scr2 = c.sb("scr2", [128, 4, 64], F32)
        m8 = c.sb("m8", [128, 4, 16], F32)
        thr = c.sb("thr", [128, 4], F32)
        psS = [c.ps("psS%d" % i, [128, 512]) for i in range(2)]
        psOs = [c.ps("psO%d" % i, [128, 512]) for i in range(2)]
        psDs = [c.ps("psD%d" % i, [128, 512]) for i in range(2)]
        psG = c.ps("psG", [128, 512])
        psI = c.ps("psIX", [128, 512])
        psX = psI
        sm1 = c.sb("sm1b", [128, 4, 64], F32)

        btl = c.sb("btl", [128, 192], F32)
        c.dma("sp", btl[:], btl_d, writes=[btl])
        c.op("pool", "memset", ones[:], 1.0, writes=[ones])
        c.op("pool", "memset", tiny[:], 1e-18, writes=[tiny])
        for th in range(2):
            tsl = slice(th * TOK, (th + 1) * TOK)
            for r in range(4):
                c.dma("sp", qT[:, r, tsl], b16[th * HROWS16 + r * 128:th * HROWS16 + (r + 1) * 128, :],
                      writes=[(qT.name, r)])
                c.dma("sp", kT[:, r, tsl], b16[th * HROWS16 + 512 + r * 128:th * HROWS16 + 512 + (r + 1) * 128, :],
                      writes=[(kT.name, r)])
            toff = (th * HROWS16 + 1536) * TOK
            c.dma("sp", vs[:, th * 16:(th + 1) * 16, :],
                  bass.AP(b16.tensor, toff, [[768, 128], [768 * 128, 16], [1, 128]]), writes=[vs])
            c.dma("sp", vw[:, th * 16:(th + 1) * 16, :],
                  bass.AP(b16.tensor, toff + 128, [[768, 128], [768 * 128, 16], [1, 128]]), writes=[vw])
        c.dma("pool", masks[:], masks_d, writes=[masks])
        c.dma("pool", ekc[:], ekc_d, writes=[ekc])
        c.dma("pool", ekw[:], ekw_d, writes=[ekw])
        for r in range(4):
            c.op("dve", "memset", nq[r][:], 0.0, writes=[nq[r]])
            c.dma("pool", nq[r][64:67, :], posq_d[r], writes=[nq[r]])
        for i in range(2):
            c.op("dve", "memset", gst32[i][:], 0.0, writes=[gst32[i]])
            c.op("dve", "memset", gh[i][:], 0.0, writes=[gh[i]])
        c.op("dve", "memset", g1[:], 0.0, writes=[(g1.name, 0), (g1.name, 1)])
        c.op("dve", "memset", kccT[:], 0.0, writes=[kccT])
        c.op("dve", "memset", vcc[:], 0.0, writes=[(vcc.name, 0), (vcc.name, 1)])
        c.dma("pool", ek[:], ek_d, writes=[ek])
        c.dma("pool", share[:], share_d, writes=[share])
        c.dma("sp", btab[:], btab_d, writes=[btab])
        c.dma("pool", selg[:], selg_d, writes=[selg])
        c.dma("pool", ident[:], ident_d, writes=[ident])
        c.dma("sp", identf[:], ident_d, writes=[identf])
        for i in range(2):
            c.dma("pool", w1[i][:], w1_d[i], writes=[w1[i]])
            c.dma("pool", peT[:, i, :], pe_d[i], writes=[(peT.name, i)])
            c.dma("pool", w2[:, i, :], w2_d[i], writes=[(w2.name, i)])

        bg = list(pre_hook()) if pre_hook is not None else []
        for i in range(2):
            for j in range(32):
                c.op("pe", "matmul", psX[:, 0:1], w1[i][:, j, :], peT[:, i, j:j + 1], start=(j == 0),
                     stop=(j == 31), reads=[w1[i], (peT.name, i)], writes=[psX])
            c.op("dve", "tensor_copy", b1[:, i:i + 1], psX[:, 0:1], reads=[psX], writes=[(b1.name, i)])
            for j in range(32):
                c.op("pe", "matmul", psI[:, 0:NCMP], w1[i][:, j, :], kT[:, i, j:j + 16 * (NCMP - 1) + 1:16],
                     start=(j == 0), stop=(j == 31), reads=[w1[i], (kT.name, i)], writes=[psI])
            c.op("act", "activation", g1[:, i, 0:NCMP], psI[:, 0:NCMP], AF.Gelu_apprx_tanh,
                 bias=b1[:, i:i + 1], scale=1.0, reads=[psI, (b1.name, i)], writes=[(g1.name, i)])
        c.op("pe", "matmul", psI[:, 0:NCMP], w2[:, 0, :], g1[:, 0, 0:NCMP], start=True, stop=True,
             reads=[(w2.name, 0), (g1.name, 0)], writes=[psI])
        c.op("dve", "tensor_copy", kccT[:, 0:NCMP], psI[:, 0:NCMP], reads=[psI], writes=[kccT])
        for cb in range(2):
            M = 128
            c.op("pe", "matmul", psX[0:M, 0:128], g1[:, 1, cb * 128:cb * 128 + M], w2[:, 1, :], start=True,
                 stop=True, reads=[(w2.name, 1), (g1.name, 1)], writes=[psX])
            c.op("dve", "tensor_copy", vcc[0:M, cb, :], psX[0:M, 0:128], reads=[psX], writes=[(vcc.name, cb)])

        sctr = [0]
        octr = [0]

        def next_S():
            p = psS[sctr[0] % 2]
            e = Et[sctr[0] % 3]
            sctr[0] += 1
            return p, e

        def run_blocks(blocks):
            psO = psOs[octr[0] % 2]
            psD = psDs[octr[0] % 2]
            octr[0] += 1
            n = len(blocks)
            pend = None

            def pv(b, e, i):
                M = b["M"]
                c.op("pe", "matmul", psO[:], b["v"], e[0:M, :], start=(i == 0), stop=(i == n - 1),
                     reads=[b["vkey"], e], writes=[psO])
                c.op("pe", "matmul", psD[:], ones[0:M, :], e[0:M, :], start=(i == 0), stop=(i == n - 1),
                     reads=[ones, e], writes=[psD])
                if b.get("keep") is not None:
                    dst, key = b["keep"]
                    c.op("pool", "tensor_copy", dst, e[0:M, :], reads=[e], writes=[key])

            for i, b in enumerate(blocks):
                p, e = next_S()
                M = b["M"]
                ns = len(b["s_ops"])
                for j, (lh, rh, rd) in enumerate(b["s_ops"]):
                    c.op("pe", "matmul", p[0:M, :], lh, rh, start=(j == 0), stop=(j == ns - 1), reads=rd, writes=[p])
                c.op("act", "activation", e[0:M, :], p[0:M, :], AF.Exp, bias=b["bias"], scale=1.0,
                     reads=[p, btl], writes=[e])
                if pend is not None:
                    pv(*pend)
                pend = (b, e, i)
            pv(*pend)
            return psO, psD

        def branch_finish(tc, r, br, first, psO, psD):
            g = gh[tc % 2]
            c.op("act", "activation", lnd[:], psD[:], AF.Ln, bias=tiny[:, 0:1], scale=1.0, reads=[psD, tiny], writes=[lnd])
            c.op("act", "activation", rd[:], lnd[:], AF.Exp, scale=-1.0, reads=[lnd], writes=[rd])
            n = r * 3 + br
            c.op("pe", "matmul", psG[:], selg[:, n * 128:(n + 1) * 128], g[:], start=True, stop=True,
                 reads=[selg, g], writes=[psG])
            c.op("dve", "tensor_tensor", wgt[:], psG[:], rd[:], ALU.mult, reads=[psG, rd], writes=[wgt])
            if first:
                c.op("dve", "tensor_tensor", acc[:], psO[:], wgt[:], ALU.mult, reads=[psO, wgt], writes=[acc])
            else:
                c.op("dve", "tensor_tensor", tmp[:], psO[:], wgt[:], ALU.mult, reads=[psO, wgt], writes=[tmp])
                c.op("pool", "tensor_tensor", acc[:], acc[:], tmp[:], ALU.add, reads=[acc, tmp], writes=[acc])

        def front(tc):
            ts = slice(tc * 512, (tc + 1) * 512)
            g = gh[tc % 2]
            g32 = gst32[tc % 2]
            cacc = caccs[tc % 2]

            def load_gates(tcn):
                gr0 = (tcn // 4) * HROWS32 + 256
                gsrc = b32[gr0:gr0 + 12, (tcn % 4) * 512:(tcn % 4 + 1) * 512]
                c.dma("sp", gst32[tcn % 2][0:12, :], gsrc, writes=[gst32[tcn % 2]])
                c.dma("sp", gst32[tcn % 2][32:44, :], gsrc, writes=[gst32[tcn % 2]])

            if tc == 0:
                load_gates(0)
            c.op("act", "activation", g[0:12, :], g32[0:12, :], AF.Copy, reads=[g32], writes=[g])
            c.op("act", "activation", hb[32:44, :], g32[32:44, :], AF.Copy, reads=[g32], writes=[hb])
            c.op("dve", "tensor_tensor", g[32:44, :], g32[32:44, :], hb[32:44, :], ALU.subtract,
                 reads=[g32, hb], writes=[g])
            if tc + 1 < 8:
                load_gates(tc + 1)
            cbs = [0] if tc < 4 else [0, 1]
            for r in range(4):
                blocks = []
                for cb in cbs:
                    M = 128
                    rel = tc - 4 * cb
                    ops = [(kccT[:, cb * 128:cb * 128 + M], qT[:, r, ts], [kccT, (qT.name, r)]),
                           (ekc[:], nq[r][:], [ekc, nq[r]])]
                    if cb == 1:
                        ops.append((ident[:], masks[:, 13 + rel, :], [ident, masks]))
                    elif rel <= 4:
                        ops.append((ident[:], masks[:, 8 + rel, :], [ident, masks]))
                    bi = r * 16 + cb * 8 + tc
                    blocks.append(dict(M=M, s_ops=ops, bias=btl[0:M, bi:bi + 1], v=vcc[0:M, cb, :],
                                       vkey=(vcc.name, cb), keep=(Pn[0:M, r, cb, :], (Pn.name, r, cb))))
                psO, psD = run_blocks(blocks)
                branch_finish(tc, r, 0, True, psO, psD)
                for cb in cbs:
                    M = 128
                    c.op("dve", "tensor_tensor", Pn[0:M, r, cb, :], Pn[0:M, r, cb, :], rd[0:M, :], ALU.mult,
                         reads=[(Pn.name, r, cb), rd], writes=[(Pn.name, r, cb)])
                c.op("dve", "tensor_copy", cacc[:, r, :], acc[:], reads=[acc], writes=[(cacc.name, r)])
            for tb in range(4):
                gtb = tc * 4 + tb
                n = 0
                tot = 4 * len(cbs)
                for r in range(4):
                    for cb in cbs:
                        M = 128
                        c.op("pe", "matmul", psI[:, tb * 64:(tb + 1) * 64], Pn[0:M, r, cb, tb * 128:(tb + 1) * 128],
                             share[0:M, cb, :], start=(n == 0), stop=(n == tot - 1),
                             reads=[(Pn.name, r, cb), share], writes=[psI])
                        n += 1
            K4 = range(4)
            for tb in K4:
                c.op("dve", "tensor_tensor", scr[:, tb, :], psI[:, tb * 64:(tb + 1) * 64], btab[:, tc * 4 + tb, :],
                     ALU.add, reads=[psI, btab], writes=[(scr.name, tb)])
            for tb in K4:
                c.op("dve", "max", m8[:, tb, 0:8], scr[:, tb, :], reads=[(scr.name, tb)], writes=[(m8.name, tb)])
            for tb in K4:
                c.op("dve", "match_replace", scr2[:, tb, :], m8[:, tb, 0:8], scr[:, tb, :], -1e30,
                     reads=[(scr.name, tb), (m8.name, tb)], writes=[(scr2.name, tb)])
            for tb in K4:
                c.op("dve", "max", m8[:, tb, 8:16], scr2[:, tb, :], reads=[(scr2.name, tb)], writes=[(m8.name, tb)])
            for tb in K4:
                c.op("dve", "tensor_scalar", thr[:, tb:tb + 1], m8[:, tb, 15:16], -1e29, None, ALU.max,
                     reads=[(m8.name, tb)], writes=[(thr.name, tb)])
            for tb in K4:
                c.op("dve", "tensor_scalar", sm1[:, tb, :], scr[:, tb, :], thr[:, tb:tb + 1], 1.0, ALU.is_ge,
                     ALU.subtract, reads=[(scr.name, tb), (thr.name, tb)], writes=[(sm1.name, tb)])

        def mid(tc):
            ts = slice(tc * 512, (tc + 1) * 512)
            cbs = [0] if tc < 4 else [0, 1]
            cacc = caccs[tc % 2]
            for tb in range(4):
                c.op("pe", "transpose", psX[0:64, tb * 128:(tb + 1) * 128], sm1[:, tb, :], identf[:],
                     reads=[(sm1.name, tb), identf], writes=[psX])
            c.op("act", "activation", nq[0][0:64, :], psX[0:64, :], AF.Copy, reads=[psX], writes=[nq[0]])
            for r in range(1, 4):
                c.op("pool", "tensor_copy", nq[r][0:64, :], nq[0][0:64, :], reads=[nq[0]], writes=[nq[r]])

        def sw(tc):
            ts = slice(tc * 512, (tc + 1) * 512)
            cbs = [0] if tc < 4 else [0, 1]
            cacc = caccs[tc % 2]
            for r in range(4):
                for _ in range(2):
                    if bg:
                        d_, s_ = bg.pop(0)
                        c.dma("pool", d_, s_)
                c.op("dve", "tensor_copy", acc[:], cacc[:, r, :], reads=[(cacc.name, r)], writes=[acc])
                blocks = []
                for kb in range(4 * tc + 4):
                    rel = kb - 4 * tc
                    ks = slice(kb * 128, (kb + 1) * 128)
                    ops = [(kT[:, 2, ks], qT[:, r, ts], [(kT.name, 2), (qT.name, r)]),
                           (ek[:, ks], nq[r][:], [ek, nq[r]])]
                    if rel >= 0:
                        ops.append((ident[:], masks[:, rel, :], [ident, masks]))
                    bi = 64 + r * 32 + rel + 28
                    blocks.append(dict(M=128, s_ops=ops, bias=btl[:, bi:bi + 1], v=vs[:, kb, :], vkey=vs))
                psO, psD = run_blocks(blocks)
                branch_finish(tc, r, 1, False, psO, psD)
                blocks = []
                for kb in range(max(0, 4 * tc - 4), 4 * tc + 4):
                    rel = kb - 4 * tc
                    ks = slice(kb * 128, (kb + 1) * 128)
                    mi = rel if rel >= 0 else 4 + (rel + 4)
                    ops = [(kT[:, 3, ks], qT[:, r, ts], [(kT.name, 3), (qT.name, r)]),
                           (ekw[:], nq[r][:], [ekw, nq[r]]),
                           (ident[:], masks[:, mi, :], [ident, masks])]
                    bi = 64 + r * 32 + rel + 28
                    blocks.append(dict(M=128, s_ops=ops, bias=btl[:, bi:bi + 1], v=vw[:, kb, :], vkey=vw))
                psO, psD = run_blocks(blocks)
                branch_finish(tc, r, 2, False, psO, psD)
                o = ost[r % 2]
                c.op("act", "activation", o[:], acc[:], AF.Copy, reads=[acc], writes=[o])
                c.dma("sp", out_d[r * 128:(r + 1) * 128, ts], o[:], reads=[o])

        front(0)
        mid(0)
        for tc in range(8):
            if tc + 1 < 8:
                front(tc + 1)
            sw(tc)
            if tc + 1 < 8:
                mid(tc + 1)
        while bg:
            d_, s_ = bg.pop(0)
            c.dma("pool", d_, s_)
        c.finish()
        if not standalone:
            c.release()
        print("B1: ins", c.n_ins, "waits", c.n_wait)
    return nc


def ret_consts(hf):
    n = np.arange(128, dtype=np.float64)
    dec = np.zeros((2, 128, 128), np.float32)
    zeta = np.zeros((128, 2), np.float32)
    xi = np.zeros((2, 1, 128), np.float32)
    dc = []
    for i in range(2):
        h = 2 * hf + i
        lg = np.log1p(-2.0 ** (-5.0 - h))
        rel = n[None, :] - n[:, None]
        dec[i] = np.where(rel >= 0, np.exp(lg * np.maximum(rel, 0)), 0.0) * SCALE
        zeta[:, i] = np.exp(lg * (127.0 - n)) * SCALE
        xi[i, 0] = np.exp(lg * (n + 1.0))
        dc.append(float(np.exp(lg * 128.0)))
    dct = np.ascontiguousarray(np.broadcast_to(np.asarray(dc, np.float32)[None, :], (128, 2)))
    return dict(d_dec=np.ascontiguousarray(dec.transpose(1, 0, 2)), d_zeta=zeta, d_xi=xi, d_dc=dct)


def build_B2(nc=None, dr=None, pfx="", semstack=None):
    standalone = nc is None
    if standalone:
        nc = bass.Bass("TRN2", target_bir_lowering=False)
        dr = _default_dr(nc)
    dt = dr
    b16 = dt("b16", [2 * HROWS16, TOK], BF16)
    b32 = dt("b32", [2 * HROWS32, TOK], F32)
    gn_d = dt("d_gn", [128, 2], F32)
    dec_d = dt("d_dec", [128, 2, 128], F32)
    zeta_d = dt("d_zeta", [128, 2], F32)
    xi_d = dt("d_xi", [2, 1, 128], F32)
    dc_d = dt("d_dc", [128, 2], F32)
    out_d = dt("bout", [768, T], BF16, "ExternalOutput")
    with contextlib.ExitStack() as st:
        c = Ctx(nc, st, pfx, semstack)
        rq = c.sb("rq", [128, 2, T], BF16)
        rk = c.sb("rk", [128, 2, T], BF16)
        qx = c.sb("qx", [128, 2, T], BF16)
        rktm = c.sb("rktm", [128, 32, 256], BF16)
        rvtm = c.sb("rvtm", [128, 32, 256], BF16)
        kz = c.sb("kz", [128, 32, 256], BF16)
        gn = c.sb("gn", [128, 2], F32)
        dec = c.sb("dec", [128, 2, 128], F32)
        zeta = c.sb("zeta", [128, 2], F32)
        xib = c.sb("xib", [128, 2, 128], F32)
        onesf = c.sb("onesf", [128, 128], F32)
        eps_t = c.sb("eps_t", [128, 1], F32)
        state = [c.sb("state%d" % i, [128, 128], F32) for i in range(2)]
        prevb = [c.sb("prevb%d" % i, [128, 128], BF16) for i in range(2)]
        scb = [c.sb("scb%d" % i, [128, 128], BF16) for i in range(2)]
        ysb = c.sb("ysb", [128, 512], F32)
        ysq = c.sb("ysq", [128, 512], F32)
        m2 = c.sb("m2", [128, 512], F32)
        var = c.sb("var", [128, 512], F32)
        sd = c.sb("sd", [128, 512], F32)
        yc = c.sb("yc", [128, 512], F32)
        rgt = [c.sb("rgt%d" % i, [128, 512], F32) for i in range(2)]
        ost = [c.sb("ost%d" % i, [128, 512], BF16) for i in range(2)]
        psSc = [c.ps("psSc%d" % i, [128, 512]) for i in range(2)]
        psY = [c.ps("psY%d" % i, [128, 512]) for i in range(2)]
        psKVs = [c.ps("psKV%d" % i, [128, 512]) for i in range(2)]
        psM = c.ps("psM", [128, 512])
        psE = c.ps("psE", [128, 512])

        c.op("pool", "memset", onesf[:], 1.0 / 128.0, writes=[onesf])
        c.op("pool", "memset", eps_t[:], EPS, writes=[eps_t])
        for i in range(2):
            for th in range(2):
                tsl = slice(th * TOK, (th + 1) * TOK)
                c.dma("sp", rq[:, i, tsl], b16[th * HROWS16 + 1024 + i * 128:th * HROWS16 + 1024 + (i + 1) * 128, :],
                      writes=[(rq.name, i)])
                c.dma("sp", rk[:, i, tsl], b16[th * HROWS16 + 1280 + i * 128:th * HROWS16 + 1280 + (i + 1) * 128, :],
                      writes=[(rk.name, i)])
            c.dma("sp", xib[:, i, :], bcast_rows(xi_d[i], 128), writes=[(xib.name, i)])
            c.op("pool", "memset", state[i][:], 0.0, writes=[state[i]])
            c.op("pool", "memset", prevb[i][:], 0.0, writes=[prevb[i]])
        for th in range(2):
            toff = (th * HROWS16 + 1536) * TOK
            c.dma("sp", rktm[:, th * 16:(th + 1) * 16, :],
                  bass.AP(b16.tensor, toff + 256, [[768, 128], [768 * 128, 16], [1, 256]]), writes=[rktm])
            c.dma("sp", rvtm[:, th * 16:(th + 1) * 16, :],
                  bass.AP(b16.tensor, toff + 512, [[768, 128], [768 * 128, 16], [1, 256]]), writes=[rvtm])
        c.dma("sp", gn[:], gn_d, writes=[gn])
        c.dma("sp", dec[:], dec_d, writes=[dec])
        c.dma("sp", zeta[:], zeta_d, writes=[zeta])
        dct = c.sb("dct", [128, 2], F32)
        c.dma("sp", dct[:], dc_d, writes=[dct])
        for i in range(2):
            c.op("dve", "tensor_scalar", kz[:, :, i * 128:(i + 1) * 128], rktm[:, :, i * 128:(i + 1) * 128],
                 zeta[:, i:i + 1], None, ALU.mult, reads=[rktm, zeta], writes=[(kz.name, i)])
            xb = xib[:, i, :]
            xbb = bass.AP(xb.tensor, xb.offset, [list(xb.ap[0]), [0, 32], list(xb.ap[-1])])
            c.op("pool", "tensor_tensor", qx[:, i, :].rearrange("p (c n) -> p c n", n=128),
                 rq[:, i, :].rearrange("p (c n) -> p c n", n=128), xbb, ALU.mult,
                 reads=[(rq.name, i), (xib.name, i)], writes=[(qx.name, i)])
        k = 0
        for grp in range(8):
            for i in range(2):
                pY = psY[i]
                for cc in range(4):
                    ch = grp * 4 + cc
                    cs = slice(ch * 128, (ch + 1) * 128)
                    hs = slice(i * 128, (i + 1) * 128)
                    pS = psSc[k % 2]
                    sb_ = scb[k % 2]
                    k += 1
                    c.op("pe", "matmul", pS[:, 0:128], rk[:, i, cs], rq[:, i, cs], start=True, stop=True,
                         reads=[(rk.name, i), (rq.name, i)], writes=[pS])
                    c.op("dve", "tensor_tensor", sb_[:], pS[:, 0:128], dec[:, i, :], ALU.mult,
                         reads=[pS, dec], writes=[sb_])
                    c.op("pe", "matmul", pY[:, cc * 128:(cc + 1) * 128], rvtm[:, ch, hs], sb_[:], start=True,
                         stop=False, reads=[rvtm, sb_], writes=[pY])
                    c.op("pe", "matmul", pY[:, cc * 128:(cc + 1) * 128], prevb[i][:], qx[:, i, cs], start=False,
                         stop=True, reads=[prevb[i], (qx.name, i)], writes=[pY])
                    psKV = psKVs[i]
                    c.op("pe", "matmul", psKV[:, 0:128], kz[:, ch, hs], rvtm[:, ch, hs],
                         start=True, stop=True, reads=[(kz.name, i), rvtm], writes=[psKV])
                    c.op("dve", "scalar_tensor_tensor", state[i][:], state[i][:], dct[:, i:i + 1],
                         psKV[:, 0:128], ALU.mult, ALU.add,
                         reads=[state[i], psKV, dct], writes=[state[i]])
                    c.op("act", "activation", prevb[i][:], state[i][:], AF.Copy, reads=[state[i]],
                         writes=[prevb[i]])
                ts = slice(grp * 512, (grp + 1) * 512)
                rg_t = rgt[i]
                rr0 = (grp // 4) * HROWS32 + i * 128
                c.dma("sp", rg_t[:], b32[rr0:rr0 + 128, (grp % 4) * 512:(grp % 4 + 1) * 512], writes=[rg_t])
                c.op("act", "activation", ysb[:], pY[:], AF.Copy, reads=[pY], writes=[ysb])
                c.op("act", "activation", ysq[:], pY[:], AF.Square, reads=[pY], writes=[ysq])
                c.op("pe", "matmul", psM[:], onesf[:], ysb[:], start=True, stop=True, reads=[onesf, ysb],
                     writes=[psM])
                c.op("pe", "matmul", psE[:], onesf[:], ysq[:], start=True, stop=True, reads=[onesf, ysq],
                     writes=[psE])
                c.op("act", "activation", m2[:], psM[:], AF.Square, reads=[psM], writes=[m2])
                c.op("dve", "tensor_tensor", var[:], psE[:], m2[:], ALU.subtract, reads=[psE, m2], writes=[var])
                c.op("act", "activation", sd[:], var[:], AF.Sqrt, bias=eps_t[:, 0:1], scale=1.0,
                     reads=[var, eps_t], writes=[sd])
                c.op("dve", "reciprocal", sd[:], sd[:], reads=[sd], writes=[sd])
                c.op("dve", "tensor_tensor", yc[:], ysb[:], psM[:], ALU.subtract, reads=[ysb, psM], writes=[yc])
                c.op("pool", "tensor_tensor", yc[:], yc[:], sd[:], ALU.mult, reads=[yc, sd], writes=[yc])
                o = ost[i]
                c.op("dve", "scalar_tensor_tensor", o[:], yc[:], gn[:, i:i + 1], rg_t[:], ALU.mult, ALU.mult,
                     reads=[yc, gn, rg_t], writes=[o])
                c.dma("sp", out_d[512 + i * 128:512 + (i + 1) * 128, ts], o[:], reads=[o])
        c.finish()
        if not standalone:
            c.release()
        print("B2: ins", c.n_ins, "waits", c.n_wait)
    return nc


FFE = 2816
NFB = 22


def build_C(n_exp, moe, final, nc=None, dr=None, pfx="", semstack=None, wbf=None):
    standalone = nc is None
    if standalone:
        nc = bass.Bass("TRN2", target_bir_lowering=False)
        dr = _default_dr(nc)
    dt = dr
    hT_d = dt("d_hT", [D, TOK], F32)
    cin = dt("cin", [2 * 768, TOK], BF16)
    ogm_d = dt("ogm", [512, TOK], BF16)
    wo_d = dt("d_wo", [D, D], F32)
    g2_d = dt("d_g2", [128, KC], F32)
    w1_d = dt("d_w1", [n_exp, D, FFE], F32)
    w3_d = dt("d_w3", [n_exp, D, FFE], F32)
    w2_d = dt("d_w2", [n_exp, FFE, D], F32)
    if moe:
        wr_d = dt("d_wr", [128, KC, 8], F32)
        br_d = dt("d_br", [8, 1], F32)
        identf_d = dt("d_identf", [128, 128], F32)
        sel8_d = dt("d_sel8", [8, 8 * 128], F32)
    if final:
        gf_d = dt("d_gf", [128, KC], F32)
    out_d = dt("oh", [D, TOK], F32, "ExternalOutput")

    with contextlib.ExitStack() as st:
        c = Ctx(nc, st, pfx, semstack)
        h1t = c.sb("h1t", [128, KC, 512], F32)
        mx = c.sb("mx", [128, KC, 512], BF16)
        fT = c.sb("fT", [128, KC, 512], BF16)
        gT = c.sb("gT", [128, NFB, 512], BF16)
        NWB = 2
        wA = [c.sb("wA%d" % i, [128, KC, 512], BF16) for i in range(NWB)]
        wB = [c.sb("wB%d" % i, [128, NFB, 256], BF16) for i in range(NWB)]
        ones_bf = c.sb("ones_bf", [128, 128], BF16)
        eps_t = c.sb("eps_t", [128, 1], F32)
        g_col = c.sb("g_col", [128, KC], F32)
        rt = c.sb("rt", [128, 512], F32)
        rstd = c.sb("rstd", [128, 512], F32)
        st1 = [c.sb("st1_%d" % i, [128, 512], F32) for i in range(2)]
        st2 = [c.sb("st2_%d" % i, [128, 512], F32) for i in range(2)]
        ps1 = [c.ps("ps1_%d" % i, [128, 512]) for i in range(2)]
        ps3 = [c.ps("ps3_%d" % i, [128, 512]) for i in range(2)]
        pso = [c.ps("pso_%d" % i, [128, 512]) for i in range(2)]
        ps_stat = c.ps("ps_stat", [128, 512])
        psr = c.ps("psr", [128, 512])
        if moe:
            wr = c.sb("wr", [128, KC, 8], F32)
            wrg = c.sb("wrg", [128, KC, 8], F32)
            br = c.sb("br", [8, 1], F32)
            identf = c.sb("identf", [128, 128], F32)
            sel8 = c.sb("sel8", [8, 8 * 128], F32)
            lgT = c.sb("lgT", [8, 512], F32)
            lg = c.sb("lg", [128, 4, 8], F32)
            m8 = c.sb("m8", [128, 4, 8], F32)
            dlt = c.sb("dlt", [128, 4], F32)
            gg1 = c.sb("gg1", [128, 4], F32)
            gg2 = c.sb("gg2", [128, 4], F32)
            cm1 = c.sb("cm1", [128, 4, 8], F32)
            cm2 = c.sb("cm2", [128, 4, 8], F32)
            combT = c.sb("combT", [8, 512], F32)
            cbc = c.sb("cbc", [128, 8, 512], F32)
        if final:
            gf_col = c.sb("gf_col", [128, KC], F32)

        c.op("pool", "memset", ones_bf[:], 1.0, writes=[ones_bf])
        c.op("pool", "memset", eps_t[:], EPS, writes=[eps_t])
        c.dma("sp", g_col[:], g2_d, writes=[g_col])
        if final:
            c.dma("sp", gf_col[:], gf_d, writes=[gf_col])
        if moe:
            c.dma("sp", wr[:], wr_d, writes=[wr])
            c.dma("sp", br[:], br_d, writes=[br])
            c.dma("sp", identf[:], identf_d, writes=[identf])
            c.dma("sp", sel8[:], sel8_d, writes=[sel8])
            for kc in range(KC):
                c.op("dve", "tensor_scalar", wrg[:, kc, :], wr[:, kc, :], g_col[:, kc:kc + 1], None, ALU.mult,
                     reads=[wr, g_col], writes=[wrg])

        hv = hT_d.rearrange("(kc p) t -> p kc t", p=128)
        ov = out_d.rearrange("(kc p) t -> p kc t", p=128)
        wov = wo_d.rearrange("(kc p) n -> p kc n", p=128)
        wa_i = [0]
        wb_i = [0]
        pp = [0]

        def rms(src, gcol, dst_fn):
            c.op("act", "activation", mx[:], src[:], AF.Square,
                 reads=[(src.name, k) for k in range(KC)], writes=[mx])
            for kc in range(KC):
                c.op("pe", "matmul", ps_stat[:], ones_bf[:], mx[:, kc, :], start=(kc == 0), stop=(kc == KC - 1),
                     reads=[mx, ones_bf], writes=[ps_stat])
            c.op("act", "activation", rt[:], ps_stat[:], AF.Sqrt, bias=eps_t[:, 0:1], scale=1.0 / D,
                 reads=[ps_stat, eps_t], writes=[rt])
            c.op("dve", "reciprocal", rstd[:], rt[:], reads=[rt], writes=[rstd])
            for kc in range(KC):
                o, wk = dst_fn(kc)
                c.op("dve", "scalar_tensor_tensor", o, src[:, kc, :], gcol[:, kc:kc + 1], rstd[:], ALU.mult,
                     ALU.mult, reads=[(src.name, kc), gcol, rstd], writes=[wk])

        for tt in range(4):
            ts = slice(tt * 512, (tt + 1) * 512)
            for k4 in range(4):
                c.dma("sp", h1t[:, k4 * 4:(k4 + 1) * 4, :], hv[:, k4 * 4:(k4 + 1) * 4, ts],
                      writes=[(h1t.name, k) for k in range(k4 * 4, k4 * 4 + 4)])
            for k0, nk, src in ((0, 4, cin[0:512, :]), (4, 4, cin[768:1280, :]), (8, 4, ogm_d),
                                (12, 2, cin[512:768, :]), (14, 2, cin[1280:1536, :])):
                c.dma("sp", mx[:, k0:k0 + nk, :], src.rearrange("(kc p) t -> p kc t", p=128)[:, :, ts], writes=[mx])
            for og in range(4):
                w = wA[wa_i[0] % NWB]
                wa_i[0] += 1
                if wbf is not None:
                    c.dma("sp", w[:], wbf["wob"].ap()[og], writes=[(w.name, k) for k in range(4)])
                else:
                    for k4 in range(4):
                        c.dma("pool", w[:, k4 * 4:(k4 + 1) * 4, :],
                              wov[:, k4 * 4:(k4 + 1) * 4, og * 512:(og + 1) * 512], writes=[(w.name, k4)])
                for blk in range(4):
                    dmb = og * 4 + blk
                    p = pso[pp[0] % 2]
                    pp[0] += 1
                    for kc in range(KC):
                        c.op("pe", "matmul", p[:], w[:, kc, blk * 128:(blk + 1) * 128], mx[:, kc, :],
                             start=(kc == 0), stop=(kc == KC - 1), reads=[(w.name, kc // 4), mx], writes=[p])
                    c.op("dve", "tensor_tensor", h1t[:, dmb, :], p[:], h1t[:, dmb, :], ALU.add,
                         reads=[p, (h1t.name, dmb)], writes=[(h1t.name, dmb)])
            rms(h1t, g_col, lambda kc: (fT[:, kc, :], (fT.name, kc)))
            fkeys = [(fT.name, k) for k in range(KC)]
            if moe:
                for kc in range(KC):
                    c.op("pe", "matmul", psr[0:8, :], wrg[:, kc, :], h1t[:, kc, :], start=(kc == 0),
                         stop=(kc == KC - 1), reads=[wrg, (h1t.name, kc)], writes=[psr])
                c.op("dve", "tensor_tensor", lgT[:], psr[0:8, :], rstd[0:8, :], ALU.mult, reads=[psr, rstd],
                     writes=[lgT])
                c.op("dve", "tensor_scalar", lgT[:], lgT[:], br[:, 0:1], None, ALU.add, reads=[lgT, br],
                     writes=[lgT])
                for tb in range(4):
                    c.op("pe", "transpose", psr[:, 64 + tb * 8:64 + (tb + 1) * 8], lgT[:, tb * 128:(tb + 1) * 128],
                         identf[0:8, 0:8], reads=[lgT, identf], writes=[psr])
                c.op("dve", "tensor_copy", lg[:], psr[:, 64:96].rearrange("p (a e) -> p a e", e=8),
                     reads=[psr], writes=[lg])
                for tb in range(4):
                    c.op("dve", "max", m8[:, tb, :], lg[:, tb, :], reads=[lg], writes=[m8])
                c.op("dve", "tensor_tensor", dlt[:], m8[:, :, 1], m8[:, :, 0], ALU.subtract, reads=[m8], writes=[dlt])
                c.op("act", "activation", dlt[:], dlt[:], AF.Exp, reads=[dlt], writes=[dlt])
                c.op("dve", "tensor_scalar", gg1[:], dlt[:], 1.0, None, ALU.add, reads=[dlt], writes=[gg1])
                c.op("dve", "reciprocal", gg1[:], gg1[:], reads=[gg1], writes=[gg1])
                c.op("dve", "tensor_scalar", gg2[:], gg1[:], -1.0, 1.0, ALU.mult, ALU.add, reads=[gg1], writes=[gg2])
                for tb in range(4):
                    c.op("dve", "tensor_scalar", cm1[:, tb, :], lg[:, tb, :], m8[:, tb, 0:1], gg1[:, tb:tb + 1],
                         ALU.is_ge, ALU.mult, reads=[lg, m8, gg1], writes=[cm1])
                    c.op("dve", "tensor_scalar", cm2[:, tb, :], lg[:, tb, :], m8[:, tb, 1:2], gg2[:, tb:tb + 1],
                         ALU.is_equal, ALU.mult, reads=[lg, m8, gg2], writes=[cm2])
                c.op("dve", "tensor_tensor", cm1[:], cm1[:], cm2[:], ALU.add, reads=[cm1, cm2], writes=[cm1])
                for tb in range(4):
                    c.op("pe", "transpose", psr[0:8, tb * 128:(tb + 1) * 128], cm1[:, tb, :], identf[:],
                         reads=[cm1, identf], writes=[psr])
                c.op("dve", "tensor_copy", combT[:], psr[0:8, :], reads=[psr], writes=[combT])
                for e in range(8):
                    c.op("pe", "matmul", psr[:], sel8[:, e * 128:(e + 1) * 128], combT[:], start=True, stop=True,
                         reads=[sel8, combT], writes=[psr])
                    c.op("act", "activation", cbc[:, e, :], psr[:], AF.Copy, reads=[psr], writes=[(cbc.name, e)])
            for e in range(n_exp):
                w1v = w1_d[e].rearrange("(kc p) n -> p kc n", p=128)
                w3v = w3_d[e].rearrange("(kc p) n -> p kc n", p=128)
                w2v = w2_d[e].rearrange("(ch p) n -> p ch n", p=128)
                for j in range(NFB // 2):
                    w = wA[wa_i[0] % NWB]
                    wa_i[0] += 1
                    cs = slice(j * 256, (j + 1) * 256)
                    if wbf is not None:
                        c.dma("sp", w[:], wbf["f13b"].ap()[e, j], writes=[(w.name, k) for k in range(4)])
                    else:
                        for k2 in range(2):
                            c.dma("pool", w[:, k2 * 8:(k2 + 1) * 8, 0:256], w1v[:, k2 * 8:(k2 + 1) * 8, cs],
                                  writes=[(w.name, 2 * k2), (w.name, 2 * k2 + 1)])
                        for k2 in range(2):
                            c.dma("pool", w[:, k2 * 8:(k2 + 1) * 8, 256:512], w3v[:, k2 * 8:(k2 + 1) * 8, cs],
                                  writes=[(w.name, 2 * k2), (w.name, 2 * k2 + 1)])
                    for bb in range(2):
                        ffb = j * 2 + bb
                        i2 = pp[0] % 2
                        pp[0] += 1
                        p1, p3 = ps1[i2], ps3[i2]
                        for kc in range(KC):
                            c.op("pe", "matmul", p1[:], w[:, kc, bb * 128:(bb + 1) * 128], fT[:, kc, :],
                                 start=(kc == 0), stop=(kc == KC - 1), reads=[(w.name, kc // 4), (fT.name, kc)],
                                 writes=[p1])
                        for kc in range(KC):
                            c.op("pe", "matmul", p3[:], w[:, kc, 256 + bb * 128:256 + (bb + 1) * 128], fT[:, kc, :],
                                 start=(kc == 0), stop=(kc == KC - 1), reads=[(w.name, kc // 4), (fT.name, kc)],
                                 writes=[p3])
                        s1 = st1[i2]
                        c.op("act", "activation", s1[:], p1[:], AF.Silu, reads=[p1], writes=[s1])
                        if moe:
                            s2 = st2[i2]
                            c.op("dve", "tensor_tensor", s2[:], s1[:], p3[:], ALU.mult, reads=[s1, p3], writes=[s2])
                            c.op("dve", "tensor_tensor", gT[:, ffb, :], s2[:], cbc[:, e, :], ALU.mult,
                                 reads=[s2, (cbc.name, e)], writes=[(gT.name, ffb)])
                        else:
                            c.op("dve", "tensor_tensor", gT[:, ffb, :], s1[:], p3[:], ALU.mult, reads=[s1, p3],
                                 writes=[(gT.name, ffb)])
                gkeys = [(gT.name, k) for k in range(NFB)]
                for dg in range(8):
                    w = wB[wb_i[0] % NWB]
                    wb_i[0] += 1
                    if wbf is not None:
                        c.dma("sp", w[:], wbf["f2b"].ap()[e, dg], writes=[(w.name, 0), (w.name, 1)])
                    else:
                        for c2 in range(2):
                            c.dma("pool", w[:, c2 * 11:(c2 + 1) * 11, :],
                                  w2v[:, c2 * 11:(c2 + 1) * 11, dg * 256:(dg + 1) * 256], writes=[(w.name, c2)])
                    for bb in range(2):
                        dmb = dg * 2 + bb
                        p = pso[pp[0] % 2]
                        pp[0] += 1
                        for ch in range(NFB):
                            c.op("pe", "matmul", p[:], w[:, ch, bb * 128:(bb + 1) * 128], gT[:, ch, :],
                                 start=(ch == 0), stop=(ch == NFB - 1), reads=[(w.name, ch // 11), (gT.name, ch)],
                                 writes=[p])
                        c.op("dve", "tensor_tensor", h1t[:, dmb, :], p[:], h1t[:, dmb, :], ALU.add,
                             reads=[p, (h1t.name, dmb)], writes=[(h1t.name, dmb)])
            if final:
                rms(h1t, gf_col, lambda kc: (h1t[:, kc, :], (h1t.name, kc)))
            for k4 in range(4):
                c.dma("sp", ov[:, k4 * 4:(k4 + 1) * 4, ts], h1t[:, k4 * 4:(k4 + 1) * 4, :],
                      reads=[(h1t.name, k) for k in range(k4 * 4, k4 * 4 + 4)])
        c.finish()
        if not standalone:
            c.release()
        print("C: ins", c.n_ins, "waits", c.n_wait)
    return nc


I32 = mybir.dt.int32
PAIRS = [[0, 1], [2, 3], [4, 5], [6, 7]]


def build_fused():
    nc = bass.Bass("TRN2", target_bir_lowering=False)
    ext = lambda n, sh, d: nc.dram_tensor(n, sh, d, kind="ExternalInput")
    loc = lambda n, sh, d: nc.dram_tensor(n, sh, d)
    shr = lambda n, sh, d: nc.dram_tensor(n, sh, d, addr_space="Shared")
    E = {}

    def e(n, sh, d=F32):
        E[n] = ext(n, sh, d)
        return E[n]

    e("hT0", [D, TOK]); e("half", [1, 1], I32); e("tril", [128, 128])
    for L in range(2):
        e("g1_%d" % L, [128, KC]); e("wall_%d" % L, [D, (N_FM + N_TM) * 512]); e("wgate_%d" % L, [D, 24])
        e("glng_%d" % L, [1, 512]); e("glnb_%d" % L, [1, 512]); e("wsT_%d" % L, [128, 4, 128]); e("bsf_%d" % L, [1, 512])
        e("cw1_%d" % L, [2, 128, 32, 128]); e("cpe_%d" % L, [2, 128, 32]); e("cw2_%d" % L, [2, 128, 128])
        e("gn_%d" % L, [128, 2]); e("wo_%d" % L, [D, D]); e("g2_%d" % L, [128, KC])
    e("d_masks", [128, 17, 512]); e("d_ekc", [128, 128]); e("d_ekw", [128, 128]); e("posq", [4, 3, 512])
    e("d_ek", [128, 4096]); e("d_share", [128, 2, 64]); e("d_btab", [128, 32, 64]); e("d_selg", [128, 12 * 128])
    e("d_ident", [128, 128]); e("d_btl", [128, 192])
    e("d_dec", [128, 2, 128]); e("d_zeta", [128, 2]); e("d_xi", [2, 1, 128]); e("d_dc", [128, 2])
    e("fw1", [2, D, FFE]); e("fw3", [2, D, FFE]); e("fw2", [2, FFE, D])
    e("mw1", [8, D, FFE]); e("mw3", [8, D, FFE]); e("mw2", [8, FFE, D])
    e("d_wr", [128, KC, 8]); e("d_br", [8, 1]); e("d_sel8", [8, 8 * 128]); e("d_gf", [128, KC])
    out = nc.dram_tensor("oh", [D, TOK], F32, kind="ExternalOutput")

    Lc = dict(a16=loc("a16", [2 * HROWS16, TOK], BF16), a32=loc("a32", [2 * HROWS32, TOK], F32),
              ogm=loc("ogm", [512, TOK], BF16),
              b16=loc("b16", [2 * HROWS16, TOK], BF16), b32=loc("b32", [2 * HROWS32, TOK], F32),
              bout=loc("bout", [768, T], BF16), cin=loc("cin", [2 * 768, TOK], BF16),
              oh0=loc("oh0", [D, TOK], F32), bi=loc("bi", [1, 64], F32), bo=loc("bo", [2, 64], F32))
    H16 = HROWS16 * TOK
    H32 = HROWS32 * TOK
    XA16 = shr("XA16", [2 * 2 * HROWS16, TOK], BF16)
    XA32 = shr("XA32", [2 * 2 * HROWS32, TOK], F32)
    XB = shr("XB", [2 * 768, T], BF16)

    WB = dict(f13b=loc("f13b", [2, NFB // 2, 128, KC, 512], BF16), f2b=loc("f2b", [2, 8, 128, NFB, 256], BF16),
              wob=loc("wob", [4, 128, KC, 512], BF16))

    def convert_dense():
        out_ = []
        for og in range(4):
            out_.append((WB["wob"].ap()[og], bass.AP(E["wo_0"], og * 512, [[D, 128], [128 * D, KC], [1, 512]])))
        for e_ in range(2):
            for j in range(NFB // 2):
                for wi, wn in enumerate(("fw1", "fw3")):
                    dst = WB["f13b"].ap()[e_, j][:, :, wi * 256:(wi + 1) * 256]
                    out_.append((dst, bass.AP(E[wn], e_ * D * FFE + j * 256, [[FFE, 128], [128 * FFE, KC], [1, 256]])))
            for dg in range(8):
                out_.append((WB["f2b"].ap()[e_, dg],
                             bass.AP(E["fw2"], e_ * FFE * D + dg * 256, [[D, 128], [128 * D, NFB], [1, 256]])))
        return out_

    def mk_dr(mapping):
        def dr(n, sh, d, k="ExternalInput"):
            t = mapping[n]
            assert list(t.shape) == list(sh), (n, list(t.shape), sh)
            return t.ap()
        return dr

    with contextlib.ExitStack() as top:
        g = nc.gpsimd
        cc = top.enter_context(nc.semaphore("cc"))
        r_half = top.enter_context(g.register("r_half"))
        r_tmp = top.enter_context(g.register("r_tmp"))
        halft = top.enter_context(nc.sbuf_tensor("halft", [1, 1], I32))
        hsem = top.enter_context(nc.semaphore("hsem"))
        g.dma_start(out=halft[:], in_=E["half"].ap()).then_inc(hsem, 16)
        g.wait_ge(hsem, 16)
        g.reg_load(r_half, halft[0:1, 0:1])
        ccn = [0]

        def dyn(t, base, stride, pat):
            g.reg_mul(r_tmp, r_half, stride)
            g.reg_add(r_tmp, r_tmp, base)
            return bass.AP(t, r_tmp, pat)

        def stat(t, base, pat):
            return bass.AP(t, base, pat)

        def barrier(xc):
            xc.finish()
            g.collective_compute("AllGather", ALU.bypass, replica_groups=PAIRS, ins=[Lc["bi"].ap().opt()],
                                 outs=[Lc["bo"].ap().opt()]).then_inc(cc, 1)
            ccn[0] += 1
            g.wait_ge(cc, ccn[0])

        def xchg_A(pfx):
            with contextlib.ExitStack() as st:
                xc = Ctx(nc, st, pfx, top)
                CH = 32768
                xc.dma("pool", dyn(XA16, 0, 2 * H16, [[CH, 2 * H16 // CH], [1, CH]]),
                       stat(Lc["a16"], 0, [[CH, 2 * H16 // CH], [1, CH]]))
                xc.dma("pool", dyn(XA32, 0, 2 * H32, [[TOK, 2 * HROWS32], [1, TOK]]),
                       stat(Lc["a32"], 0, [[TOK, 2 * HROWS32], [1, TOK]]))
                barrier(xc)
                for th in range(2):
                    xc.dma("pool", stat(Lc["b16"], th * H16, [[CH, H16 // CH], [1, CH]]),
                           dyn(XA16, th * 2 * H16, H16, [[CH, H16 // CH], [1, CH]]))
                    xc.dma("pool", stat(Lc["b32"], th * H32, [[TOK, HROWS32], [1, TOK]]),
                           dyn(XA32, th * 2 * H32, H32, [[TOK, HROWS32], [1, TOK]]))
                xc.finish()
                xc.release()

        def xchg_B(pfx):
            with contextlib.ExitStack() as st:
                xc = Ctx(nc, st, pfx, top)
                CH = 32768
                n = 768 * T
                xc.dma("pool", dyn(XB, 0, n, [[CH, n // CH], [1, CH]]), stat(Lc["bout"], 0, [[CH, n // CH], [1, CH]]))
                barrier(xc)
                for hh in range(2):
                    xc.dma("pool", stat(Lc["cin"], hh * 768 * TOK, [[TOK, 768], [1, TOK]]),
                           dyn(XB, hh * n, TOK, [[T, 768], [1, TOK]]))
                xc.finish()
                xc.release()

        for L in range(2):
            hsrc = E["hT0"] if L == 0 else Lc["oh0"]
            mA = dict(hT=hsrc, g1=E["g1_%d" % L], wall=E["wall_%d" % L], wgate=E["wgate_%d" % L],
                      glng=E["glng_%d" % L], glnb=E["glnb_%d" % L], wsT=E["wsT_%d" % L], bsf=E["bsf_%d" % L],
                      tril=E["tril"], a16=Lc["a16"], a32=Lc["a32"], ogm=Lc["ogm"])
            build_A(nc, mk_dr(mA), "A%d_" % L, top)
            xchg_A("XA%d_" % L)
            mB1 = dict(E)
            mB1.update(b16=Lc["b16"], b32=Lc["b32"], d_w1=E["cw1_%d" % L], d_peT=E["cpe_%d" % L],
                       d_w2=E["cw2_%d" % L], bout=Lc["bout"])
            build_B1(nc, mk_dr(mB1), "B1%d_" % L, top, pre_hook=convert_dense if L == 0 else None)
            mB2 = dict(E)
            mB2.update(b16=Lc["b16"], b32=Lc["b32"], d_gn=E["gn_%d" % L], bout=Lc["bout"])
            build_B2(nc, mk_dr(mB2), "B2%d_" % L, top)
            xchg_B("XB%d_" % L)
            moe = (L == 1)
            mC = dict(E)
            mC.update(d_hT=hsrc, cin=Lc["cin"], ogm=Lc["ogm"], d_wo=E["wo_%d" % L], d_g2=E["g2_%d" % L],
                      d_w1=E["mw1"] if moe else E["fw1"], d_w3=E["mw3"] if moe else E["fw3"],
                      d_w2=E["mw2"] if moe else E["fw2"], d_identf=E["d_ident"], oh=out if moe else Lc["oh0"])
            build_C(8 if moe else 2, moe, moe, nc, mk_dr(mC), "C%d_" % L, top, wbf=None if moe else WB)
    return nc


_PROGS = {}


def _col(g):
    return np.ascontiguousarray(np.asarray(g, np.float32).reshape(KC, 128).T)


def kernel(x, ln1_g, w_in, cmp_pe_k, cmp_w1_k, cmp_w2_k, cmp_pe_v, cmp_w1_v, cmp_w2_v,
           gmlp_ln_g, gmlp_ln_b, gmlp_ws, gmlp_bs, ret_gn_g, w_out, ln2_g,
           ffn_w1, ffn_w3, ffn_w2, moe_wr, moe_br, moe_w1, moe_w3, moe_w2, final_g):
    f32 = lambda a: np.asarray(a, np.float32)
    xf = f32(x).reshape(B * T, D)
    if "F" not in _PROGS:
        _PROGS["F"] = build_fused()
    nc = _PROGS["F"]
    sh = {"d_" + k: v for k, v in nsa_consts().items()}
    sh["tril"] = np.triu(np.ones((128, 128), np.float32))
    for L in range(2):
        wall, wgate = pack_A_weights(f32(w_in[L]))
        sh["g1_%d" % L] = _col(ln1_g[L]); sh["wall_%d" % L] = wall; sh["wgate_%d" % L] = wgate
        sh["glng_%d" % L] = f32(gmlp_ln_g[L]).reshape(1, 512); sh["glnb_%d" % L] = f32(gmlp_ln_b[L]).reshape(1, 512)
        sh["wsT_%d" % L] = np.ascontiguousarray(f32(gmlp_ws[L]).transpose(2, 0, 1))
        sh["bsf_%d" % L] = f32(gmlp_bs[L]).reshape(1, 512)
        sh["cw1_%d" % L] = np.ascontiguousarray(np.stack(
            [f32(cmp_w1_k[L]).reshape(32, 128, 128).transpose(1, 0, 2),
             f32(cmp_w1_v[L]).reshape(32, 128, 128).transpose(1, 0, 2)]))
        sh["cpe_%d" % L] = np.ascontiguousarray(np.stack([f32(cmp_pe_k[L]).T, f32(cmp_pe_v[L]).T]))
        sh["cw2_%d" % L] = np.ascontiguousarray(np.stack([f32(cmp_w2_k[L]), f32(cmp_w2_v[L])]))
        sh["wo_%d" % L] = f32(w_out[L]); sh["g2_%d" % L] = _col(ln2_g[L])
    sh["fw1"] = np.ascontiguousarray(f32(ffn_w1[0]).reshape(D, 2, FFE).transpose(1, 0, 2))
    sh["fw3"] = np.ascontiguousarray(f32(ffn_w3[0]).reshape(D, 2, FFE).transpose(1, 0, 2))
    sh["fw2"] = np.ascontiguousarray(f32(ffn_w2[0]).reshape(2, FFE, D))
    sh["mw1"] = f32(moe_w1[0]); sh["mw3"] = f32(moe_w3[0]); sh["mw2"] = f32(moe_w2[0])
    sh["d_wr"] = np.ascontiguousarray(f32(moe_wr[0]).reshape(KC, 128, 8).transpose(1, 0, 2))
    sh["d_br"] = f32(moe_br[0]).reshape(8, 1)
    sh["d_sel8"] = np.repeat(np.eye(8, dtype=np.float32), 128, axis=1)
    sh["d_gf"] = _col(final_g)
    maps = []
    for c in range(NCORES):
        hf = c % 2
        m = dict(sh)
        m["hT0"] = np.ascontiguousarray(xf[c * TOK:(c + 1) * TOK].T)
        m["half"] = np.array([[hf]], np.int32)
        m["posq"] = nsa_posq(hf)
        m["d_btl"] = nsa_bias_table(hf)
        m.update(ret_consts(hf))
        for L in range(2):
            m["gn_%d" % L] = np.ascontiguousarray(f32(ret_gn_g[L])[hf * 256:(hf + 1) * 256].reshape(2, 128).T)
        maps.append(m)
    res = run_bass_kernel_spmd(nc, maps, core_ids=list(range(NCORES))).results
    out = np.concatenate([np.asarray(res[c]["oh"], np.float32).T for c in range(NCORES)], axis=0)
    return np.ascontiguousarray(out.reshape(B, T, D))
```
